# Optimizing a Trainium2 kernel written in Bass

```python
import math
import jax
import jax.numpy as jnp
from jax import lax
import numpy as np


D_MODEL = 1024
BATCH = 16
SEQ = 2048
DEPTH = 2

MIX_WIDTH = D_MODEL
DA_HEADS = 4
DA_QK_DIM = 64
DA_V_DIM = 2 * DA_QK_DIM
DA_WIDTH = DA_HEADS * DA_V_DIM
GLA_HEADS = 4
GLA_V_DIM = 64
GLA_K_DIM = GLA_V_DIM // 2
GLA_WIDTH = GLA_HEADS * GLA_V_DIM
GLA_GATE_RANK = 16
GLA_GATE_NORM = 16.0
GLA_CHUNK = 64
POOL_WINDOWS = (2, 4, 8, 16)
POOL_GROUPS = 4
POOL_WIDTH = MIX_WIDTH - DA_WIDTH - GLA_WIDTH
POOL_GROUP_DIM = POOL_WIDTH // POOL_GROUPS
DA_Q_COLS = DA_HEADS * 2 * DA_QK_DIM
DA_K_COLS = DA_HEADS * 2 * DA_QK_DIM
DA_V_COLS = DA_WIDTH
GLA_Q_COLS = GLA_HEADS * GLA_K_DIM
GLA_K_COLS = GLA_HEADS * GLA_K_DIM
GLA_V_COLS = GLA_WIDTH
GLA_R_COLS = GLA_WIDTH
IN_SIZES = (DA_Q_COLS, DA_K_COLS, DA_V_COLS, GLA_Q_COLS, GLA_K_COLS, GLA_V_COLS, GLA_GATE_RANK, GLA_R_COLS, POOL_WIDTH)
IN_TOTAL = sum(IN_SIZES)
FFN_HIDDEN = -(-8 * D_MODEL // (3 * 256)) * 256
REL_BUCKETS = 32
REL_MAX_DIST = 128
Q_BLOCK = 128
EPS = 1e-6

kernel_name = 'hymba_style_diffattn_gla_pool_hybrid'


def rms_norm(x, g):
    xf = x.astype(jnp.float32)
    y = xf * lax.rsqrt(jnp.mean(xf * xf, axis=-1, keepdims=True) + EPS)
    return (y * g.astype(jnp.float32)).astype(x.dtype)


def t5_bucket(dist):
    max_exact = REL_BUCKETS // 2
    d = jnp.maximum(dist, 0)
    large = max_exact + (jnp.log(jnp.maximum(d, 1).astype(jnp.float32) / max_exact)
                         / math.log(REL_MAX_DIST / max_exact) * (REL_BUCKETS - max_exact)).astype(jnp.int32)
    large = jnp.minimum(large, REL_BUCKETS - 1)
    return jnp.where(d < max_exact, d, large)


def diff_attention(q, k, v, lam, bias_by_dist):
    S = q.shape[1]
    scale = DA_QK_DIM ** -0.5
    outs = []
    for i in range(S // Q_BLOCK):
        q0 = i * Q_BLOCK
        kend = q0 + Q_BLOCK
        qb = q[:, q0:kend]
        kb = k[:, :kend]
        vb = v[:, :kend]
        s = jnp.einsum('bqhmd,bkhmd->bhmqk', qb, kb).astype(jnp.float32) * scale
        dist = jnp.arange(q0, kend)[:, None] - jnp.arange(kend)[None, :]
        bias = bias_by_dist[jnp.clip(dist, 0, S - 1)].astype(jnp.float32)
        s = s + jnp.transpose(bias, (2, 0, 1))[None, :, None]
        s = jnp.where(dist[None, None, None] >= 0, s, -1e30)
        p = jax.nn.softmax(s, axis=-1)
        w = p[:, :, 0] - lam * p[:, :, 1]
        outs.append(jnp.einsum('bhqk,bkhd->bqhd', w.astype(v.dtype), vb))
    return jnp.concatenate(outs, axis=1)


def gla_chunked(q, k, v, log_a):
    B, S, H, dk = q.shape
    dv = v.shape[-1]
    C = GLA_CHUNK
    N = S // C

    def chunkify(t):
        return t.astype(jnp.float32).reshape(B, N, C, H, t.shape[-1]).transpose(1, 0, 3, 2, 4)

    qc = chunkify(q) * (dk ** -0.5)
    kc = chunkify(k)
    vc = chunkify(v)
    b = jnp.cumsum(chunkify(log_a), axis=3)
    q_dec = qc * jnp.exp(b)
    k_inv = kc * jnp.exp(-b)
    k_end = kc * jnp.exp(b[..., -1:, :] - b)
    chunk_decay = jnp.exp(b[..., -1, :])
    causal = jnp.tril(jnp.ones((C, C), jnp.float32))
    attn = jnp.einsum('nbhqd,nbhkd->nbhqk', q_dec, k_inv) * causal
    o_intra = jnp.einsum('nbhqk,nbhkv->nbhqv', attn, vc)

    def step(state, inp):
        q_c, k_c, v_c, dec_c = inp
        o_inter = jnp.einsum('bhqd,bhdv->bhqv', q_c, state)
        state = state * dec_c[..., None] + jnp.einsum('bhkd,bhkv->bhdv', k_c, v_c)
        return state, o_inter

    state0 = jnp.zeros((B, H, dk, dv), jnp.float32)
    _, o_inter = lax.scan(step, state0, (q_dec, k_end, vc, chunk_decay))
    o = o_intra + o_inter
    return o.transpose(1, 0, 3, 2, 4).reshape(B, S, H, dv).astype(v.dtype)


def multiscale_pool(u, pool_w, pool_scale):
    B, S, _ = u.shape
    uf = u.astype(jnp.float32)
    cs = jnp.concatenate([jnp.zeros((B, 1, POOL_WIDTH), jnp.float32), jnp.cumsum(uf, axis=1)], axis=1)
    t = jnp.arange(S)
    outs = []
    for g, win in enumerate(POOL_WINDOWS):
        lo, hi = g * POOL_GROUP_DIM, (g + 1) * POOL_GROUP_DIM
        csg = cs[..., lo:hi]
        prev = jnp.concatenate([jnp.zeros((B, win - 1, POOL_GROUP_DIM), jnp.float32), csg[:, :S + 1 - win]], axis=1)
        count = jnp.minimum(t + 1, win).astype(jnp.float32)
        pooled = (csg[:, 1:] - prev) / count[None, :, None] - uf[..., lo:hi]
        outs.append(jnp.einsum('bsc,cd->bsd', pooled, pool_w[g].astype(jnp.float32)))
    y = jnp.concatenate(outs, axis=-1) * pool_scale.astype(jnp.float32)
    return y.astype(u.dtype)


def setup_inputs(seed: int = 0) -> dict:
    key = jax.random.key(seed)
    ks = jax.random.split(key, 18)
    f32 = jnp.float32

    def nrm(k, shape, scale):
        return jax.random.normal(k, shape, f32) * scale

    def gain(k, shape):
        return 1.0 + 0.1 * jax.random.normal(k, shape, f32)

    return {
        'x': nrm(ks[0], (BATCH, SEQ, D_MODEL), 1.0),
        'attn_norm_g': gain(ks[1], (DEPTH, D_MODEL)),
        'w_in': nrm(ks[2], (DEPTH, D_MODEL, IN_TOTAL), D_MODEL ** -0.5),
        'q_norm_g': gain(ks[3], (DEPTH, DA_QK_DIM)),
        'k_norm_g': gain(ks[4], (DEPTH, DA_QK_DIM)),
        'lambda_vecs': nrm(ks[5], (DEPTH, 4, DA_QK_DIM), 0.1),
        'da_subln_g': gain(ks[6], (DEPTH, DA_V_DIM)),
        'rel_bias': nrm(ks[7], (REL_BUCKETS, DA_HEADS), 0.5),
        'gla_gate_w': nrm(ks[8], (DEPTH, GLA_GATE_RANK, GLA_HEADS * GLA_K_DIM), GLA_GATE_RANK ** -0.5),
        'gla_gate_b': nrm(ks[9], (DEPTH, GLA_HEADS * GLA_K_DIM), 0.1),
        'gla_norm_g': gain(ks[10], (DEPTH, GLA_V_DIM)),
        'pool_w': nrm(ks[11], (DEPTH, POOL_GROUPS, POOL_GROUP_DIM, POOL_GROUP_DIM), POOL_GROUP_DIM ** -0.5),
        'pool_scale': gain(ks[12], (DEPTH, POOL_WIDTH)),
        'w_out': nrm(ks[13], (DEPTH, MIX_WIDTH, D_MODEL), MIX_WIDTH ** -0.5),
        'ffn_norm_g': gain(ks[14], (DEPTH, D_MODEL)),
        'w_gate': nrm(ks[15], (DEPTH, D_MODEL, FFN_HIDDEN), D_MODEL ** -0.5),
        'w_up': nrm(ks[16], (DEPTH, D_MODEL, FFN_HIDDEN), D_MODEL ** -0.5),
        'w_down': nrm(ks[17], (DEPTH, FFN_HIDDEN, D_MODEL), FFN_HIDDEN ** -0.5),
    }


def reference(x, attn_norm_g, w_in, q_norm_g, k_norm_g, lambda_vecs, da_subln_g, rel_bias,
              gla_gate_w, gla_gate_b, gla_norm_g, pool_w, pool_scale, w_out,
              ffn_norm_g, w_gate, w_up, w_down):
    B, S, _ = x.shape
    split_points = np.cumsum(IN_SIZES)[:-1].tolist()
    bias_by_dist = rel_bias[t5_bucket(jnp.arange(S))]
    h = x
    for l in range(DEPTH):
        hn = rms_norm(h, attn_norm_g[l])
        proj = jnp.einsum('bsd,de->bse', hn, w_in[l])
        da_q, da_k, da_v, g_q, g_k, g_v, g_lr, g_r, pool_u = jnp.split(proj, split_points, axis=-1)

        q = rms_norm(da_q.reshape(B, S, DA_HEADS, 2, DA_QK_DIM), q_norm_g[l])
        k = rms_norm(da_k.reshape(B, S, DA_HEADS, 2, DA_QK_DIM), k_norm_g[l])
        v = da_v.reshape(B, S, DA_HEADS, DA_V_DIM)
        lv = lambda_vecs[l].astype(jnp.float32)
        lam_init = 0.8 - 0.6 * math.exp(-0.3 * l)
        lam = jnp.exp(jnp.sum(lv[0] * lv[1])) - jnp.exp(jnp.sum(lv[2] * lv[3])) + lam_init
        a_out = diff_attention(q, k, v, lam, bias_by_dist)
        a_out = rms_norm(a_out, da_subln_g[l]) * (1.0 - lam_init)
        a_out = a_out.reshape(B, S, DA_WIDTH).astype(h.dtype)

        z = jnp.einsum('bsr,rk->bsk', g_lr, gla_gate_w[l]) + gla_gate_b[l]
        log_a = jax.nn.log_sigmoid(z.astype(jnp.float32)) / GLA_GATE_NORM
        o = gla_chunked(g_q.reshape(B, S, GLA_HEADS, GLA_K_DIM),
                        g_k.reshape(B, S, GLA_HEADS, GLA_K_DIM),
                        g_v.reshape(B, S, GLA_HEADS, GLA_V_DIM),
                        log_a.reshape(B, S, GLA_HEADS, GLA_K_DIM))
        o = rms_norm(o, gla_norm_g[l]).reshape(B, S, GLA_WIDTH)
        b_out = (o * jax.nn.silu(g_r)).astype(h.dtype)

        c_out = multiscale_pool(pool_u, pool_w[l], pool_scale[l]).astype(h.dtype)

        mixed = jnp.concatenate([a_out, b_out, c_out], axis=-1)
        h = h + jnp.einsum('bse,ed->bsd', mixed, w_out[l])

        hn = rms_norm(h, ffn_norm_g[l])
        ff = jax.nn.silu(jnp.einsum('bsd,df->bsf', hn, w_gate[l])) * jnp.einsum('bsd,df->bsf', hn, w_up[l])
        h = h + jnp.einsum('bsf,fd->bsd', ff, w_down[l])
    return h
```

```python
import math
from contextlib import ExitStack

import numpy as np
import concourse.bass as bass
import concourse.mybir as mybir
from concourse.bass_utils import run_bass_kernel_spmd

F32 = mybir.dt.float32
BF16 = mybir.dt.bfloat16
AF = mybir.ActivationFunctionType
ALU = mybir.AluOpType
AX = mybir.AxisListType

D = 1024
DEPTH = 2
FFN = 2816
IN_TOTAL = 2576
N_CORES = 8
EPS = 1e-6
NEG = -30000.0
NPV = 26
ENGINES = ("pe", "act", "dve", "pool", "sp")
SAME_ENGINE_SYNC = True


def _esz(dt):
    s = str(dt)
    if "64" in s:
        return 8
    if "32" in s:
        return 4
    if "16" in s:
        return 2
    return 1


def region(ap):
    pat = ap.ap
    off = int(ap.offset)
    esz = _esz(ap.dtype)
    name = ap.tensor.name
    sp = str(ap.space).upper()
    if "DRAM" in sp or "HBM" in sp:
        lo = hi = off
        for s, c in pat:
            if s >= 0:
                hi += s * (c - 1)
            else:
                lo += s * (c - 1)
        return (name, 0, 1, lo * esz, (hi + 1) * esz)
    pstep, pcnt = pat[0]
    p0 = off // pstep
    f0 = off % pstep
    lo = hi = f0
    for s, c in pat[1:]:
        if s >= 0:
            hi += s * (c - 1)
        else:
            lo += s * (c - 1)
    if "PSUM" in sp:
        b0 = (lo * esz) // 2048 * 2048
        b1 = ((hi + 1) * esz + 2047) // 2048 * 2048
        return ("@" + name, 0, 128, b0, b1)
    return (name, p0, p0 + pcnt, lo * esz, (hi + 1) * esz)


class Op:
    __slots__ = ("eng", "fn", "deps", "needs_inc", "tick", "is_dma", "grp", "grp_val", "waits")

    def __init__(self, eng, fn):
        self.eng = eng
        self.fn = fn
        self.deps = set()
        self.needs_inc = False
        self.tick = None
        self.is_dma = False
        self.grp = None
        self.grp_val = None
        self.waits = None


class DmaGroup:
    def __init__(self, name):
        self.name = name
        self.sem = None
        self.count = 0
        self.final = False


class Prog:
    def __init__(self):
        self.ops = {e: [] for e in ENGINES}
        self.res = {}
        self.groups = []

    def group(self, name, final=False):
        g = DmaGroup(name)
        g.final = final
        self.groups.append(g)
        return g

    def _track(self, op, reads, writes):
        rregs = [region(a) for a in reads]
        wregs = [region(a) for a in writes]
        for (name, p0, p1, b0, b1) in rregs:
            psum = name[0] == "@"
            for e in self.res.setdefault(name, []):
                if (e[4] == "w" or (psum and e[5].eng != op.eng)) and e[0] < p1 and p0 < e[1] and e[2] < b1 and b0 < e[3]:
                    op.deps.add(e[5])
        for (name, p0, p1, b0, b1) in wregs:
            for e in self.res.setdefault(name, []):
                if e[0] < p1 and p0 < e[1] and e[2] < b1 and b0 < e[3]:
                    op.deps.add(e[5])
        op.deps.discard(op)
        for (name, p0, p1, b0, b1) in rregs:
            lst = self.res[name]
            if not op.is_dma:
                lst[:] = [e for e in lst if not (e[4] == "r" and e[5].eng == op.eng and not e[5].is_dma
                                                 and p0 <= e[0] and e[1] <= p1 and b0 <= e[2] and e[3] <= b1)]
            lst.append([p0, p1, b0, b1, "r", op])
        for (name, p0, p1, b0, b1) in wregs:
            lst = self.res[name]
            lst[:] = [e for e in lst if not (p0 <= e[0] and e[1] <= p1 and b0 <= e[2] and e[3] <= b1)]
            lst.append([p0, p1, b0, b1, "w", op])

    def op(self, eng, fn, reads=(), writes=()):
        o = Op(eng, fn)
        self._track(o, reads, writes)
        self.ops[eng].append(o)
        return o

    def dma(self, queue, out, in_, grp, **kw):
        o = Op(queue, lambda e: e.dma_start(out=out, in_=in_, **kw))
        o.is_dma = True
        o.grp = grp
        grp.count += 16
        o.grp_val = grp.count
        self._track(o, [in_], [out])
        self.ops[queue].append(o)
        return o

    def mm(self, out, lhsT, rhs, start=True, stop=True):
        return self.op("pe", lambda e: e.matmul(out, lhsT=lhsT, rhs=rhs, start=start, stop=stop),
                       [lhsT, rhs], [out])

    def transpose(self, out, in_, ident):
        return self.op("pe", lambda e: e.transpose(out, in_, ident), [in_, ident], [out])

    def act(self, out, in_, func, bias=None, scale=None):
        kw = {}
        reads = [in_]
        if bias is not None:
            kw["bias"] = bias
            if not isinstance(bias, (int, float)):
                reads.append(bias)
        if scale is not None:
            kw["scale"] = scale
            if not isinstance(scale, (int, float)):
                reads.append(scale)
        return self.op("act", lambda e: e.activation(out=out, in_=in_, func=func, **kw), reads, [out])

    def tt(self, eng, out, in0, in1, op):
        return self.op(eng, lambda e: e.tensor_tensor(out=out, in0=in0, in1=in1, op=op), [in0, in1], [out])

    def ts(self, eng, out, in0, s1, op0, s2=None, op1=None):
        reads = [in0]
        if not isinstance(s1, (int, float)):
            reads.append(s1)
        if s2 is not None and not isinstance(s2, (int, float)):
            reads.append(s2)
        if op1 is None:
            return self.op(eng, lambda e: e.tensor_scalar(out=out, in0=in0, scalar1=s1, scalar2=None, op0=op0),
                           reads, [out])
        return self.op(eng, lambda e: e.tensor_scalar(out=out, in0=in0, scalar1=s1, scalar2=s2, op0=op0, op1=op1),
                       reads, [out])

    def stt(self, out, in0, scalar, in1, op0, op1):
        reads = [in0, in1]
        if not isinstance(scalar, (int, float)):
            reads.append(scalar)
        return self.op("dve", lambda e: e.scalar_tensor_tensor(out=out, in0=in0, scalar=scalar, in1=in1,
                                                                 op0=op0, op1=op1), reads, [out])

    def copy(self, eng, out, in_):
        if eng == "act":
            return self.op(eng, lambda e: e.copy(out=out, in_=in_), [in_], [out])
        return self.op(eng, lambda e: e.tensor_copy(out=out, in_=in_), [in_], [out])

    def memset(self, eng, ap, val):
        return self.op(eng, lambda e: e.memset(ap, val), [], [ap])

    def recip(self, out, in_):
        return self.op("dve", lambda e: e.reciprocal(out=out, in_=in_), [in_], [out])

    def emit(self, nc, es):
        def skip(d, o):
            return d.eng == o.eng and not o.is_dma and (o.eng == "pe" or not SAME_ENGINE_SYNC)

        for e in ENGINES:
            for o in self.ops[e]:
                for d in o.deps:
                    if d.is_dma or skip(d, o):
                        continue
                    d.needs_inc = True
        esem = {e: es.enter_context(nc.semaphore("sem_" + e)) for e in ENGINES}
        for g in self.groups:
            g.sem = es.enter_context(nc.semaphore("dg_" + g.name))
        for e in ENGINES:
            t = 0
            for o in self.ops[e]:
                if o.needs_inc and not o.is_dma:
                    t += 1
                    o.tick = t
        for e in ENGINES:
            seen = {}
            for o in self.ops[e]:
                w = {}
                for d in o.deps:
                    if d.is_dma:
                        key = ("g", id(d.grp))
                        sem, val = d.grp.sem, d.grp_val
                    else:
                        if skip(d, o):
                            continue
                        key = ("e", d.eng)
                        sem, val = esem[d.eng], d.tick
                    if seen.get(key, 0) >= val:
                        continue
                    if key not in w or w[key][1] < val:
                        w[key] = (sem, val)
                for key, (sem, val) in w.items():
                    seen[key] = val
                o.waits = list(w.values())
        engobj = {"pe": "tensor", "act": "scalar", "dve": "vector", "pool": "gpsimd", "sp": "sync"}
        finals = [(g.sem, g.count) for g in self.groups if g.count > 0 and g.final]
        with nc.Block() as block:
            for e in ENGINES:
                def body(eng, ops=self.ops[e], e=e):
                    for o in ops:
                        for sem, val in o.waits:
                            eng.wait_ge(sem, val)
                        ins = o.fn(eng)
                        if o.is_dma:
                            ins.then_inc(o.grp.sem, 16)
                        elif o.needs_inc:
                            ins.then_inc(esem[e], 1)
                    if e == "sp":
                        for sem, val in finals:
                            eng.wait_ge(sem, val)

                getattr(block, engobj[e])(body)


def _t5_bucket_np(d):
    d = np.maximum(d, 0)
    max_exact = 16
    large = max_exact + (np.log(np.maximum(d, 1).astype(np.float32) / max_exact)
                         / math.log(128 / max_exact) * (32 - max_exact)).astype(np.int32)
    large = np.minimum(large, 31)
    return np.where(d < max_exact, d, large)


F_IDENT = 0
F_MASKNEG = 128
F_TRICAT = 256
F_U = 386
F_INVW = 514
F_INVC = 516
NCF = 548
B_GLAMASK = 0
B_BLK64 = 512
B_ONES = 640
B_BDMASK = 768
B_HM = 1024
B_HMF = 1028
NCB = 1540


def _consts():
    cf = np.zeros((128, NCF), np.float32)
    cb = np.zeros((128, NCB), np.float32)
    i = np.arange(128)
    cf[:, F_IDENT:F_IDENT + 128] = np.eye(128, dtype=np.float32)
    cf[:, F_MASKNEG:F_MASKNEG + 128] = np.where(i[:, None] > i[None, :], NEG, 0.0)
    same = (i[:, None] // 64) == (i[None, :] // 64)
    gm = (same & (i[:, None] <= i[None, :])).astype(np.float32)
    cb[:, B_GLAMASK:B_GLAMASK + 512] = np.tile(gm, (1, 4))
    cf[:, F_TRICAT:F_TRICAT + 128] = gm * (-1.0 / 16.0)
    cf[:, F_TRICAT + 128] = np.where(i < 64, -1.0 / 16.0, 0.0)
    cf[:, F_TRICAT + 129] = np.where(i >= 64, -1.0 / 16.0, 0.0)
    cf[:, F_U:F_U + 128] = (same & (i[:, None] > i[None, :])).astype(np.float32) * (-1.0 / 16.0)
    cb[:, B_BLK64:B_BLK64 + 128] = same.astype(np.float32)
    hrow = i // 32
    hcol = np.arange(256) // 64
    cb[:, B_BDMASK:B_BDMASK + 256] = (hrow[:, None] == hcol[None, :]).astype(np.float32)
    wins = (2, 4, 8, 16)
    for pt in range(2):
        for half in range(2):
            win = wins[pt * 2 + half]
            rows = slice(half * 64, half * 64 + 64)
            cf[rows, F_INVW + pt] = 1.0 / win
            t = np.arange(16)
            cf[rows, F_INVC + pt * 16:F_INVC + pt * 16 + 16] = 1.0 / np.minimum(t + 1, win)
    cb[:, B_ONES:B_ONES + 128] = 1.0
    cb[:, B_HM:B_HM + 4] = (hrow[:, None] == np.arange(4)[None, :]).astype(np.float32)
    hf = (np.arange(4)[:, None] == (np.arange(128) // 32)[None, :]).astype(np.float32).reshape(1, 512)
    cb[:, B_HMF:B_HMF + 512] = hf
    return cf, cb


def _bias_index():
    k = np.arange(128)[:, None]
    q = np.arange(128)[None, :]
    d0 = np.clip(q - k, 0, None)
    d1 = q - k + 128
    idx = np.stack([_t5_bucket_np(d0), _t5_bucket_np(d1)], axis=1)
    return idx


def build_program(S, NSEQ, layers):
    NT = S // 128
    NB = S // 512
    nc = bass.Bass("TRN2", target_bir_lowering=False)
    dt_in = lambda n, shp: nc.dram_tensor(n, shp, F32, kind="ExternalInput").ap()
    x = dt_in("x", [NSEQ, S, D])
    w_in = dt_in("w_in", [DEPTH, D, IN_TOTAL])
    w_out = dt_in("w_out", [DEPTH, D, D])
    w_gate = dt_in("w_gate", [DEPTH, D, FFN])
    w_up = dt_in("w_up", [DEPTH, D, FFN])
    w_down = dt_in("w_down", [DEPTH, FFN, D])
    pvec_d = dt_in("pvec", [DEPTH, 128, NPV])
    gw17_d = dt_in("gw17", [DEPTH, 17, 128])
    pwbd_d = dt_in("pwbd", [DEPTH, 128, 256])
    biasT_d = dt_in("biasT", [128, 4 * 2 * 128])
    cstf_d = dt_in("cstf", [128, NCF])
    cstb_d = dt_in("cstb", [128, NCB])
    lvb_d = dt_in("lvb", [DEPTH, 128, 256])
    out = nc.dram_tensor("out", [NSEQ, S, D], F32, kind="ExternalOutput").ap()

    P = Prog()
    KB = 1024
    ARENA = 196 * KB
    with ExitStack() as es:
        arena = es.enter_context(nc.sbuf_tensor("arena", [128, ARENA // 2], BF16))
        cst = es.enter_context(nc.sbuf_tensor("cstf_sb", [128, NCF], F32))
        cstb = es.enter_context(nc.sbuf_tensor("cstb_sb", [128, NCB], BF16))
        biasT = es.enter_context(nc.sbuf_tensor("biasT_sb", [128, 4, 2, 128], F32))
        pv = [es.enter_context(nc.sbuf_tensor(f"pv{l}", [128, NPV], F32)) for l in range(DEPTH)]
        pv2 = [es.enter_context(nc.sbuf_tensor(f"pvb{l}", [128, 8], F32)) for l in range(DEPTH)]
        gw17 = [es.enter_context(nc.sbuf_tensor(f"gw{l}", [17, 128], BF16)) for l in range(DEPTH)]
        pwbd = [es.enter_context(nc.sbuf_tensor(f"pw{l}", [128, 2, 128], BF16)) for l in range(DEPTH)]
        cvec = es.enter_context(nc.sbuf_tensor("cvec", [128, 4], F32))
        ps = es.enter_context(nc.psum_tensor("ps", [128, 8, 512], F32))

        def V(off, shape, dt=BF16, p0=0):
            n = 1
            for s in shape[1:]:
                n *= s
            esz = _esz(dt)
            a = arena[p0:p0 + shape[0], off // 2: off // 2 + (n * esz) // 2]
            if dt != BF16:
                a = a.bitcast(dt)
            if len(shape) == 3:
                a = a.rearrange("p (a b) -> p a b", a=shape[1])
            elif len(shape) == 4:
                a = a.rearrange("p (a b c) -> p a b c", a=shape[1], b=shape[2])
            return a

        hT = V(0, [128, 8, S], F32)
        hn = V(64 * KB, [128, 8, S])
        mixed = V(96 * KB, [128, 8, S])
        wslot = [V(128 * KB + 8 * KB * i, [128, 8, 512]) for i in range(4)]
        T0 = 160 * KB
        ident = cst[:, F_IDENT:F_IDENT + 128]
        onesb = cstb[:, B_ONES:B_ONES + 128]
        blk64 = cstb[:, B_BLK64:B_BLK64 + 128]
        glamask = cstb[:, B_GLAMASK:B_GLAMASK + 512]
        tricat = cst[:, F_TRICAT:F_TRICAT + 130]
        umat = cst[:, F_U:F_U + 128]
        bdmask = cstb[:, B_BDMASK:B_BDMASK + 256]
        eps_t = cvec[:, 0:1]
        one_t = cvec[:, 1:2]

        bank_ctr = [0]

        def nextbank(lo=0, n=8):
            b = lo + bank_ctr[0] % n
            bank_ctr[0] += 1
            return b

        gsetup = P.group("setup")
        gx = [P.group("x0"), P.group("x1")]
        go = [P.group("o0", final=True), P.group("o1", final=True)]
        gW = {}

        def wgrp(name):
            if name not in gW:
                gW[name] = P.group(name)
            return gW[name]

        def setup():
            P.dma("sp", cst[:], cstf_d, P.group("c_cstf"))
            P.dma("pool", cstb[:], cstb_d, P.group("c_cstb"))
            P.dma("sp", biasT[:].rearrange("p a b c -> p (a b c)"), biasT_d, P.group("c_bias"))
            for l in layers:
                P.dma("sp", pv[l][:], pvec_d[l], P.group(f"c_pv{l}"))
                P.dma("pool", gw17[l][:], gw17_d[l], P.group(f"c_gw{l}"))
                P.dma("pool", pwbd[l][:].rearrange("p a b -> p (a b)"), pwbd_d[l], P.group(f"c_pw{l}"))
            P.memset("dve", cvec[:, 0:1], EPS)
            P.memset("dve", cvec[:, 1:2], 1.0)
            l0 = layers[0]
            for h in range(4):
                P.ts("dve", biasT[:, h, :, :], biasT[:, h, :, :], pv[l0][:, 22 + h:23 + h], ALU.subtract)
                P.tt("dve", biasT[:, h, 0, :], biasT[:, h, 0, :], cst[:, F_MASKNEG:F_MASKNEG + 128], ALU.add)
            for l in layers:
                lam_init = 0.8 - 0.6 * math.exp(-0.3 * l)
                lvt = V(T0 + l * 2 * KB, [128, 256], F32)
                lamt = V(T0 + l * 2 * KB + KB, [128, 2, 64], F32)
                P.dma("sp", lvt, lvb_d[l], P.group(f"c_lv{l}"))
                lvb = lvt.rearrange("p (a b) -> p a b", a=4)
                P.tt("dve", lamt, lvb[:, 0::2, :], lvb[:, 1::2, :], ALU.mult)
                P.op("dve", lambda e, l=l, lamt=lamt: e.tensor_reduce(out=pv2[l][:, 4:6], in_=lamt, axis=AX.X, op=ALU.add),
                     [lamt], [pv2[l][:, 4:6]])
                P.act(pv2[l][:, 4:6], pv2[l][:, 4:6], AF.Exp)
                P.tt("dve", pv2[l][:, 6:7], pv2[l][:, 4:5], pv2[l][:, 5:6], ALU.subtract)
                P.ts("dve", pv2[l][:, 1:2], pv2[l][:, 6:7], lam_init, ALU.add, -1.0, ALU.mult)
                P.ts("dve", pv2[l][:, 0:1], pv[l][:, 16:17], 0.125, ALU.mult)
                P.ts("dve", pv2[l][:, 2:3], pv[l][:, 18:19], 1.0 - lam_init, ALU.mult)

        def load_x(s):
            for t in range(NT):
                xs = V(64 * KB + (t % 2) * 4 * KB, [128, D], F32)
                P.dma("sp", xs, x[s, t * 128:(t + 1) * 128, :], gx[t % 2])
                for half in range(2):
                    b = nextbank()
                    for kk in range(4):
                        k = half * 4 + kk
                        P.transpose(ps[:, b, kk * 128:(kk + 1) * 128], xs[:, k * 128:(k + 1) * 128], ident)
                    P.copy("dve" if half == 0 else "act", hT[:, half * 4:(half + 1) * 4, t * 128:(t + 1) * 128],
                           ps[:, b, :].rearrange("p (a b) -> p a b", a=4))

        def store_out(s):
            for t in range(NT):
                ys = V(64 * KB + (t % 2) * 4 * KB, [128, D], F32)
                for half in range(2):
                    b = nextbank()
                    for kk in range(4):
                        k = half * 4 + kk
                        P.transpose(ps[:, b, kk * 128:(kk + 1) * 128], hT[:, k, t * 128:(t + 1) * 128], ident)
                    P.copy("dve" if half == 0 else "act", ys[:, half * 512:(half + 1) * 512], ps[:, b, :])
                P.dma("sp", out[s, t * 128:(t + 1) * 128, :], ys, go[t % 2])

        def rmsnorm(l, gbase, toff):
            sqv = V(toff, [128, 8, 512])
            lnv2 = [V(toff + 8 * KB + i * 2 * KB, [128, 512], F32) for i in range(2)]
            banks = {}

            def stage_a(tb):
                sl = slice(tb * 512, (tb + 1) * 512)
                P.act(sqv, hT[:, :, sl], AF.Square)
                b = nextbank()
                banks[tb] = b
                for k in range(8):
                    P.mm(ps[:, b, :], lhsT=onesb, rhs=sqv[:, k, :], start=(k == 0), stop=(k == 7))

            def stage_b(tb):
                sl = slice(tb * 512, (tb + 1) * 512)
                lnv = lnv2[tb % 2]
                P.act(lnv, ps[:, banks[tb], :], AF.Ln, scale=1.0 / D, bias=eps_t)
                P.act(lnv, lnv, AF.Exp, scale=-0.5)
                for k in range(8):
                    P.stt(hn[:, k, sl], hT[:, k, sl], pv[l][:, gbase + k:gbase + k + 1], lnv, ALU.mult, ALU.mult)

            stage_a(0)
            for tb in range(NB):
                if tb + 1 < NB:
                    stage_a(tb + 1)
                stage_b(tb)

        def win_view(l):
            return w_in[l].rearrange("(k p) e -> p k e", p=128)

        def da_phase(l):
            wv = win_view(l)
            P.dma("pool", wslot[0], wv[:, :, 0:512], wgrp("ws0"))
            P.dma("pool", wslot[1], wv[:, :, 512:1024], wgrp("ws1"))
            P.dma("pool", wslot[2], wv[:, :, 1024:1536], wgrp("ws2"))
            P.dma("pool", wslot[3], wv[:, :, 1536:2048], wgrp("ws3"))
            qn = V(T0, [128, S])
            kn = V(T0 + 4 * KB, [128, S])
            vst = V(T0 + 8 * KB, [128, NT, 128])
            pt4 = [V(T0 + 12 * KB + i * KB, [128, 2, 256]) for i in range(4)]
            TT = T0 + 16 * KB
            raw = [V(TT + i * 2 * KB, [128, 512], F32) for i in range(2)]
            sqb = [V(TT + 4 * KB + i * KB, [128, 512]) for i in range(2)]
            lnv = [V(TT + 6 * KB + i * 2 * KB, [128, 512], F32) for i in range(2)]
            FT = TT + 10 * KB
            lc = V(FT, [128, 512], F32)
            fin = []
            for base in (FT + 2 * KB, TT):
                fin.append(dict(t0=V(base, [128, 256], F32), t1=V(base + KB, [128, 256], F32),
                                cc=V(base + 2 * KB, [128, 256], F32), sq2=V(base + 3 * KB, [128, 256]),
                                lnf=V(base + 3 * KB + 512, [128, 256], F32)))
            qpad = [V(FT + 7 * KB + i * KB, [128, 2, 256]) for i in range(2)]
            P.memset("pool", qpad[0], 0.0)
            P.memset("pool", qpad[1], 0.0)
            pending = []
            for h in range(4):
                hc = slice(h * 128, (h + 1) * 128)
                while pending:
                    pending.pop(0)[1]()
                jobs = []
                for (wi, dst, gcol) in ((0, qn, pv2[l][:, 0:1]), (1, kn, pv[l][:, 17:18])):
                    for tb in range(NB):
                        sl = slice(tb * 512, (tb + 1) * 512)
                        b = nextbank()
                        for k in range(8):
                            P.mm(ps[:, b, :], lhsT=wslot[wi][:, k, hc], rhs=hn[:, k, sl], start=(k == 0), stop=(k == 7))
                        jobs.append((b, dst, gcol, sl))
                cb = {}

                def ch_a(ji):
                    b, dst, gcol, sl = jobs[ji]
                    P.copy("dve", raw[ji % 2], ps[:, b, :])
                    P.tt("pool", sqb[ji % 2], raw[ji % 2], raw[ji % 2], ALU.mult)

                def ch_b(ji):
                    b2 = nextbank()
                    cb[ji] = b2
                    P.mm(ps[:, b2, :], lhsT=blk64, rhs=sqb[ji % 2])
                    P.act(lnv[ji % 2], ps[:, b2, :], AF.Ln, scale=1.0 / 64, bias=eps_t)
                    P.act(lnv[ji % 2], lnv[ji % 2], AF.Exp, scale=-0.5)

                def ch_c(ji):
                    b, dst, gcol, sl = jobs[ji]
                    P.stt(dst[:, sl], raw[ji % 2], gcol, lnv[ji % 2], ALU.mult, ALU.mult)

                nj = len(jobs)
                ch_a(0)
                if nj > 1:
                    ch_a(1)
                ch_b(0)
                for ji in range(nj):
                    if ji + 1 < nj:
                        ch_b(ji + 1)
                    ch_c(ji)
                    if ji + 2 < nj:
                        ch_a(ji + 2)
                for tg in range(NT // 4):
                    b = nextbank()
                    for t4 in range(4):
                        t = tg * 4 + t4
                        for k in range(8):
                            P.mm(ps[:, b, t4 * 128:(t4 + 1) * 128], lhsT=hn[:, k, t * 128:(t + 1) * 128],
                                 rhs=wslot[2][:, k, hc], start=(k == 0), stop=(k == 7))
                    P.copy("dve", vst[:, tg * 4:(tg + 1) * 4, :], ps[:, b, :].rearrange("p (a b) -> p a b", a=4))
                if h == 3:
                    P.dma("pool", wslot[0][:, :, 0:272], wv[:, :, 2048:2320], wgrp("ws0"))
                    P.dma("pool", wslot[1][:, :, 0:256], wv[:, :, 2320:2576], wgrp("ws1"))
                NC2 = S // 256
                steps = [(c, j) for c in range(NC2) for j in range(2 * c + 2)]
                LA = 3
                cf = pv[l][:, 22 + h:23 + h]
                qp_done = set()

                def geom(c, j):
                    q0 = max(j, 2 * c) * 128
                    q1 = (2 * c + 2) * 128
                    return q0, q1, q1 - q0, q0 - 2 * c * 128

                def scores(i):
                    c, j = steps[i]
                    q0, q1, n, off = geom(c, j)
                    sb = 4 + (i % 4)
                    if c not in qp_done:
                        qp_done.add(c)
                        P.copy("pool", qpad[c % 2][0:64, 0, :], qn[0:64, c * 256:(c + 1) * 256])
                        P.copy("pool", qpad[c % 2][64:128, 1, :], qn[64:128, c * 256:(c + 1) * 256])
                    for m in range(2):
                        P.mm(ps[:, sb, m * 256:m * 256 + n], lhsT=kn[:, j * 128:(j + 1) * 128],
                             rhs=qpad[c % 2][:, m, off:256])
                    sc3 = ps[:, sb, :].rearrange("p (m q) -> p m q", m=2)
                    if j >= 2 * c:
                        bt = biasT[:, h, 0, :]
                        btb = bass.AP(bt.tensor, bt.offset, [list(bt.ap[0]), [0, 2], [1, 128]])
                        P.tt("dve", sc3[:, :, 0:128], sc3[:, :, 0:128], btb, ALU.add)
                    if 2 * c <= j + 1 <= 2 * c + 1:
                        o = (j + 1) * 128 - q0
                        bt = biasT[:, h, 1, :]
                        btb = bass.AP(bt.tensor, bt.offset, [list(bt.ap[0]), [0, 2], [1, 128]])
                        P.tt("dve", sc3[:, :, o:o + 128], sc3[:, :, o:o + 128], btb, ALU.add)
                    P.act(pt4[i % 4][:, :, 0:n], sc3[:, :, 0:n], AF.Exp, bias=cf)

                def pvs(i):
                    c, j = steps[i]
                    nk = 2 * c + 2
                    q0, q1, n, off = geom(c, j)
                    bO = 2 * (c % 2)
                    bL = bO + 1
                    for m in range(2):
                        pt = pt4[i % 4][:, m, 0:n]
                        st = (j == 0 and m == 0)
                        P.op("pe", lambda e, o_=ps[:, bO, m * 256 + off:(m + 1) * 256], pt=pt, st=st, j=j:
                             e.matmul(o_, lhsT=vst[:, j, :], rhs=pt, start=st, stop=(j == nk - 1), skip_group_check=True),
                             [vst[:, j, :], pt], [ps[:, bO, m * 256 + off:(m + 1) * 256]])
                        P.op("pe", lambda e, o_=ps[:, bL, m * 256 + off:(m + 1) * 256], pt=pt, st=st:
                             e.matmul(o_, lhsT=onesb, rhs=pt, start=st, stop=(j == nk - 1), skip_group_check=True),
                             [onesb, pt], [ps[:, bL, m * 256 + off:(m + 1) * 256]])
                    if j == nk - 1:
                        finalize(c)

                def finalize(c):
                    sl = slice(c * 256, (c + 1) * 256)
                    bO = 2 * (c % 2)
                    bL = bO + 1
                    f = fin[c % 2]
                    t0, t1, cc, sq2, lnf = f["t0"], f["t1"], f["cc"], f["sq2"], f["lnf"]
                    P.copy("dve", lc, ps[:, bL, :])
                    P.tt("dve", t0, ps[:, bO, 0:256], lc[:, 256:512], ALU.mult)
                    P.tt("dve", t1, ps[:, bO, 256:512], lc[:, 0:256], ALU.mult)
                    P.tt("pool", cc, lc[:, 0:256], lc[:, 256:512], ALU.mult)
                    P.stt(t0, t1, pv2[l][:, 1:2], t0, ALU.mult, ALU.add)
                    P.tt("pool", sq2, t0, t0, ALU.mult)
                    P.tt("pool", cc, cc, cc, ALU.mult)

                    def tail(h=h, sl=sl, bL=bL, t0=t0, cc=cc, sq2=sq2, lnf=lnf):
                        P.mm(ps[:, bL, 0:256], lhsT=onesb, rhs=sq2)
                        P.stt(lnf, cc, EPS * 128.0, ps[:, bL, 0:256], ALU.mult, ALU.add)
                        P.act(lnf, lnf, AF.Ln, scale=1.0 / 128)
                        P.act(lnf, lnf, AF.Exp, scale=-0.5)
                        P.stt(mixed[:, h, sl], t0, pv2[l][:, 2:3], lnf, ALU.mult, ALU.mult)
                    pending.append([min(6, 2 * c + 3), tail])

                for i in range(min(LA, len(steps))):
                    scores(i)
                for i in range(len(steps)):
                    if i + LA < len(steps):
                        scores(i + LA)
                    pvs(i)
                    for pnd in list(pending):
                        pnd[0] -= 1
                        if pnd[0] <= 0:
                            pending.remove(pnd)
                            pnd[1]()
            while pending:
                pending.pop(0)[1]()

        def gla_pool_phase(l):
            wv = win_view(l)
            wA, wB, wC = wslot[3], wslot[0], wslot[1]
            BW = 256
            NBG = S // BW
            TPB = BW // 128
            NTL = NBG * TPB
            UW = BW + 16
            G0 = T0

            def blkset(bs):
                o = G0 + bs * 6 * KB
                return dict(gqT=V(o, [128, BW]), gkT=V(o + 512, [128, BW]), gktok=V(o + 1024, [128, TPB, 128]),
                            gvp=V(o + 1536, [128, TPB, 256]), gvpad=V(o + 2560, [128, TPB, 4, 128]),
                            lr17=V(o + 4608, [17, BW]), srT=V(o + 5120, [128, 2, BW]))
            BS = [blkset(0), blkset(1)]
            F0 = G0 + 12 * KB

            def feset(ts):
                o = F0 + ts * 3 * KB
                return dict(qdec=V(o, [128, 128]), qdec32=V(o + 256, [128, 128], F32), kbd=V(o + 768, [128, 4, 128]),
                            dec=V(o + 1792, [128, 2], F32), AT=V(o + 1824, [128, 4, 128]))
            FS = [feset(0), feset(1)]
            S0 = F0 + 6 * KB
            e1 = V(S0, [128, 128], F32)
            spl = V(S0 + 512, [128, 128], F32)
            Eq = V(S0 + 1024, [128, 128], F32)
            Ek = V(S0 + 1536, [128, 128], F32)
            Ee = V(S0 + 2048, [128, 128], F32)
            kinv = V(S0 + 2560, [128, 128])
            kend = V(S0 + 2816, [128, 128])
            qbd = V(S0 + 3072, [128, 4, 128])
            Sfp = V(S0 + 4096, [128, 256], F32)
            U0 = S0 + 5 * KB
            ubuf = V(U0, [128, 2, UW], F32)
            Y1 = U0 + 2304
            Abuf = V(Y1, [128, 2, UW], F32)
            Bbuf = V(Y1 + 2176, [128, 2, UW], F32)
            pooled = V(Y1 + 4352, [128, 2, BW])
            tmp16 = V(Y1 + 5376, [128, 16], F32)
            Y2 = Y1 + 5632
            oT = V(Y2, [128, 2, BW], F32)
            sqg = V(Y2 + 2048, [128, 2, BW])
            lng = V(Y2 + 3072, [128, 2, BW], F32)
            assert Y2 + 5120 <= ARENA, (Y2 + 5120, ARENA)

            wo = w_out[l].rearrange("(k p) d -> p k d", p=128)
            P.dma("pool", wslot[2], wo[:, :, 0:512], wgrp("ws2"))

            P.memset("pool", BS[0]["gvpad"], 0.0)
            P.memset("pool", BS[1]["gvpad"], 0.0)
            P.memset("dve", Sfp, 0.0)
            P.memset("pool", ubuf[:, :, 0:16], 0.0)
            hm2 = cstb[:, B_HM:B_HM + 4]
            hm_b = bass.AP(hm2.tensor, hm2.offset, [list(hm2.ap[0]), [1, 4], [0, 128]])
            hmf = cstb[:, B_HMF:B_HMF + 512].rearrange("p (a b) -> p a b", a=4)

            def proj(tb):
                B_ = BS[tb % 2]
                sl = slice(tb * BW, (tb + 1) * BW)
                for (dst, c0) in ((B_["gqT"], 0), (B_["gkT"], 128)):
                    b = nextbank()
                    for k in range(8):
                        P.mm(ps[:, b, 0:BW], lhsT=wA[:, k, c0:c0 + 128], rhs=hn[:, k, sl], start=(k == 0), stop=(k == 7))
                    P.copy("act", dst, ps[:, b, 0:BW])
                b = nextbank()
                for k in range(8):
                    P.mm(ps[0:16, b, 0:BW], lhsT=wB[:, k, 0:16], rhs=hn[:, k, sl], start=(k == 0), stop=(k == 7))
                P.memset("pool", B_["lr17"], 1.0)
                P.copy("dve", B_["lr17"][0:16, :], ps[0:16, b, 0:BW])
                for pt in range(2):
                    b = nextbank()
                    for k in range(8):
                        P.mm(ps[:, b, 0:BW], lhsT=wB[:, k, 16 + pt * 128:16 + (pt + 1) * 128], rhs=hn[:, k, sl],
                             start=(k == 0), stop=(k == 7))
                    P.act(B_["srT"][:, pt, :], ps[:, b, 0:BW], AF.Silu)
                for t4 in range(TPB):
                    t = tb * TPB + t4
                    b = nextbank()
                    for k in range(8):
                        P.mm(ps[:, b, 0:384], lhsT=hn[:, k, t * 128:(t + 1) * 128], rhs=wA[:, k, 128:512],
                             start=(k == 0), stop=(k == 7))
                    P.copy("act", B_["gktok"][:, t4, :], ps[:, b, 0:128])
                    P.copy("dve", B_["gvp"][:, t4, :], ps[:, b, 128:384])
                    src = ps[:, b, 128:384].rearrange("p (h v) -> p h v", h=4)
                    P.copy("act", B_["gvpad"][:, t4, 0::2, 0:64], src[:, 0::2, :])
                    P.copy("dve", B_["gvpad"][:, t4, 1::2, 64:128], src[:, 1::2, :])
                for pt in range(2):
                    b = nextbank()
                    for k in range(8):
                        P.mm(ps[:, b, 0:BW], lhsT=wC[:, k, pt * 128:(pt + 1) * 128], rhs=hn[:, k, sl],
                             start=(k == 0), stop=(k == 7))
                    P.copy("act", ubuf[:, pt, 16:UW], ps[:, b, 0:BW])
                P.tt("pool", Abuf[:, :, 1:UW], ubuf[:, :, 1:UW], ubuf[:, :, 0:UW - 1], ALU.add)
                P.tt("pool", Bbuf[:, :, 3:UW], Abuf[:, :, 3:UW], Abuf[:, :, 1:UW - 2], ALU.add)
                P.tt("pool", Abuf[:, 1, 7:UW], Bbuf[:, 1, 7:UW], Bbuf[:, 1, 3:UW - 4], ALU.add)
                P.tt("pool", Bbuf[:, 1, 15:UW], Abuf[:, 1, 15:UW], Abuf[:, 1, 7:UW - 8], ALU.add)
                for pt in range(2):
                    for half in range(2):
                        rows = slice(half * 64, half * 64 + 64)
                        src = (Abuf if half == 0 else Bbuf)
                        P.stt(pooled[rows, pt, :], src[rows, pt, 16:UW], cst[rows, F_INVW + pt:F_INVW + pt + 1],
                              ubuf[rows, pt, 16:UW], ALU.mult, ALU.subtract)
                        if tb == 0:
                            P.tt("dve", tmp16[rows, :], src[rows, pt, 16:32],
                                 cst[rows, F_INVC + pt * 16:F_INVC + pt * 16 + 16], ALU.mult)
                            P.tt("dve", pooled[rows, pt, 0:16], tmp16[rows, :], ubuf[rows, pt, 16:32], ALU.subtract)
                for pt in range(2):
                    b = nextbank()
                    P.mm(ps[:, b, 0:BW], lhsT=pwbd[l][:, pt, :], rhs=pooled[:, pt, :])
                    P.ts("dve", mixed[:, 6 + pt, sl], ps[:, b, 0:BW], pv[l][:, 20 + pt:21 + pt], ALU.mult)
                P.copy("pool", ubuf[:, :, 0:16], ubuf[:, :, BW:UW])

            def front(t):
                B_ = BS[(t // TPB) % 2]
                F_ = FS[t % 2]
                t4 = t % TPB
                cols = slice(t4 * 128, (t4 + 1) * 128)
                b = nextbank()
                P.mm(ps[:, b, 0:128], lhsT=B_["lr17"][0:17, cols], rhs=gw17[l][:])
                P.act(e1, ps[:, b, 0:128], AF.Exp, scale=-1.0)
                P.act(spl, e1, AF.Ln, bias=one_t)
                bA = nextbank()
                P.mm(ps[:, bA, 0:130], lhsT=spl, rhs=tricat)
                bB = nextbank()
                P.mm(ps[:, bB, 0:128], lhsT=umat, rhs=spl)
                P.act(Eq, ps[:, bA, 0:128], AF.Exp)
                P.act(Ek, ps[:, bA, 0:128], AF.Exp, scale=-1.0)
                P.act(F_["dec"], ps[:, bA, 128:130], AF.Exp)
                P.act(Ee, ps[:, bB, 0:128], AF.Exp)
                P.stt(F_["qdec32"], B_["gqT"][:, cols], 32.0 ** -0.5, Eq, ALU.mult, ALU.mult)
                P.copy("dve", F_["qdec"], F_["qdec32"])
                P.tt("dve", kinv, B_["gkT"][:, cols], Ek, ALU.mult)
                P.tt("dve", kend, B_["gktok"][:, t4, :], Ee, ALU.mult)
                kd = kend
                kd_b = bass.AP(kd.tensor, kd.offset, [list(kd.ap[0]), [0, 4], [1, 128]])
                P.tt("dve", F_["kbd"], kd_b, hmf, ALU.mult)
                qd = F_["qdec"]
                qd_b = bass.AP(qd.tensor, qd.offset, [list(qd.ap[0]), [0, 4], [1, 128]])
                P.tt("dve", qbd, qd_b, hm_b, ALU.mult)
                b3 = nextbank()
                P.mm(ps[:, b3, :], lhsT=kinv, rhs=qbd.rearrange("p a b -> p (a b)"))
                P.tt("dve", F_["AT"].rearrange("p a b -> p (a b)"), ps[:, b3, :], glamask, ALU.mult)

            def back(t):
                B_ = BS[(t // TPB) % 2]
                F_ = FS[t % 2]
                t4 = t % TPB
                cols = slice(t4 * 128, (t4 + 1) * 128)
                AT, kbd, dec = F_["AT"], F_["kbd"], F_["dec"]
                b4 = [nextbank(), nextbank()]
                for hp in range(2):
                    o_ap = ps[:, b4[hp], 0:128]
                    P.mm(o_ap, lhsT=B_["gvpad"][:, t4, 2 * hp, :], rhs=AT[:, 2 * hp, :], start=True, stop=False)
                    P.mm(o_ap, lhsT=B_["gvpad"][:, t4, 2 * hp + 1, :], rhs=AT[:, 2 * hp + 1, :], start=False, stop=False)
                    P.mm(ps[:, b4[hp], 0:64], lhsT=Sfp[:, hp * 128:(hp + 1) * 128], rhs=F_["qdec32"][:, 0:64],
                         start=False, stop=False)
                for ch in range(2):
                    rows = slice(ch * 64, ch * 64 + 64)
                    b5 = nextbank()
                    for hh in range(4):
                        P.mm(ps[:, b5, hh * 64:(hh + 1) * 64], lhsT=kbd[rows, hh, :],
                             rhs=B_["gvp"][rows, t4, hh * 64:(hh + 1) * 64], start=(hh == 0), stop=(hh == 3))
                    P.stt(Sfp, Sfp, dec[:, ch:ch + 1], ps[:, b5, 0:256], ALU.mult, ALU.add)
                    if ch == 0:
                        for hp in range(2):
                            P.mm(ps[:, b4[hp], 64:128], lhsT=Sfp[:, hp * 128:(hp + 1) * 128],
                                 rhs=F_["qdec32"][:, 64:128], start=False, stop=True)
                            P.copy("act", oT[:, hp, cols], ps[:, b4[hp], 0:128])

            def norm_gate(tb):
                B_ = BS[tb % 2]
                sl = slice(tb * BW, (tb + 1) * BW)
                P.tt("pool", sqg, oT, oT, ALU.mult)
                for pt in range(2):
                    b = nextbank()
                    P.mm(ps[:, b, 0:BW], lhsT=blk64, rhs=sqg[:, pt, :])
                    P.act(lng[:, pt, :], ps[:, b, 0:BW], AF.Ln, scale=1.0 / 64, bias=eps_t)
                P.act(lng, lng, AF.Exp, scale=-0.5)
                for pt in range(2):
                    P.stt(lng[:, pt, :], oT[:, pt, :], pv[l][:, 19:20], lng[:, pt, :], ALU.mult, ALU.mult)
                    P.tt("dve", mixed[:, 4 + pt, sl], lng[:, pt, :], B_["srT"][:, pt, :], ALU.mult)

            proj(0)
            front(0)
            for t in range(NTL):
                tb = t // TPB
                if t + 1 < NTL:
                    if (t + 1) % TPB == 0:
                        proj(tb + 1)
                    front(t + 1)
                back(t)
                if t % TPB == TPB - 1:
                    norm_gate(tb)

        def wout_phase(l):
            wo = w_out[l].rearrange("(k p) d -> p k d", p=128)
            P.dma("pool", wslot[3], wo[:, :, 512:1024], wgrp("ws3"))
            for dh in range(2):
                slot = wslot[2 + dh]
                for dc in range(4):
                    d = dh * 4 + dc
                    for tb in range(NB):
                        sl = slice(tb * 512, (tb + 1) * 512)
                        b = nextbank()
                        for k in range(8):
                            P.mm(ps[:, b, :], lhsT=slot[:, k, dc * 128:(dc + 1) * 128], rhs=mixed[:, k, sl],
                                 start=(k == 0), stop=(k == 7))
                        P.tt("dve", hT[:, d, sl], hT[:, d, sl], ps[:, b, :], ALU.add)

        def ffn_phase(l):
            wg = w_gate[l].rearrange("(k p) f -> p k f", p=128)
            wu = w_up[l].rearrange("(k p) f -> p k f", p=128)
            wd = w_down[l].rearrange("(c p) d -> p c d", p=128)
            ffT = V(96 * KB, [128, 11, S])
            gsl = [V(172 * KB, [128, 8, 512]), V(156 * KB, [128, 8, 512])]
            usl = [V(180 * KB, [128, 8, 512]), V(164 * KB, [128, 8, 512])]
            dsl = [V(140 * KB, [128, 11, 256]), V(140 * KB + 5632, [128, 11, 256])]
            sg = [V(152 * KB + i * 2 * KB, [128, 512], F32) for i in range(2)]
            groups = []
            for fh in range(2):
                f0 = fh * 1408
                for (o, w) in ((0, 512), (512, 512), (1024, 384)):
                    groups.append((fh, f0 + o, w))

            def load_group(gi):
                fh, fo, w = groups[gi]
                P.dma("pool", gsl[gi % 2][:, :, 0:w], wg[:, :, fo:fo + w], wgrp(f"fg{gi % 2}"))
                P.dma("pool", usl[gi % 2][:, :, 0:w], wu[:, :, fo:fo + w], wgrp(f"fu{gi % 2}"))

            load_group(0)
            load_group(1)
            rmsnorm(l, 8, 96 * KB)
            dcount = 0
            sgi = 0
            for gi, (fh, fo, w) in enumerate(groups):
                for fc in range(w // 128):
                    fidx = (fo - fh * 1408) // 128 + fc
                    fcs = slice(fc * 128, (fc + 1) * 128)
                    for tb in range(NB):
                        sl = slice(tb * 512, (tb + 1) * 512)
                        bg = nextbank()
                        for k in range(8):
                            P.mm(ps[:, bg, :], lhsT=gsl[gi % 2][:, k, fcs], rhs=hn[:, k, sl], start=(k == 0), stop=(k == 7))
                        bu = nextbank()
                        for k in range(8):
                            P.mm(ps[:, bu, :], lhsT=usl[gi % 2][:, k, fcs], rhs=hn[:, k, sl], start=(k == 0), stop=(k == 7))
                        s_ = sg[sgi % 2]
                        sgi += 1
                        P.act(s_, ps[:, bg, :], AF.Silu)
                        P.tt("dve", ffT[:, fidx, sl], s_, ps[:, bu, :], ALU.mult)
                if gi + 2 < len(groups):
                    load_group(gi + 2)
                if gi % 3 == 2:
                    for dp in range(4):
                        slot = dsl[dcount % 2]
                        P.dma("pool", slot, wd[:, fh * 11:(fh + 1) * 11, dp * 256:(dp + 1) * 256], wgrp(f"fd{dcount % 2}"))
                        dcount += 1
                        for dc in range(2):
                            d = dp * 2 + dc
                            for tb in range(NB):
                                sl = slice(tb * 512, (tb + 1) * 512)
                                b = nextbank()
                                for c in range(11):
                                    P.mm(ps[:, b, :], lhsT=slot[:, c, dc * 128:(dc + 1) * 128], rhs=ffT[:, c, sl],
                                         start=(c == 0), stop=(c == 10))
                                P.tt("dve", hT[:, d, sl], hT[:, d, sl], ps[:, b, :], ALU.add)

        import os as _os
        stages = _os.environ.get("KSTAGES", "setup,n,da,gla,wo,ffn").split(",")
        if "setup" in stages:
            setup()
        for s in range(NSEQ):
            load_x(s)
            for l in layers:
                if "n" in stages:
                    rmsnorm(l, 0, T0)
                if "da" in stages:
                    da_phase(l)
                if "gla" in stages:
                    gla_pool_phase(l)
                if "wo" in stages:
                    wout_phase(l)
                if "ffn" in stages:
                    ffn_phase(l)
            store_out(s)
        P.emit(nc, es)
    return nc


def _host_prep(inp):
    f = lambda k: np.ascontiguousarray(np.asarray(inp[k], dtype=np.float32))
    depth = DEPTH
    p = np.arange(128)
    pvec = np.zeros((depth, 128, NPV), np.float32)
    ang, fng = f("attn_norm_g"), f("ffn_norm_g")
    qg, kg, sg_, gg = f("q_norm_g"), f("k_norm_g"), f("da_subln_g"), f("gla_norm_g")
    psc, lv, rb = f("pool_scale"), f("lambda_vecs"), f("rel_bias")
    for l in range(depth):
        pvec[l, :, 0:8] = ang[l].reshape(8, 128).T
        pvec[l, :, 8:16] = fng[l].reshape(8, 128).T
        pvec[l, :, 16] = qg[l][p % 64]
        pvec[l, :, 17] = kg[l][p % 64]
        pvec[l, :, 18] = sg_[l]
        pvec[l, :, 19] = gg[l][p % 64]
        pvec[l, :, 20:22] = psc[l].reshape(2, 128).T
        pvec[l, :, 22:26] = rb[31][None, :]
    gw17 = np.concatenate([f("gla_gate_w"), f("gla_gate_b")[:, None, :]], axis=1)
    pw = f("pool_w")
    pwbd = np.zeros((depth, 128, 2, 128), np.float32)
    for l in range(depth):
        for g in range(4):
            pt, half = divmod(g, 2)
            pwbd[l, half * 64:(half + 1) * 64, pt, half * 64:(half + 1) * 64] = pw[l, g]
    idx = _bias_index()
    bt = rb[idx]
    biasT = np.ascontiguousarray(np.transpose(bt, (0, 3, 1, 2))).reshape(128, 4 * 2 * 128)
    return {
        "w_in": f("w_in"), "w_out": f("w_out"), "w_gate": f("w_gate"), "w_up": f("w_up"), "w_down": f("w_down"),
        "pvec": pvec, "gw17": np.ascontiguousarray(gw17), "pwbd": pwbd.reshape(depth, 128, 256),
        "biasT": biasT, "cstf": _consts()[0], "cstb": _consts()[1],
        "lvb": np.ascontiguousarray(np.broadcast_to(lv.reshape(depth, 1, 256), (depth, 128, 256))),
    }


_CACHE = {}


def kernel(**inputs):
    x = np.ascontiguousarray(np.asarray(inputs["x"], dtype=np.float32))
    B, S, _ = x.shape
    nseq = B // N_CORES
    shared = _host_prep(inputs)
    key = (S, nseq)
    if key not in _CACHE:
        _CACHE[key] = build_program(S, nseq, list(range(DEPTH)))
    nc = _CACHE[key]
    in_maps = []
    for c in range(N_CORES):
        m = dict(shared)
        m["x"] = np.ascontiguousarray(x[c * nseq:(c + 1) * nseq])
        in_maps.append(m)
    res = run_bass_kernel_spmd(nc, in_maps, core_ids=list(range(N_CORES)))
    return np.concatenate([np.asarray(r["out"]) for r in res.results], axis=0).astype(np.float32)
```

```python
import math
from contextlib import ExitStack

import numpy as np
import concourse.bass as bass
import concourse.mybir as mybir
from concourse.bass_utils import run_bass_kernel_spmd

F32 = mybir.dt.float32
BF16 = mybir.dt.bfloat16
AF = mybir.ActivationFunctionType
ALU = mybir.AluOpType
AX = mybir.AxisListType

D = 1024
DEPTH = 2
FFN = 2816
IN_TOTAL = 2576
N_CORES = 8
EPS = 1e-6
NEG = -30000.0
NPV = 26
ENGINES = ("pe", "act", "dve", "pool", "sp")
SAME_ENGINE_SYNC = True


def _esz(dt):
    s = str(dt)
    if "64" in s:
        return 8
    if "32" in s:
        return 4
    if "16" in s:
        return 2
    return 1


def region(ap):
    pat = ap.ap
    off = int(ap.offset)
    esz = _esz(ap.dtype)
    name = ap.tensor.name
    sp = str(ap.space).upper()
    if "DRAM" in sp or "HBM" in sp:
        lo = hi = off
        for s, c in pat:
            if s >= 0:
                hi += s * (c - 1)
            else:
                lo += s * (c - 1)
        return (name, 0, 1, lo * esz, (hi + 1) * esz)
    pstep, pcnt = pat[0]
    p0 = off // pstep
    f0 = off % pstep
    lo = hi = f0
    for s, c in pat[1:]:
        if s >= 0:
            hi += s * (c - 1)
        else:
            lo += s * (c - 1)
    if "PSUM" in sp:
        b0 = (lo * esz) // 2048 * 2048
        b1 = ((hi + 1) * esz + 2047) // 2048 * 2048
        return ("@" + name, 0, 128, b0, b1)
    return (name, p0, p0 + pcnt, lo * esz, (hi + 1) * esz)


class Op:
    __slots__ = ("eng", "fn", "deps", "needs_inc", "tick", "is_dma", "grp", "grp_val", "waits")

    def __init__(self, eng, fn):
        self.eng = eng
        self.fn = fn
        self.deps = set()
        self.needs_inc = False
        self.tick = None
        self.is_dma = False
        self.grp = None
        self.grp_val = None
        self.waits = None


class DmaGroup:
    def __init__(self, name):
        self.name = name
        self.sem = None
        self.count = 0
        self.final = False


class Prog:
    def __init__(self):
        self.ops = {e: [] for e in ENGINES}
        self.res = {}
        self.groups = []

    def group(self, name, final=False):
        g = DmaGroup(name)
        g.final = final
        self.groups.append(g)
        return g

    def _track(self, op, reads, writes):
        rregs = [region(a) for a in reads]
        wregs = [region(a) for a in writes]
        for (name, p0, p1, b0, b1) in rregs:
            psum = name[0] == "@"
            for e in self.res.setdefault(name, []):
                if (e[4] == "w" or (psum and e[5].eng != op.eng)) and e[0] < p1 and p0 < e[1] and e[2] < b1 and b0 < e[3]:
                    op.deps.add(e[5])
        for (name, p0, p1, b0, b1) in wregs:
            for e in self.res.setdefault(name, []):
                if e[0] < p1 and p0 < e[1] and e[2] < b1 and b0 < e[3]:
                    op.deps.add(e[5])
        op.deps.discard(op)
        for (name, p0, p1, b0, b1) in rregs:
            lst = self.res[name]
            if not op.is_dma:
                lst[:] = [e for e in lst if not (e[4] == "r" and e[5].eng == op.eng and not e[5].is_dma
                                                 and p0 <= e[0] and e[1] <= p1 and b0 <= e[2] and e[3] <= b1)]
            lst.append([p0, p1, b0, b1, "r", op])
        for (name, p0, p1, b0, b1) in wregs:
            lst = self.res[name]
            lst[:] = [e for e in lst if not (p0 <= e[0] and e[1] <= p1 and b0 <= e[2] and e[3] <= b1)]
            lst.append([p0, p1, b0, b1, "w", op])

    def op(self, eng, fn, reads=(), writes=()):
        o = Op(eng, fn)
        self._track(o, reads, writes)
        self.ops[eng].append(o)
        return o

    def dma(self, queue, out, in_, grp, **kw):
        o = Op(queue, lambda e: e.dma_start(out=out, in_=in_, **kw))
        o.is_dma = True
        o.grp = grp
        grp.count += 16
        o.grp_val = grp.count
        self._track(o, [in_], [out])
        self.ops[queue].append(o)
        return o

    def mm(self, out, lhsT, rhs, start=True, stop=True):
        return self.op("pe", lambda e: e.matmul(out, lhsT=lhsT, rhs=rhs, start=start, stop=stop),
                       [lhsT, rhs], [out])

    def transpose(self, out, in_, ident):
        return self.op("pe", lambda e: e.transpose(out, in_, ident), [in_, ident], [out])

    def act(self, out, in_, func, bias=None, scale=None):
        kw = {}
        reads = [in_]
        if bias is not None:
            kw["bias"] = bias
            if not isinstance(bias, (int, float)):
                reads.append(bias)
        if scale is not None:
            kw["scale"] = scale
            if not isinstance(scale, (int, float)):
                reads.append(scale)
        return self.op("act", lambda e: e.activation(out=out, in_=in_, func=func, **kw), reads, [out])

    def tt(self, eng, out, in0, in1, op):
        return self.op(eng, lambda e: e.tensor_tensor(out=out, in0=in0, in1=in1, op=op), [in0, in1], [out])

    def ts(self, eng, out, in0, s1, op0, s2=None, op1=None):
        reads = [in0]
        if not isinstance(s1, (int, float)):
            reads.append(s1)
        if s2 is not None and not isinstance(s2, (int, float)):
            reads.append(s2)
        if op1 is None:
            return self.op(eng, lambda e: e.tensor_scalar(out=out, in0=in0, scalar1=s1, scalar2=None, op0=op0),
                           reads, [out])
        return self.op(eng, lambda e: e.tensor_scalar(out=out, in0=in0, scalar1=s1, scalar2=s2, op0=op0, op1=op1),
                       reads, [out])

    def stt(self, out, in0, scalar, in1, op0, op1):
        reads = [in0, in1]
        if not isinstance(scalar, (int, float)):
            reads.append(scalar)
        return self.op("dve", lambda e: e.scalar_tensor_tensor(out=out, in0=in0, scalar=scalar, in1=in1,
                                                                 op0=op0, op1=op1), reads, [out])

    def copy(self, eng, out, in_):
        if eng == "act":
            return self.op(eng, lambda e: e.copy(out=out, in_=in_), [in_], [out])
        return self.op(eng, lambda e: e.tensor_copy(out=out, in_=in_), [in_], [out])

    def memset(self, eng, ap, val):
        return self.op(eng, lambda e: e.memset(ap, val), [], [ap])

    def recip(self, out, in_):
        return self.op("dve", lambda e: e.reciprocal(out=out, in_=in_), [in_], [out])

    def emit(self, nc, es):
        def skip(d, o):
            return d.eng == o.eng and not o.is_dma and (o.eng == "pe" or not SAME_ENGINE_SYNC)

        for e in ENGINES:
            for o in self.ops[e]:
                for d in o.deps:
                    if d.is_dma or skip(d, o):
                        continue
                    d.needs_inc = True
        esem = {e: es.enter_context(nc.semaphore("sem_" + e)) for e in ENGINES}
        for g in self.groups:
            g.sem = es.enter_context(nc.semaphore("dg_" + g.name))
        for e in ENGINES:
            t = 0
            for o in self.ops[e]:
                if o.needs_inc and not o.is_dma:
                    t += 1
                    o.tick = t
        for e in ENGINES:
            seen = {}
            for o in self.ops[e]:
                w = {}
                for d in o.deps:
                    if d.is_dma:
                        key = ("g", id(d.grp))
                        sem, val = d.grp.sem, d.grp_val
                    else:
                        if skip(d, o):
                            continue
                        key = ("e", d.eng)
                        sem, val = esem[d.eng], d.tick
                    if seen.get(key, 0) >= val:
                        continue
                    if key not in w or w[key][1] < val:
                        w[key] = (sem, val)
                for key, (sem, val) in w.items():
                    seen[key] = val
                o.waits = list(w.values())
        engobj = {"pe": "tensor", "act": "scalar", "dve": "vector", "pool": "gpsimd", "sp": "sync"}
        finals = [(g.sem, g.count) for g in self.groups if g.count > 0 and g.final]
        with nc.Block() as block:
            for e in ENGINES:
                def body(eng, ops=self.ops[e], e=e):
                    for o in ops:
                        for sem, val in o.waits:
                            eng.wait_ge(sem, val)
                        ins = o.fn(eng)
                        if o.is_dma:
                            ins.then_inc(o.grp.sem, 16)
                        elif o.needs_inc:
                            ins.then_inc(esem[e], 1)
                    if e == "sp":
                        for sem, val in finals:
                            eng.wait_ge(sem, val)

                getattr(block, engobj[e])(body)


def _t5_bucket_np(d):
    d = np.maximum(d, 0)
    max_exact = 16
    large = max_exact + (np.log(np.maximum(d, 1).astype(np.float32) / max_exact)
                         / math.log(128 / max_exact) * (32 - max_exact)).astype(np.int32)
    large = np.minimum(large, 31)
    return np.where(d < max_exact, d, large)


F_IDENT = 0
F_MASKNEG = 128
F_TRICAT = 256
F_U = 386
F_INVW = 514
F_INVC = 516
NCF = 548
B_GLAMASK = 0
B_BLK64 = 512
B_ONES = 640
B_BDMASK = 768
B_HM = 1024
B_HMF = 1028
NCB = 1540


def _consts():
    cf = np.zeros((128, NCF), np.float32)
    cb = np.zeros((128, NCB), np.float32)
    i = np.arange(128)
    cf[:, F_IDENT:F_IDENT + 128] = np.eye(128, dtype=np.float32)
    cf[:, F_MASKNEG:F_MASKNEG + 128] = np.where(i[:, None] > i[None, :], NEG, 0.0)
    same = (i[:, None] // 64) == (i[None, :] // 64)
    gm = (same & (i[:, None] <= i[None, :])).astype(np.float32)
    cb[:, B_GLAMASK:B_GLAMASK + 512] = np.tile(gm, (1, 4))
    cf[:, F_TRICAT:F_TRICAT + 128] = gm * (-1.0 / 16.0)
    cf[:, F_TRICAT + 128] = np.where(i < 64, -1.0 / 16.0, 0.0)
    cf[:, F_TRICAT + 129] = np.where(i >= 64, -1.0 / 16.0, 0.0)
    cf[:, F_U:F_U + 128] = (same & (i[:, None] > i[None, :])).astype(np.float32) * (-1.0 / 16.0)
    cb[:, B_BLK64:B_BLK64 + 128] = same.astype(np.float32)
    hrow = i // 32
    hcol = np.arange(256) // 64
    cb[:, B_BDMASK:B_BDMASK + 256] = (hrow[:, None] == hcol[None, :]).astype(np.float32)
    wins = (2, 4, 8, 16)
    for pt in range(2):
        for half in range(2):
            win = wins[pt * 2 + half]
            rows = slice(half * 64, half * 64 + 64)
            cf[rows, F_INVW + pt] = 1.0 / win
            t = np.arange(16)
            cf[rows, F_INVC + pt * 16:F_INVC + pt * 16 + 16] = 1.0 / np.minimum(t + 1, win)
    cb[:, B_ONES:B_ONES + 128] = 1.0
    cb[:, B_HM:B_HM + 4] = (hrow[:, None] == np.arange(4)[None, :]).astype(np.float32)
    hf = (np.arange(4)[:, None] == (np.arange(128) // 32)[None, :]).astype(np.float32).reshape(1, 512)
    cb[:, B_HMF:B_HMF + 512] = hf
    return cf, cb


def _bias_index():
    k = np.arange(128)[:, None]
    q = np.arange(128)[None, :]
    d0 = np.clip(q - k, 0, None)
    d1 = q - k + 128
    idx = np.stack([_t5_bucket_np(d0), _t5_bucket_np(d1)], axis=1)
    return idx


def build_program(S, NSEQ, layers):
    NT = S // 128
    NB = S // 512
    nc = bass.Bass("TRN2", target_bir_lowering=False)
    dt_in = lambda n, shp: nc.dram_tensor(n, shp, F32, kind="ExternalInput").ap()
    x = dt_in("x", [NSEQ, S, D])
    w_in = dt_in("w_in", [DEPTH, D, IN_TOTAL])
    w_out = dt_in("w_out", [DEPTH, D, D])
    w_gate = dt_in("w_gate", [DEPTH, D, FFN])
    w_up = dt_in("w_up", [DEPTH, D, FFN])
    w_down = dt_in("w_down", [DEPTH, FFN, D])
    pvec_d = dt_in("pvec", [DEPTH, 128, NPV])
    gw17_d = dt_in("gw17", [DEPTH, 17, 128])
    pwbd_d = dt_in("pwbd", [DEPTH, 128, 256])
    biasT_d = dt_in("biasT", [128, 4 * 2 * 128])
    cstf_d = dt_in("cstf", [128, NCF])
    cstb_d = dt_in("cstb", [128, NCB])
    lvb_d = dt_in("lvb", [DEPTH, 128, 256])
    out = nc.dram_tensor("out", [NSEQ, S, D], F32, kind="ExternalOutput").ap()

    P = Prog()
    KB = 1024
    ARENA = 196 * KB
    with ExitStack() as es:
        arena = es.enter_context(nc.sbuf_tensor("arena", [128, ARENA // 2], BF16))
        cst = es.enter_context(nc.sbuf_tensor("cstf_sb", [128, NCF], F32))
        cstb = es.enter_context(nc.sbuf_tensor("cstb_sb", [128, NCB], BF16))
        biasT = es.enter_context(nc.sbuf_tensor("biasT_sb", [128, 4, 2, 128], F32))
        pv = [es.enter_context(nc.sbuf_tensor(f"pv{l}", [128, NPV], F32)) for l in range(DEPTH)]
        pv2 = [es.enter_context(nc.sbuf_tensor(f"pvb{l}", [128, 8], F32)) for l in range(DEPTH)]
        gw17 = [es.enter_context(nc.sbuf_tensor(f"gw{l}", [17, 128], BF16)) for l in range(DEPTH)]
        pwbd = [es.enter_context(nc.sbuf_tensor(f"pw{l}", [128, 2, 128], BF16)) for l in range(DEPTH)]
        cvec = es.enter_context(nc.sbuf_tensor("cvec", [128, 4], F32))
        ps = es.enter_context(nc.psum_tensor("ps", [128, 8, 512], F32))

        def V(off, shape, dt=BF16, p0=0):
            n = 1
            for s in shape[1:]:
                n *= s
            esz = _esz(dt)
            a = arena[p0:p0 + shape[0], off // 2: off // 2 + (n * esz) // 2]
            if dt != BF16:
                a = a.bitcast(dt)
            if len(shape) == 3:
                a = a.rearrange("p (a b) -> p a b", a=shape[1])
            elif len(shape) == 4:
                a = a.rearrange("p (a b c) -> p a b c", a=shape[1], b=shape[2])
            return a

        hT = V(0, [128, 8, S], F32)
        hn = V(64 * KB, [128, 8, S])
        mixed = V(96 * KB, [128, 8, S])
        wslot = [V(128 * KB + 8 * KB * i, [128, 8, 512]) for i in range(4)]
        T0 = 160 * KB
        ident = cst[:, F_IDENT:F_IDENT + 128]
        onesb = cstb[:, B_ONES:B_ONES + 128]
        blk64 = cstb[:, B_BLK64:B_BLK64 + 128]
        glamask = cstb[:, B_GLAMASK:B_GLAMASK + 512]
        tricat = cst[:, F_TRICAT:F_TRICAT + 130]
        umat = cst[:, F_U:F_U + 128]
        bdmask = cstb[:, B_BDMASK:B_BDMASK + 256]
        eps_t = cvec[:, 0:1]
        one_t = cvec[:, 1:2]

        bank_ctr = [0]

        def nextbank(lo=0, n=8):
            b = lo + bank_ctr[0] % n
            bank_ctr[0] += 1
            return b

        gsetup = P.group("setup")
        gx = [P.group("x0"), P.group("x1")]
        go = [P.group("o0", final=True), P.group("o1", final=True)]
        gW = {}

        def wgrp(name):
            if name not in gW:
                gW[name] = P.group(name)
            return gW[name]

        def setup():
            P.dma("sp", cst[:], cstf_d, P.group("c_cstf"))
            P.dma("pool", cstb[:], cstb_d, P.group("c_cstb"))
            P.dma("sp", biasT[:].rearrange("p a b c -> p (a b c)"), biasT_d, P.group("c_bias"))
            for l in layers:
                P.dma("sp", pv[l][:], pvec_d[l], P.group(f"c_pv{l}"))
                P.dma("pool", gw17[l][:], gw17_d[l], P.group(f"c_gw{l}"))
                P.dma("pool", pwbd[l][:].rearrange("p a b -> p (a b)"), pwbd_d[l], P.group(f"c_pw{l}"))
            P.memset("dve", cvec[:, 0:1], EPS)
            P.memset("dve", cvec[:, 1:2], 1.0)
            l0 = layers[0]
            for h in range(4):
                P.ts("dve", biasT[:, h, :, :], biasT[:, h, :, :], pv[l0][:, 22 + h:23 + h], ALU.subtract)
                P.tt("dve", biasT[:, h, 0, :], biasT[:, h, 0, :], cst[:, F_MASKNEG:F_MASKNEG + 128], ALU.add)
            P.act(biasT[:].rearrange("p a b c -> p (a b c)"), biasT[:].rearrange("p a b c -> p (a b c)"), AF.Exp)
            for l in layers:
                lam_init = 0.8 - 0.6 * math.exp(-0.3 * l)
                lvt = V(T0 + l * 2 * KB, [128, 256], F32)
                lamt = V(T0 + l * 2 * KB + KB, [128, 2, 64], F32)
                P.dma("sp", lvt, lvb_d[l], P.group(f"c_lv{l}"))
                lvb = lvt.rearrange("p (a b) -> p a b", a=4)
                P.tt("dve", lamt, lvb[:, 0::2, :], lvb[:, 1::2, :], ALU.mult)
                P.op("dve", lambda e, l=l, lamt=lamt: e.tensor_reduce(out=pv2[l][:, 4:6], in_=lamt, axis=AX.X, op=ALU.add),
                     [lamt], [pv2[l][:, 4:6]])
                P.act(pv2[l][:, 4:6], pv2[l][:, 4:6], AF.Exp)
                P.tt("dve", pv2[l][:, 6:7], pv2[l][:, 4:5], pv2[l][:, 5:6], ALU.subtract)
                P.ts("dve", pv2[l][:, 1:2], pv2[l][:, 6:7], lam_init, ALU.add, -1.0, ALU.mult)
                P.ts("dve", pv2[l][:, 0:1], pv[l][:, 16:17], 0.125, ALU.mult)
                P.ts("dve", pv2[l][:, 2:3], pv[l][:, 18:19], 1.0 - lam_init, ALU.mult)

        def load_x(s):
            for t in range(NT):
                xs = V(64 * KB + (t % 2) * 4 * KB, [128, D], F32)
                P.dma("sp", xs, x[s, t * 128:(t + 1) * 128, :], gx[t % 2])
                for half in range(2):
                    b = nextbank()
                    for kk in range(4):
                        k = half * 4 + kk
                        P.transpose(ps[:, b, kk * 128:(kk + 1) * 128], xs[:, k * 128:(k + 1) * 128], ident)
                    P.copy("dve" if half == 0 else "act", hT[:, half * 4:(half + 1) * 4, t * 128:(t + 1) * 128],
                           ps[:, b, :].rearrange("p (a b) -> p a b", a=4))

        def store_out(s):
            for t in range(NT):
                ys = V(64 * KB + (t % 2) * 4 * KB, [128, D], F32)
                for half in range(2):
                    b = nextbank()
                    for kk in range(4):
                        k = half * 4 + kk
                        P.transpose(ps[:, b, kk * 128:(kk + 1) * 128], hT[:, k, t * 128:(t + 1) * 128], ident)
                    P.copy("dve" if half == 0 else "act", ys[:, half * 512:(half + 1) * 512], ps[:, b, :])
                P.dma("sp", out[s, t * 128:(t + 1) * 128, :], ys, go[t % 2])

        def rmsnorm(l, gbase, toff):
            sqv = V(toff, [128, 8, 512])
            lnv2 = [V(toff + 8 * KB + i * 2 * KB, [128, 512], F32) for i in range(2)]
            banks = {}

            def stage_a(tb):
                sl = slice(tb * 512, (tb + 1) * 512)
                P.act(sqv, hT[:, :, sl], AF.Square)
                b = nextbank()
                banks[tb] = b
                for k in range(8):
                    P.mm(ps[:, b, :], lhsT=onesb, rhs=sqv[:, k, :], start=(k == 0), stop=(k == 7))

            def stage_b(tb):
                sl = slice(tb * 512, (tb + 1) * 512)
                lnv = lnv2[tb % 2]
                P.act(lnv, ps[:, banks[tb], :], AF.Ln, scale=1.0 / D, bias=eps_t)
                P.act(lnv, lnv, AF.Exp, scale=-0.5)
                for k in range(8):
                    P.stt(hn[:, k, sl], hT[:, k, sl], pv[l][:, gbase + k:gbase + k + 1], lnv, ALU.mult, ALU.mult)

            stage_a(0)
            for tb in range(NB):
                if tb + 1 < NB:
                    stage_a(tb + 1)
                stage_b(tb)

        def win_view(l):
            return w_in[l].rearrange("(k p) e -> p k e", p=128)

        def da_phase(l):
            wv = win_view(l)
            P.dma("pool", wslot[0], wv[:, :, 0:512], wgrp("ws0"))
            P.dma("pool", wslot[1], wv[:, :, 512:1024], wgrp("ws1"))
            P.dma("pool", wslot[2], wv[:, :, 1024:1536], wgrp("ws2"))
            P.dma("pool", wslot[3], wv[:, :, 1536:2048], wgrp("ws3"))
            qn = V(T0, [128, S])
            kn = V(T0 + 4 * KB, [128, S])
            vst = V(T0 + 8 * KB, [128, NT, 128])
            pt4 = [V(T0 + 12 * KB + i * KB, [128, 2, 256]) for i in range(4)]
            TT = T0 + 16 * KB
            raw = [V(TT + i * 2 * KB, [128, 512], F32) for i in range(2)]
            sqb = [V(TT + 4 * KB + i * KB, [128, 512]) for i in range(2)]
            lnv = [V(TT + 6 * KB + i * 2 * KB, [128, 512], F32) for i in range(2)]
            FT = TT + 10 * KB
            lc = V(FT, [128, 512], F32)
            fin = []
            for base in (FT + 2 * KB, TT):
                fin.append(dict(t0=V(base, [128, 256], F32), t1=V(base + KB, [128, 256], F32),
                                cc=V(base + 2 * KB, [128, 256], F32), sq2=V(base + 3 * KB, [128, 256]),
                                lnf=V(base + 3 * KB + 512, [128, 256], F32)))
            qpad = [V(FT + 7 * KB + i * KB, [128, 2, 256]) for i in range(2)]
            P.memset("pool", qpad[0], 0.0)
            P.memset("pool", qpad[1], 0.0)
            pending = []
            for h in range(4):
                hc = slice(h * 128, (h + 1) * 128)
                while pending:
                    pending.pop(0)[1]()
                jobs = []
                for (wi, dst, gcol) in ((0, qn, pv2[l][:, 0:1]), (1, kn, pv[l][:, 17:18])):
                    for tb in range(NB):
                        sl = slice(tb * 512, (tb + 1) * 512)
                        b = nextbank()
                        for k in range(8):
                            P.mm(ps[:, b, :], lhsT=wslot[wi][:, k, hc], rhs=hn[:, k, sl], start=(k == 0), stop=(k == 7))
                        jobs.append((b, dst, gcol, sl))
                cb = {}

                def ch_a(ji):
                    b, dst, gcol, sl = jobs[ji]
                    P.copy("dve", raw[ji % 2], ps[:, b, :])
                    P.tt("pool", sqb[ji % 2], raw[ji % 2], raw[ji % 2], ALU.mult)

                def ch_b(ji):
                    b2 = nextbank()
                    cb[ji] = b2
                    P.mm(ps[:, b2, :], lhsT=blk64, rhs=sqb[ji % 2])
                    P.act(lnv[ji % 2], ps[:, b2, :], AF.Ln, scale=1.0 / 64, bias=eps_t)
                    P.act(lnv[ji % 2], lnv[ji % 2], AF.Exp, scale=-0.5)

                def ch_c(ji):
                    b, dst, gcol, sl = jobs[ji]
                    P.stt(dst[:, sl], raw[ji % 2], gcol, lnv[ji % 2], ALU.mult, ALU.mult)

                nj = len(jobs)
                ch_a(0)
                if nj > 1:
                    ch_a(1)
                ch_b(0)
                for ji in range(nj):
                    if ji + 1 < nj:
                        ch_b(ji + 1)
                    ch_c(ji)
                    if ji + 2 < nj:
                        ch_a(ji + 2)
                for tg in range(NT // 4):
                    b = nextbank()
                    for t4 in range(4):
                        t = tg * 4 + t4
                        for k in range(8):
                            P.mm(ps[:, b, t4 * 128:(t4 + 1) * 128], lhsT=hn[:, k, t * 128:(t + 1) * 128],
                                 rhs=wslot[2][:, k, hc], start=(k == 0), stop=(k == 7))
                    P.copy("dve", vst[:, tg * 4:(tg + 1) * 4, :], ps[:, b, :].rearrange("p (a b) -> p a b", a=4))
                if h == 3:
                    P.dma("pool", wslot[0][:, :, 0:272], wv[:, :, 2048:2320], wgrp("ws0"))
                    P.dma("pool", wslot[1][:, :, 0:256], wv[:, :, 2320:2576], wgrp("ws1"))
                NC2 = S // 256
                steps = [(c, j) for c in range(NC2) for j in range(2 * c + 2)]
                LA = 3
                cf = pv[l][:, 22 + h:23 + h]
                qp_done = set()

                def geom(c, j):
                    q0 = max(j, 2 * c) * 128
                    q1 = (2 * c + 2) * 128
                    return q0, q1, q1 - q0, q0 - 2 * c * 128

                def scores(i):
                    c, j = steps[i]
                    q0, q1, n, off = geom(c, j)
                    sb = 4 + (i % 4)
                    if c not in qp_done:
                        qp_done.add(c)
                        P.copy("pool", qpad[c % 2][0:64, 0, :], qn[0:64, c * 256:(c + 1) * 256])
                        P.copy("pool", qpad[c % 2][64:128, 1, :], qn[64:128, c * 256:(c + 1) * 256])
                    for m in range(2):
                        P.mm(ps[:, sb, m * 256:m * 256 + n], lhsT=kn[:, j * 128:(j + 1) * 128],
                             rhs=qpad[c % 2][:, m, off:256])
                    sc3 = ps[:, sb, :].rearrange("p (m q) -> p m q", m=2)
                    P.act(pt4[i % 4][:, :, 0:n], sc3[:, :, 0:n], AF.Exp, bias=cf)
                    pt_ = pt4[i % 4]
                    if j >= 2 * c:
                        bt = biasT[:, h, 0, :]
                        btb = bass.AP(bt.tensor, bt.offset, [list(bt.ap[0]), [0, 2], [1, 128]])
                        P.tt("dve", pt_[:, :, 0:128], pt_[:, :, 0:128], btb, ALU.mult)
                    if 2 * c <= j + 1 <= 2 * c + 1:
                        o = (j + 1) * 128 - q0
                        bt = biasT[:, h, 1, :]
                        btb = bass.AP(bt.tensor, bt.offset, [list(bt.ap[0]), [0, 2], [1, 128]])
                        P.tt("dve", pt_[:, :, o:o + 128], pt_[:, :, o:o + 128], btb, ALU.mult)

                def pvs(i):
                    c, j = steps[i]
                    nk = 2 * c + 2
                    q0, q1, n, off = geom(c, j)
                    bO = 2 * (c % 2)
                    bL = bO + 1
                    for m in range(2):
                        pt = pt4[i % 4][:, m, 0:n]
                        st = (j == 0 and m == 0)
                        P.op("pe", lambda e, o_=ps[:, bO, m * 256 + off:(m + 1) * 256], pt=pt, st=st, j=j:
                             e.matmul(o_, lhsT=vst[:, j, :], rhs=pt, start=st, stop=(j == nk - 1), skip_group_check=True),
                             [vst[:, j, :], pt], [ps[:, bO, m * 256 + off:(m + 1) * 256]])
                        P.op("pe", lambda e, o_=ps[:, bL, m * 256 + off:(m + 1) * 256], pt=pt, st=st:
                             e.matmul(o_, lhsT=onesb, rhs=pt, start=st, stop=(j == nk - 1), skip_group_check=True),
                             [onesb, pt], [ps[:, bL, m * 256 + off:(m + 1) * 256]])
                    if j == nk - 1:
                        finalize(c)

                def finalize(c):
                    sl = slice(c * 256, (c + 1) * 256)
                    bO = 2 * (c % 2)
                    bL = bO + 1
                    f = fin[c % 2]
                    t0, t1, cc, sq2, lnf = f["t0"], f["t1"], f["cc"], f["sq2"], f["lnf"]
                    P.copy("dve", lc, ps[:, bL, :])
                    P.tt("dve", t0, ps[:, bO, 0:256], lc[:, 256:512], ALU.mult)
                    P.tt("dve", t1, ps[:, bO, 256:512], lc[:, 0:256], ALU.mult)
                    P.tt("pool", cc, lc[:, 0:256], lc[:, 256:512], ALU.mult)
                    P.stt(t0, t1, pv2[l][:, 1:2], t0, ALU.mult, ALU.add)
                    P.tt("pool", sq2, t0, t0, ALU.mult)
                    P.tt("pool", cc, cc, cc, ALU.mult)

                    def tail(h=h, sl=sl, bL=bL, t0=t0, cc=cc, sq2=sq2, lnf=lnf):
                        P.mm(ps[:, bL, 0:256], lhsT=onesb, rhs=sq2)
                        P.stt(lnf, cc, EPS * 128.0, ps[:, bL, 0:256], ALU.mult, ALU.add)
                        P.act(lnf, lnf, AF.Ln, scale=1.0 / 128)
                        P.act(lnf, lnf, AF.Exp, scale=-0.5)
                        P.stt(mixed[:, h, sl], t0, pv2[l][:, 2:3], lnf, ALU.mult, ALU.mult)
                    pending.append([min(6, 2 * c + 3), tail])

                for i in range(min(LA, len(steps))):
                    scores(i)
                for i in range(len(steps)):
                    if i + LA < len(steps):
                        scores(i + LA)
                    pvs(i)
                    for pnd in list(pending):
                        pnd[0] -= 1
                        if pnd[0] <= 0:
                            pending.remove(pnd)
                            pnd[1]()
            while pending:
                pending.pop(0)[1]()

        def gla_pool_phase(l):
            wv = win_view(l)
            wA, wB, wC = wslot[3], wslot[0], wslot[1]
            BW = 256
            NBG = S // BW
            TPB = BW // 128
            NTL = NBG * TPB
            UW = BW + 16
            G0 = T0

            def blkset(bs):
                o = G0 + bs * 6 * KB
                return dict(gqT=V(o, [128, BW]), gkT=V(o + 512, [128, BW]), gktok=V(o + 1024, [128, TPB, 128]),
                            gvp=V(o + 1536, [128, TPB, 256]), gvpad=V(o + 2560, [128, TPB, 4, 128]),
                            lr17=V(o + 4608, [17, BW]), srT=V(o + 5120, [128, 2, BW]))
            BS = [blkset(0), blkset(1)]
            F0 = G0 + 12 * KB

            def feset(ts):
                o = F0 + ts * 3 * KB
                return dict(qdec=V(o, [128, 128]), qdec32=V(o + 256, [128, 128], F32), kbd=V(o + 768, [128, 4, 128]),
                            dec=V(o + 1792, [128, 2], F32), AT=V(o + 1824, [128, 4, 128]))
            FS = [feset(0), feset(1)]
            S0 = F0 + 6 * KB
            e1 = V(S0, [128, 128], F32)
            spl = V(S0 + 512, [128, 128], F32)
            Eq = V(S0 + 1024, [128, 128], F32)
            Ek = V(S0 + 1536, [128, 128], F32)
            Ee = V(S0 + 2048, [128, 128], F32)
            kinv = V(S0 + 2560, [128, 128])
            kend = V(S0 + 2816, [128, 128])
            qbd = V(S0 + 3072, [128, 4, 128])
            Sfp = V(S0 + 4096, [128, 256], F32)
            U0 = S0 + 5 * KB
            ubuf = V(U0, [128, 2, UW], F32)
            Y1 = U0 + 2304
            Abuf = V(Y1, [128, 2, UW], F32)
            Bbuf = V(Y1 + 2176, [128, 2, UW], F32)
            pooled = V(Y1 + 4352, [128, 2, BW])
            tmp16 = V(Y1 + 5376, [128, 16], F32)
            Y2 = Y1 + 5632
            oT = V(Y2, [128, 2, BW], F32)
            sqg = V(Y2 + 2048, [128, 2, BW])
            lng = V(Y2 + 3072, [128, 2, BW], F32)
            assert Y2 + 5120 <= ARENA, (Y2 + 5120, ARENA)

            wo = w_out[l].rearrange("(k p) d -> p k d", p=128)
            P.dma("pool", wslot[2], wo[:, :, 0:512], wgrp("ws2"))

            P.memset("pool", BS[0]["gvpad"], 0.0)
            P.memset("pool", BS[1]["gvpad"], 0.0)
            P.memset("dve", Sfp, 0.0)
            P.memset("pool", ubuf[:, :, 0:16], 0.0)
            hm2 = cstb[:, B_HM:B_HM + 4]
            hm_b = bass.AP(hm2.tensor, hm2.offset, [list(hm2.ap[0]), [1, 4], [0, 128]])
            hmf = cstb[:, B_HMF:B_HMF + 512].rearrange("p (a b) -> p a b", a=4)

            def nb6():
                return nextbank(0, 6)

            def proj(tb):
                B_ = BS[tb % 2]
                sl = slice(tb * BW, (tb + 1) * BW)
                for (dst, c0) in ((B_["gqT"], 0), (B_["gkT"], 128)):
                    b = nb6()
                    for k in range(8):
                        P.mm(ps[:, b, 0:BW], lhsT=wA[:, k, c0:c0 + 128], rhs=hn[:, k, sl], start=(k == 0), stop=(k == 7))
                    P.copy("act", dst, ps[:, b, 0:BW])
                    yield
                b = nb6()
                for k in range(8):
                    P.mm(ps[0:16, b, 0:BW], lhsT=wB[:, k, 0:16], rhs=hn[:, k, sl], start=(k == 0), stop=(k == 7))
                P.memset("pool", B_["lr17"], 1.0)
                P.copy("dve", B_["lr17"][0:16, :], ps[0:16, b, 0:BW])
                yield
                for pt in range(2):
                    b = nb6()
                    for k in range(8):
                        P.mm(ps[:, b, 0:BW], lhsT=wB[:, k, 16 + pt * 128:16 + (pt + 1) * 128], rhs=hn[:, k, sl],
                             start=(k == 0), stop=(k == 7))
                    P.act(B_["srT"][:, pt, :], ps[:, b, 0:BW], AF.Silu)
                    yield
                for t4 in range(TPB):
                    t = tb * TPB + t4
                    b = nb6()
                    for k in range(8):
                        P.mm(ps[:, b, 0:384], lhsT=hn[:, k, t * 128:(t + 1) * 128], rhs=wA[:, k, 128:512],
                             start=(k == 0), stop=(k == 7))
                    P.copy("act", B_["gktok"][:, t4, :], ps[:, b, 0:128])
                    P.copy("dve", B_["gvp"][:, t4, :], ps[:, b, 128:384])
                    src = ps[:, b, 128:384].rearrange("p (h v) -> p h v", h=4)
                    P.copy("act", B_["gvpad"][:, t4, 0::2, 0:64], src[:, 0::2, :])
                    P.copy("dve", B_["gvpad"][:, t4, 1::2, 64:128], src[:, 1::2, :])
                    yield
                for pt in range(2):
                    b = nb6()
                    for k in range(8):
                        P.mm(ps[:, b, 0:BW], lhsT=wC[:, k, pt * 128:(pt + 1) * 128], rhs=hn[:, k, sl],
                             start=(k == 0), stop=(k == 7))
                    P.copy("act", ubuf[:, pt, 16:UW], ps[:, b, 0:BW])
                    yield
                P.tt("pool", Abuf[:, :, 1:UW], ubuf[:, :, 1:UW], ubuf[:, :, 0:UW - 1], ALU.add)
                P.tt("pool", Bbuf[:, :, 3:UW], Abuf[:, :, 3:UW], Abuf[:, :, 1:UW - 2], ALU.add)
                P.tt("pool", Abuf[:, 1, 7:UW], Bbuf[:, 1, 7:UW], Bbuf[:, 1, 3:UW - 4], ALU.add)
                P.tt("pool", Bbuf[:, 1, 15:UW], Abuf[:, 1, 15:UW], Abuf[:, 1, 7:UW - 8], ALU.add)
                yield
                for pt in range(2):
                    for half in range(2):
                        rows = slice(half * 64, half * 64 + 64)
                        src = (Abuf if half == 0 else Bbuf)
                        P.stt(pooled[rows, pt, :], src[rows, pt, 16:UW], cst[rows, F_INVW + pt:F_INVW + pt + 1],
                              ubuf[rows, pt, 16:UW], ALU.mult, ALU.subtract)
                        if tb == 0:
                            P.tt("dve", tmp16[rows, :], src[rows, pt, 16:32],
                                 cst[rows, F_INVC + pt * 16:F_INVC + pt * 16 + 16], ALU.mult)
                            P.tt("dve", pooled[rows, pt, 0:16], tmp16[rows, :], ubuf[rows, pt, 16:32], ALU.subtract)
                        yield
                for pt in range(2):
                    b = nb6()
                    P.mm(ps[:, b, 0:BW], lhsT=pwbd[l][:, pt, :], rhs=pooled[:, pt, :])
                    P.ts("dve", mixed[:, 6 + pt, sl], ps[:, b, 0:BW], pv[l][:, 20 + pt:21 + pt], ALU.mult)
                    yield
                P.copy("pool", ubuf[:, :, 0:16], ubuf[:, :, BW:UW])
                yield

            def front(t):
                B_ = BS[(t // TPB) % 2]
                F_ = FS[t % 2]
                t4 = t % TPB
                cols = slice(t4 * 128, (t4 + 1) * 128)
                b = nb6()
                P.mm(ps[:, b, 0:128], lhsT=B_["lr17"][0:17, cols], rhs=gw17[l][:])
                P.act(e1, ps[:, b, 0:128], AF.Exp, scale=-1.0)
                yield
                P.act(spl, e1, AF.Ln, bias=one_t)
                yield
                bA = nb6()
                P.mm(ps[:, bA, 0:130], lhsT=spl, rhs=tricat)
                bB = nb6()
                P.mm(ps[:, bB, 0:128], lhsT=umat, rhs=spl)
                P.act(Eq, ps[:, bA, 0:128], AF.Exp)
                P.act(Ek, ps[:, bA, 0:128], AF.Exp, scale=-1.0)
                P.act(F_["dec"], ps[:, bA, 128:130], AF.Exp)
                P.act(Ee, ps[:, bB, 0:128], AF.Exp)
                yield
                P.stt(F_["qdec32"], B_["gqT"][:, cols], 32.0 ** -0.5, Eq, ALU.mult, ALU.mult)
                yield
                P.copy("dve", F_["qdec"], F_["qdec32"])
                P.tt("dve", kinv, B_["gkT"][:, cols], Ek, ALU.mult)
                yield
                P.tt("dve", kend, B_["gktok"][:, t4, :], Ee, ALU.mult)
                yield
                kd = kend
                kd_b = bass.AP(kd.tensor, kd.offset, [list(kd.ap[0]), [0, 4], [1, 128]])
                P.tt("dve", F_["kbd"], kd_b, hmf, ALU.mult)
                yield
                qd = F_["qdec"]
                qd_b = bass.AP(qd.tensor, qd.offset, [list(qd.ap[0]), [0, 4], [1, 128]])
                P.tt("dve", qbd, qd_b, hm_b, ALU.mult)
                yield
                b3 = nb6()
                P.mm(ps[:, b3, :], lhsT=kinv, rhs=qbd.rearrange("p a b -> p (a b)"))
                P.tt("dve", F_["AT"].rearrange("p a b -> p (a b)"), ps[:, b3, :], glamask, ALU.mult)
                yield

            def back(t):
                B_ = BS[(t // TPB) % 2]
                F_ = FS[t % 2]
                t4 = t % TPB
                cols = slice(t4 * 128, (t4 + 1) * 128)
                AT, kbd, dec = F_["AT"], F_["kbd"], F_["dec"]
                b4 = [6, 7]
                for hp in range(2):
                    o_ap = ps[:, b4[hp], 0:128]
                    P.mm(o_ap, lhsT=B_["gvpad"][:, t4, 2 * hp, :], rhs=AT[:, 2 * hp, :], start=True, stop=False)
                    P.mm(o_ap, lhsT=B_["gvpad"][:, t4, 2 * hp + 1, :], rhs=AT[:, 2 * hp + 1, :], start=False, stop=False)
                    P.mm(ps[:, b4[hp], 0:64], lhsT=Sfp[:, hp * 128:(hp + 1) * 128], rhs=F_["qdec32"][:, 0:64],
                         start=False, stop=False)
                yield
                for ch in range(2):
                    rows = slice(ch * 64, ch * 64 + 64)
                    b5 = nb6()
                    for hh in range(4):
                        P.mm(ps[:, b5, hh * 64:(hh + 1) * 64], lhsT=kbd[rows, hh, :],
                             rhs=B_["gvp"][rows, t4, hh * 64:(hh + 1) * 64], start=(hh == 0), stop=(hh == 3))
                    P.stt(Sfp, Sfp, dec[:, ch:ch + 1], ps[:, b5, 0:256], ALU.mult, ALU.add)
                    yield
                    if ch == 0:
                        for hp in range(2):
                            P.mm(ps[:, b4[hp], 64:128], lhsT=Sfp[:, hp * 128:(hp + 1) * 128],
                                 rhs=F_["qdec32"][:, 64:128], start=False, stop=True)
                            P.copy("act", oT[:, hp, cols], ps[:, b4[hp], 0:128])
                        yield

            def norm_gate(tb):
                B_ = BS[tb % 2]
                sl = slice(tb * BW, (tb + 1) * BW)
                P.tt("pool", sqg, oT, oT, ALU.mult)
                yield
                for pt in range(2):
                    b = nb6()
                    P.mm(ps[:, b, 0:BW], lhsT=blk64, rhs=sqg[:, pt, :])
                    P.act(lng[:, pt, :], ps[:, b, 0:BW], AF.Ln, scale=1.0 / 64, bias=eps_t)
                    yield
                P.act(lng, lng, AF.Exp, scale=-0.5)
                yield
                for pt in range(2):
                    P.stt(lng[:, pt, :], oT[:, pt, :], pv[l][:, 19:20], lng[:, pt, :], ALU.mult, ALU.mult)
                    P.tt("dve", mixed[:, 4 + pt, sl], lng[:, pt, :], B_["srT"][:, pt, :], ALU.mult)
                    yield

            def chain(*gens):
                for g in gens:
                    yield from g

            def run(gens):
                gens = list(gens)
                while gens:
                    for g in list(gens):
                        try:
                            next(g)
                        except StopIteration:
                            gens.remove(g)

            run([chain(proj(0), front(0))])
            for t in range(NTL):
                tb = t // TPB
                s1 = [back(t)]
                if t % TPB == TPB - 1:
                    s1.append(norm_gate(tb))
                s2 = []
                if t + 1 < NTL:
                    if (t + 1) % TPB == 0:
                        s2.append(proj(tb + 1))
                    s2.append(front(t + 1))
                run([chain(*s1), chain(*s2)])

        def wout_phase(l):
            wo = w_out[l].rearrange("(k p) d -> p k d", p=128)
            P.dma("pool", wslot[3], wo[:, :, 512:1024], wgrp("ws3"))
            for dh in range(2):
                slot = wslot[2 + dh]
                for dc in range(4):
                    d = dh * 4 + dc
                    for tb in range(NB):
                        sl = slice(tb * 512, (tb + 1) * 512)
                        b = nextbank()
                        for k in range(8):
                            P.mm(ps[:, b, :], lhsT=slot[:, k, dc * 128:(dc + 1) * 128], rhs=mixed[:, k, sl],
                                 start=(k == 0), stop=(k == 7))
                        P.tt("dve", hT[:, d, sl], hT[:, d, sl], ps[:, b, :], ALU.add)

        def ffn_phase(l):
            wg = w_gate[l].rearrange("(k p) f -> p k f", p=128)
            wu = w_up[l].rearrange("(k p) f -> p k f", p=128)
            wd = w_down[l].rearrange("(c p) d -> p c d", p=128)
            ffT = V(96 * KB, [128, 11, S])
            gsl = [V(172 * KB, [128, 8, 512]), V(156 * KB, [128, 8, 512])]
            usl = [V(180 * KB, [128, 8, 512]), V(164 * KB, [128, 8, 512])]
            dsl = [V(140 * KB, [128, 11, 256]), V(140 * KB + 5632, [128, 11, 256])]
            sg = [V(152 * KB + i * 2 * KB, [128, 512], F32) for i in range(2)]
            groups = []
            for fh in range(2):
                f0 = fh * 1408
                for (o, w) in ((0, 512), (512, 512), (1024, 384)):
                    groups.append((fh, f0 + o, w))

            def load_group(gi):
                fh, fo, w = groups[gi]
                P.dma("pool", gsl[gi % 2][:, :, 0:w], wg[:, :, fo:fo + w], wgrp(f"fg{gi % 2}"))
                P.dma("pool", usl[gi % 2][:, :, 0:w], wu[:, :, fo:fo + w], wgrp(f"fu{gi % 2}"))

            load_group(0)
            load_group(1)
            rmsnorm(l, 8, 96 * KB)
            dcount = 0
            sgi = 0
            for gi, (fh, fo, w) in enumerate(groups):
                for fc in range(w // 128):
                    fidx = (fo - fh * 1408) // 128 + fc
                    fcs = slice(fc * 128, (fc + 1) * 128)
                    for tb in range(NB):
                        sl = slice(tb * 512, (tb + 1) * 512)
                        bg = nextbank()
                        for k in range(8):
                            P.mm(ps[:, bg, :], lhsT=gsl[gi % 2][:, k, fcs], rhs=hn[:, k, sl], start=(k == 0), stop=(k == 7))
                        bu = nextbank()
                        for k in range(8):
                            P.mm(ps[:, bu, :], lhsT=usl[gi % 2][:, k, fcs], rhs=hn[:, k, sl], start=(k == 0), stop=(k == 7))
                        s_ = sg[sgi % 2]
                        sgi += 1
                        P.act(s_, ps[:, bg, :], AF.Silu)
                        P.tt("dve", ffT[:, fidx, sl], s_, ps[:, bu, :], ALU.mult)
                if gi + 2 < len(groups):
                    load_group(gi + 2)
                if gi % 3 == 2:
                    for dp in range(4):
                        slot = dsl[dcount % 2]
                        P.dma("pool", slot, wd[:, fh * 11:(fh + 1) * 11, dp * 256:(dp + 1) * 256], wgrp(f"fd{dcount % 2}"))
                        dcount += 1
                        for dc in range(2):
                            d = dp * 2 + dc
                            for tb in range(NB):
                                sl = slice(tb * 512, (tb + 1) * 512)
                                b = nextbank()
                                for c in range(11):
                                    P.mm(ps[:, b, :], lhsT=slot[:, c, dc * 128:(dc + 1) * 128], rhs=ffT[:, c, sl],
                                         start=(c == 0), stop=(c == 10))
                                P.tt("dve", hT[:, d, sl], hT[:, d, sl], ps[:, b, :], ALU.add)

        import os as _os
        stages = _os.environ.get("KSTAGES", "setup,n,da,gla,wo,ffn").split(",")
        if "setup" in stages:
            setup()
        for s in range(NSEQ):
            load_x(s)
            for l in layers:
                if "n" in stages:
                    rmsnorm(l, 0, T0)
                if "da" in stages:
                    da_phase(l)
                if "gla" in stages:
                    gla_pool_phase(l)
                if "wo" in stages:
                    wout_phase(l)
                if "ffn" in stages:
                    ffn_phase(l)
            store_out(s)
        P.emit(nc, es)
    return nc


def _host_prep(inp):
    f = lambda k: np.ascontiguousarray(np.asarray(inp[k], dtype=np.float32))
    depth = DEPTH
    p = np.arange(128)
    pvec = np.zeros((depth, 128, NPV), np.float32)
    ang, fng = f("attn_norm_g"), f("ffn_norm_g")
    qg, kg, sg_, gg = f("q_norm_g"), f("k_norm_g"), f("da_subln_g"), f("gla_norm_g")
    psc, lv, rb = f("pool_scale"), f("lambda_vecs"), f("rel_bias")
    for l in range(depth):
        pvec[l, :, 0:8] = ang[l].reshape(8, 128).T
        pvec[l, :, 8:16] = fng[l].reshape(8, 128).T
        pvec[l, :, 16] = qg[l][p % 64]
        pvec[l, :, 17] = kg[l][p % 64]
        pvec[l, :, 18] = sg_[l]
        pvec[l, :, 19] = gg[l][p % 64]
        pvec[l, :, 20:22] = psc[l].reshape(2, 128).T
        pvec[l, :, 22:26] = rb[31][None, :]
    gw17 = np.concatenate([f("gla_gate_w"), f("gla_gate_b")[:, None, :]], axis=1)
    pw = f("pool_w")
    pwbd = np.zeros((depth, 128, 2, 128), np.float32)
    for l in range(depth):
        for g in range(4):
            pt, half = divmod(g, 2)
            pwbd[l, half * 64:(half + 1) * 64, pt, half * 64:(half + 1) * 64] = pw[l, g]
    idx = _bias_index()
    bt = rb[idx]
    biasT = np.ascontiguousarray(np.transpose(bt, (0, 3, 1, 2))).reshape(128, 4 * 2 * 128)
    return {
        "w_in": f("w_in"), "w_out": f("w_out"), "w_gate": f("w_gate"), "w_up": f("w_up"), "w_down": f("w_down"),
        "pvec": pvec, "gw17": np.ascontiguousarray(gw17), "pwbd": pwbd.reshape(depth, 128, 256),
        "biasT": biasT, "cstf": _consts()[0], "cstb": _consts()[1],
        "lvb": np.ascontiguousarray(np.broadcast_to(lv.reshape(depth, 1, 256), (depth, 128, 256))),
    }


_CACHE = {}


def kernel(**inputs):
    x = np.ascontiguousarray(np.asarray(inputs["x"], dtype=np.float32))
    B, S, _ = x.shape
    nseq = B // N_CORES
    shared = _host_prep(inputs)
    key = (S, nseq)
    if key not in _CACHE:
        _CACHE[key] = build_program(S, nseq, list(range(DEPTH)))
    nc = _CACHE[key]
    in_maps = []
    for c in range(N_CORES):
        m = dict(shared)
        m["x"] = np.ascontiguousarray(x[c * nseq:(c + 1) * nseq])
        in_maps.append(m)
    res = run_bass_kernel_spmd(nc, in_maps, core_ids=list(range(N_CORES)))
    return np.concatenate([np.asarray(r["out"]) for r in res.results], axis=0).astype(np.float32)
```

```python
import math
from contextlib import ExitStack

import numpy as np
import concourse.bass as bass
import concourse.mybir as mybir
from concourse.bass_utils import run_bass_kernel_spmd

F32 = mybir.dt.float32
BF16 = mybir.dt.bfloat16
AF = mybir.ActivationFunctionType
ALU = mybir.AluOpType
AX = mybir.AxisListType

D = 1024
DEPTH = 2
FFN = 2816
IN_TOTAL = 2576
N_CORES = 8
EPS = 1e-6
NEG = -30000.0
NPV = 26
ENGINES = ("pe", "act", "dve", "pool", "sp")
SAME_ENGINE_SYNC = True


def _esz(dt):
    s = str(dt)
    if "64" in s:
        return 8
    if "32" in s:
        return 4
    if "16" in s:
        return 2
    return 1


def region(ap):
    pat = ap.ap
    off = int(ap.offset)
    esz = _esz(ap.dtype)
    name = ap.tensor.name
    sp = str(ap.space).upper()
    if "DRAM" in sp or "HBM" in sp:
        lo = hi = off
        for s, c in pat:
            if s >= 0:
                hi += s * (c - 1)
            else:
                lo += s * (c - 1)
        return (name, 0, 1, lo * esz, (hi + 1) * esz)
    pstep, pcnt = pat[0]
    p0 = off // pstep
    f0 = off % pstep
    lo = hi = f0
    for s, c in pat[1:]:
        if s >= 0:
            hi += s * (c - 1)
        else:
            lo += s * (c - 1)
    if "PSUM" in sp:
        b0 = (lo * esz) // 2048 * 2048
        b1 = ((hi + 1) * esz + 2047) // 2048 * 2048
        return ("@" + name, 0, 128, b0, b1)
    return (name, p0, p0 + pcnt, lo * esz, (hi + 1) * esz)


class Op:
    __slots__ = ("eng", "fn", "deps", "needs_inc", "tick", "is_dma", "grp", "grp_val", "waits")

    def __init__(self, eng, fn):
        self.eng = eng
        self.fn = fn
        self.deps = set()
        self.needs_inc = False
        self.tick = None
        self.is_dma = False
        self.grp = None
        self.grp_val = None
        self.waits = None


class DmaGroup:
    def __init__(self, name):
        self.name = name
        self.sem = None
        self.count = 0
        self.final = False


class Prog:
    def __init__(self):
        self.ops = {e: [] for e in ENGINES}
        self.res = {}
        self.groups = []

    def group(self, name, final=False):
        g = DmaGroup(name)
        g.final = final
        self.groups.append(g)
        return g

    def _track(self, op, reads, writes):
        rregs = [region(a) for a in reads]
        wregs = [region(a) for a in writes]
        for (name, p0, p1, b0, b1) in rregs:
            psum = name[0] == "@"
            for e in self.res.setdefault(name, []):
                if (e[4] == "w" or (psum and e[5].eng != op.eng)) and e[0] < p1 and p0 < e[1] and e[2] < b1 and b0 < e[3]:
                    op.deps.add(e[5])
        for (name, p0, p1, b0, b1) in wregs:
            for e in self.res.setdefault(name, []):
                if e[0] < p1 and p0 < e[1] and e[2] < b1 and b0 < e[3]:
                    op.deps.add(e[5])
        op.deps.discard(op)
        for (name, p0, p1, b0, b1) in rregs:
            lst = self.res[name]
            if not op.is_dma:
                lst[:] = [e for e in lst if not (e[4] == "r" and e[5].eng == op.eng and not e[5].is_dma
                                                 and p0 <= e[0] and e[1] <= p1 and b0 <= e[2] and e[3] <= b1)]
            lst.append([p0, p1, b0, b1, "r", op])
        for (name, p0, p1, b0, b1) in wregs:
            lst = self.res[name]
            lst[:] = [e for e in lst if not (p0 <= e[0] and e[1] <= p1 and b0 <= e[2] and e[3] <= b1)]
            lst.append([p0, p1, b0, b1, "w", op])

    def op(self, eng, fn, reads=(), writes=()):
        o = Op(eng, fn)
        self._track(o, reads, writes)
        self.ops[eng].append(o)
        return o

    def dma(self, queue, out, in_, grp, **kw):
        o = Op(queue, lambda e: e.dma_start(out=out, in_=in_, **kw))
        o.is_dma = True
        o.grp = grp
        grp.count += 16
        o.grp_val = grp.count
        self._track(o, [in_], [out])
        self.ops[queue].append(o)
        return o

    def mm(self, out, lhsT, rhs, start=True, stop=True):
        return self.op("pe", lambda e: e.matmul(out, lhsT=lhsT, rhs=rhs, start=start, stop=stop),
                       [lhsT, rhs], [out])

    def transpose(self, out, in_, ident):
        return self.op("pe", lambda e: e.transpose(out, in_, ident), [in_, ident], [out])

    def act(self, out, in_, func, bias=None, scale=None):
        kw = {}
        reads = [in_]
        if bias is not None:
            kw["bias"] = bias
            if not isinstance(bias, (int, float)):
                reads.append(bias)
        if scale is not None:
            kw["scale"] = scale
            if not isinstance(scale, (int, float)):
                reads.append(scale)
        return self.op("act", lambda e: e.activation(out=out, in_=in_, func=func, **kw), reads, [out])

    def tt(self, eng, out, in0, in1, op):
        return self.op(eng, lambda e: e.tensor_tensor(out=out, in0=in0, in1=in1, op=op), [in0, in1], [out])

    def ts(self, eng, out, in0, s1, op0, s2=None, op1=None):
        reads = [in0]
        if not isinstance(s1, (int, float)):
            reads.append(s1)
        if s2 is not None and not isinstance(s2, (int, float)):
            reads.append(s2)
        if op1 is None:
            return self.op(eng, lambda e: e.tensor_scalar(out=out, in0=in0, scalar1=s1, scalar2=None, op0=op0),
                           reads, [out])
        return self.op(eng, lambda e: e.tensor_scalar(out=out, in0=in0, scalar1=s1, scalar2=s2, op0=op0, op1=op1),
                       reads, [out])

    def stt(self, out, in0, scalar, in1, op0, op1):
        reads = [in0, in1]
        if not isinstance(scalar, (int, float)):
            reads.append(scalar)
        return self.op("dve", lambda e: e.scalar_tensor_tensor(out=out, in0=in0, scalar=scalar, in1=in1,
                                                                 op0=op0, op1=op1), reads, [out])

    def copy(self, eng, out, in_):
        if eng == "act":
            return self.op(eng, lambda e: e.copy(out=out, in_=in_), [in_], [out])
        return self.op(eng, lambda e: e.tensor_copy(out=out, in_=in_), [in_], [out])

    def memset(self, eng, ap, val):
        return self.op(eng, lambda e: e.memset(ap, val), [], [ap])

    def recip(self, out, in_):
        return self.op("dve", lambda e: e.reciprocal(out=out, in_=in_), [in_], [out])

    def emit(self, nc, es):
        def skip(d, o):
            return d.eng == o.eng and not o.is_dma and (o.eng == "pe" or not SAME_ENGINE_SYNC)

        for e in ENGINES:
            for o in self.ops[e]:
                for d in o.deps:
                    if d.is_dma or skip(d, o):
                        continue
                    d.needs_inc = True
        esem = {e: es.enter_context(nc.semaphore("sem_" + e)) for e in ENGINES}
        for g in self.groups:
            g.sem = es.enter_context(nc.semaphore("dg_" + g.name))
        for e in ENGINES:
            t = 0
            for o in self.ops[e]:
                if o.needs_inc and not o.is_dma:
                    t += 1
                    o.tick = t
        for e in ENGINES:
            seen = {}
            for o in self.ops[e]:
                w = {}
                for d in o.deps:
                    if d.is_dma:
                        key = ("g", id(d.grp))
                        sem, val = d.grp.sem, d.grp_val
                    else:
                        if skip(d, o):
                            continue
                        key = ("e", d.eng)
                        sem, val = esem[d.eng], d.tick
                    if seen.get(key, 0) >= val:
                        continue
                    if key not in w or w[key][1] < val:
                        w[key] = (sem, val)
                for key, (sem, val) in w.items():
                    seen[key] = val
                o.waits = list(w.values())
        engobj = {"pe": "tensor", "act": "scalar", "dve": "vector", "pool": "gpsimd", "sp": "sync"}
        finals = [(g.sem, g.count) for g in self.groups if g.count > 0 and g.final]
        with nc.Block() as block:
            for e in ENGINES:
                def body(eng, ops=self.ops[e], e=e):
                    for o in ops:
                        for sem, val in o.waits:
                            eng.wait_ge(sem, val)
                        ins = o.fn(eng)
                        if o.is_dma:
                            ins.then_inc(o.grp.sem, 16)
                        elif o.needs_inc:
                            ins.then_inc(esem[e], 1)
                    if e == "sp":
                        for sem, val in finals:
                            eng.wait_ge(sem, val)

                getattr(block, engobj[e])(body)


def _t5_bucket_np(d):
    d = np.maximum(d, 0)
    max_exact = 16
    large = max_exact + (np.log(np.maximum(d, 1).astype(np.float32) / max_exact)
                         / math.log(128 / max_exact) * (32 - max_exact)).astype(np.int32)
    large = np.minimum(large, 31)
    return np.where(d < max_exact, d, large)


F_IDENT = 0
F_MASKNEG = 128
F_TRICAT = 256
F_U = 386
F_INVW = 514
F_INVC = 516
NCF = 548
B_GLAMASK = 0
B_BLK64 = 512
B_ONES = 640
B_BDMASK = 768
B_HM = 1024
B_HMF = 1028
NCB = 1540


def _consts():
    cf = np.zeros((128, NCF), np.float32)
    cb = np.zeros((128, NCB), np.float32)
    i = np.arange(128)
    cf[:, F_IDENT:F_IDENT + 128] = np.eye(128, dtype=np.float32)
    cf[:, F_MASKNEG:F_MASKNEG + 128] = np.where(i[:, None] > i[None, :], NEG, 0.0)
    same = (i[:, None] // 64) == (i[None, :] // 64)
    gm = (same & (i[:, None] <= i[None, :])).astype(np.float32)
    cb[:, B_GLAMASK:B_GLAMASK + 512] = np.tile(gm, (1, 4))
    cf[:, F_TRICAT:F_TRICAT + 128] = gm * (-1.0 / 16.0)
    cf[:, F_TRICAT + 128] = np.where(i < 64, -1.0 / 16.0, 0.0)
    cf[:, F_TRICAT + 129] = np.where(i >= 64, -1.0 / 16.0, 0.0)
    cf[:, F_U:F_U + 128] = (same & (i[:, None] > i[None, :])).astype(np.float32) * (-1.0 / 16.0)
    cb[:, B_BLK64:B_BLK64 + 128] = same.astype(np.float32)
    hrow = i // 32
    hcol = np.arange(256) // 64
    cb[:, B_BDMASK:B_BDMASK + 256] = (hrow[:, None] == hcol[None, :]).astype(np.float32)
    wins = (2, 4, 8, 16)
    for pt in range(2):
        for half in range(2):
            win = wins[pt * 2 + half]
            rows = slice(half * 64, half * 64 + 64)
            cf[rows, F_INVW + pt] = 1.0 / win
            t = np.arange(16)
            cf[rows, F_INVC + pt * 16:F_INVC + pt * 16 + 16] = 1.0 / np.minimum(t + 1, win)
    cb[:, B_ONES:B_ONES + 128] = 1.0
    cb[:, B_HM:B_HM + 4] = (hrow[:, None] == np.arange(4)[None, :]).astype(np.float32)
    hf = (np.arange(4)[:, None] == (np.arange(128) // 32)[None, :]).astype(np.float32).reshape(1, 512)
    cb[:, B_HMF:B_HMF + 512] = hf
    return cf, cb


def _bias_index():
    k = np.arange(128)[:, None]
    q = np.arange(128)[None, :]
    d0 = np.clip(q - k, 0, None)
    d1 = q - k + 128
    idx = np.stack([_t5_bucket_np(d0), _t5_bucket_np(d1)], axis=1)
    return idx


def build_program(S, NSEQ, layers):
    NT = S // 128
    NB = S // 512
    nc = bass.Bass("TRN2", target_bir_lowering=False)
    dt_in = lambda n, shp: nc.dram_tensor(n, shp, F32, kind="ExternalInput").ap()
    x = dt_in("x", [NSEQ, S, D])
    w_in = dt_in("w_in", [DEPTH, D, IN_TOTAL])
    w_out = dt_in("w_out", [DEPTH, D, D])
    w_gate = dt_in("w_gate", [DEPTH, D, FFN])
    w_up = dt_in("w_up", [DEPTH, D, FFN])
    w_down = dt_in("w_down", [DEPTH, FFN, D])
    pvec_d = dt_in("pvec", [DEPTH, 128, NPV])
    gw17_d = dt_in("gw17", [DEPTH, 17, 128])
    pwbd_d = dt_in("pwbd", [DEPTH, 128, 256])
    biasT_d = dt_in("biasT", [128, 4 * 2 * 128])
    cstf_d = dt_in("cstf", [128, NCF])
    cstb_d = dt_in("cstb", [128, NCB])
    lvb_d = dt_in("lvb", [DEPTH, 128, 256])
    out = nc.dram_tensor("out", [NSEQ, S, D], F32, kind="ExternalOutput").ap()

    P = Prog()
    KB = 1024
    ARENA = 196 * KB
    with ExitStack() as es:
        arena = es.enter_context(nc.sbuf_tensor("arena", [128, ARENA // 2], BF16))
        cst = es.enter_context(nc.sbuf_tensor("cstf_sb", [128, NCF], F32))
        cstb = es.enter_context(nc.sbuf_tensor("cstb_sb", [128, NCB], BF16))
        biasT = es.enter_context(nc.sbuf_tensor("biasT_sb", [128, 4, 2, 128], F32))
        pv = [es.enter_context(nc.sbuf_tensor(f"pv{l}", [128, NPV], F32)) for l in range(DEPTH)]
        pv2 = [es.enter_context(nc.sbuf_tensor(f"pvb{l}", [128, 8], F32)) for l in range(DEPTH)]
        gw17 = [es.enter_context(nc.sbuf_tensor(f"gw{l}", [17, 128], BF16)) for l in range(DEPTH)]
        pwbd = [es.enter_context(nc.sbuf_tensor(f"pw{l}", [128, 2, 128], BF16)) for l in range(DEPTH)]
        cvec = es.enter_context(nc.sbuf_tensor("cvec", [128, 4], F32))
        ps = es.enter_context(nc.psum_tensor("ps", [128, 8, 512], F32))

        def V(off, shape, dt=BF16, p0=0):
            n = 1
            for s in shape[1:]:
                n *= s
            esz = _esz(dt)
            a = arena[p0:p0 + shape[0], off // 2: off // 2 + (n * esz) // 2]
            if dt != BF16:
                a = a.bitcast(dt)
            if len(shape) == 3:
                a = a.rearrange("p (a b) -> p a b", a=shape[1])
            elif len(shape) == 4:
                a = a.rearrange("p (a b c) -> p a b c", a=shape[1], b=shape[2])
            return a

        hT = V(0, [128, 8, S], F32)
        hn = V(64 * KB, [128, 8, S])
        mixed = V(96 * KB, [128, 8, S])
        wslot = [V(128 * KB + 8 * KB * i, [128, 8, 512]) for i in range(4)]
        T0 = 160 * KB
        ident = cst[:, F_IDENT:F_IDENT + 128]
        onesb = cstb[:, B_ONES:B_ONES + 128]
        blk64 = cstb[:, B_BLK64:B_BLK64 + 128]
        glamask = cstb[:, B_GLAMASK:B_GLAMASK + 512]
        tricat = cst[:, F_TRICAT:F_TRICAT + 130]
        umat = cst[:, F_U:F_U + 128]
        bdmask = cstb[:, B_BDMASK:B_BDMASK + 256]
        eps_t = cvec[:, 0:1]
        one_t = cvec[:, 1:2]

        bank_ctr = [0]

        def nextbank(lo=0, n=8):
            b = lo + bank_ctr[0] % n
            bank_ctr[0] += 1
            return b

        gsetup = P.group("setup")
        gx = [P.group(f"x{i}") for i in range(4)]
        go = [P.group(f"o{i}", final=True) for i in range(4)]
        gW = {}

        def wgrp(name):
            if name not in gW:
                gW[name] = P.group(name)
            return gW[name]

        def setup():
            P.dma("sp", cst[:], cstf_d, P.group("c_cstf"))
            P.dma("pool", cstb[:], cstb_d, P.group("c_cstb"))
            P.dma("sp", biasT[:].rearrange("p a b c -> p (a b c)"), biasT_d, P.group("c_bias"))
            for l in layers:
                P.dma("sp", pv[l][:], pvec_d[l], P.group(f"c_pv{l}"))
                P.dma("pool", gw17[l][:], gw17_d[l], P.group(f"c_gw{l}"))
                P.dma("pool", pwbd[l][:].rearrange("p a b -> p (a b)"), pwbd_d[l], P.group(f"c_pw{l}"))
            P.memset("dve", cvec[:, 0:1], EPS)
            P.memset("dve", cvec[:, 1:2], 1.0)
            l0 = layers[0]
            for h in range(4):
                P.ts("dve", biasT[:, h, :, :], biasT[:, h, :, :], pv[l0][:, 22 + h:23 + h], ALU.subtract)
                P.tt("dve", biasT[:, h, 0, :], biasT[:, h, 0, :], cst[:, F_MASKNEG:F_MASKNEG + 128], ALU.add)
            P.act(biasT[:].rearrange("p a b c -> p (a b c)"), biasT[:].rearrange("p a b c -> p (a b c)"), AF.Exp)
            for l in layers:
                lam_init = 0.8 - 0.6 * math.exp(-0.3 * l)
                lvt = V(T0 + l * 2 * KB, [128, 256], F32)
                lamt = V(T0 + l * 2 * KB + KB, [128, 2, 64], F32)
                P.dma("sp", lvt, lvb_d[l], P.group(f"c_lv{l}"))
                lvb = lvt.rearrange("p (a b) -> p a b", a=4)
                P.tt("dve", lamt, lvb[:, 0::2, :], lvb[:, 1::2, :], ALU.mult)
                P.op("dve", lambda e, l=l, lamt=lamt: e.tensor_reduce(out=pv2[l][:, 4:6], in_=lamt, axis=AX.X, op=ALU.add),
                     [lamt], [pv2[l][:, 4:6]])
                P.act(pv2[l][:, 4:6], pv2[l][:, 4:6], AF.Exp)
                P.tt("dve", pv2[l][:, 6:7], pv2[l][:, 4:5], pv2[l][:, 5:6], ALU.subtract)
                P.ts("dve", pv2[l][:, 1:2], pv2[l][:, 6:7], lam_init, ALU.add, -1.0, ALU.mult)
                P.ts("dve", pv2[l][:, 0:1], pv[l][:, 16:17], 0.125, ALU.mult)
                P.ts("dve", pv2[l][:, 2:3], pv[l][:, 18:19], 1.0 - lam_init, ALU.mult)

        def load_x(s):
            for t in range(NT):
                xs = V(64 * KB + (t % 4) * 4 * KB, [128, D], F32)
                P.dma("sp", xs, x[s, t * 128:(t + 1) * 128, :], gx[t % 4])
                for half in range(2):
                    b = nextbank()
                    for kk in range(4):
                        k = half * 4 + kk
                        P.transpose(ps[:, b, kk * 128:(kk + 1) * 128], xs[:, k * 128:(k + 1) * 128], ident)
                    P.copy("dve" if half == 0 else "act", hT[:, half * 4:(half + 1) * 4, t * 128:(t + 1) * 128],
                           ps[:, b, :].rearrange("p (a b) -> p a b", a=4))

        def store_out(s):
            for t in range(NT):
                ys = V(64 * KB + (t % 4) * 4 * KB, [128, D], F32)
                for half in range(2):
                    b = nextbank()
                    for kk in range(4):
                        k = half * 4 + kk
                        P.transpose(ps[:, b, kk * 128:(kk + 1) * 128], hT[:, k, t * 128:(t + 1) * 128], ident)
                    P.copy("dve" if half == 0 else "act", ys[:, half * 512:(half + 1) * 512], ps[:, b, :])
                P.dma("sp", out[s, t * 128:(t + 1) * 128, :], ys, go[t % 4])

        def rmsnorm(l, gbase, toff):
            sqv = V(toff, [128, 8, 512])
            lnv2 = [V(toff + 8 * KB + i * 2 * KB, [128, 512], F32) for i in range(2)]
            banks = {}

            def stage_a(tb):
                sl = slice(tb * 512, (tb + 1) * 512)
                P.act(sqv, hT[:, :, sl], AF.Square)
                b = nextbank()
                banks[tb] = b
                for k in range(8):
                    P.mm(ps[:, b, :], lhsT=onesb, rhs=sqv[:, k, :], start=(k == 0), stop=(k == 7))

            def stage_b(tb):
                sl = slice(tb * 512, (tb + 1) * 512)
                lnv = lnv2[tb % 2]
                P.act(lnv, ps[:, banks[tb], :], AF.Ln, scale=1.0 / D, bias=eps_t)
                P.act(lnv, lnv, AF.Exp, scale=-0.5)
                for k in range(8):
                    P.stt(hn[:, k, sl], hT[:, k, sl], pv[l][:, gbase + k:gbase + k + 1], lnv, ALU.mult, ALU.mult)

            stage_a(0)
            for tb in range(NB):
                if tb + 1 < NB:
                    stage_a(tb + 1)
                stage_b(tb)

        def win_view(l):
            return w_in[l].rearrange("(k p) e -> p k e", p=128)

        def da_phase(l):
            wv = win_view(l)
            P.dma("pool", wslot[0], wv[:, :, 0:512], wgrp("ws0"))
            P.dma("pool", wslot[1], wv[:, :, 512:1024], wgrp("ws1"))
            P.dma("pool", wslot[2], wv[:, :, 1024:1536], wgrp("ws2"))
            P.dma("pool", wslot[3], wv[:, :, 1536:2048], wgrp("ws3"))
            qn = V(T0, [128, S])
            kn = V(T0 + 4 * KB, [128, S])
            vst = V(T0 + 8 * KB, [128, NT, 128])
            pt4 = [V(T0 + 12 * KB + i * KB, [128, 2, 256]) for i in range(4)]
            TT = T0 + 16 * KB
            raw = [V(TT + i * 2 * KB, [128, 512], F32) for i in range(2)]
            sqb = [V(TT + 4 * KB + i * KB, [128, 512]) for i in range(2)]
            lnv = [V(TT + 6 * KB + i * 2 * KB, [128, 512], F32) for i in range(2)]
            FT = TT + 10 * KB
            lc = V(FT, [128, 512], F32)
            fin = []
            for base in (FT + 2 * KB, TT):
                fin.append(dict(t0=V(base, [128, 256], F32), t1=V(base + KB, [128, 256], F32),
                                cc=V(base + 2 * KB, [128, 256], F32), sq2=V(base + 3 * KB, [128, 256]),
                                lnf=V(base + 3 * KB + 512, [128, 256], F32)))
            qpad = [V(FT + 7 * KB + i * KB, [128, 2, 256]) for i in range(2)]
            P.memset("pool", qpad[0], 0.0)
            P.memset("pool", qpad[1], 0.0)
            pending = []
            for h in range(4):
                hc = slice(h * 128, (h + 1) * 128)
                while pending:
                    pending.pop(0)[1]()
                jobs = []
                for (wi, dst, gcol) in ((0, qn, pv2[l][:, 0:1]), (1, kn, pv[l][:, 17:18])):
                    for tb in range(NB):
                        sl = slice(tb * 512, (tb + 1) * 512)
                        b = nextbank()
                        for k in range(8):
                            P.mm(ps[:, b, :], lhsT=wslot[wi][:, k, hc], rhs=hn[:, k, sl], start=(k == 0), stop=(k == 7))
                        jobs.append((b, dst, gcol, sl))
                cb = {}

                def ch_a(ji):
                    b, dst, gcol, sl = jobs[ji]
                    P.copy("dve", raw[ji % 2], ps[:, b, :])
                    P.tt("pool", sqb[ji % 2], raw[ji % 2], raw[ji % 2], ALU.mult)

                def ch_b(ji):
                    b2 = nextbank()
                    cb[ji] = b2
                    P.mm(ps[:, b2, :], lhsT=blk64, rhs=sqb[ji % 2])
                    P.act(lnv[ji % 2], ps[:, b2, :], AF.Ln, scale=1.0 / 64, bias=eps_t)
                    P.act(lnv[ji % 2], lnv[ji % 2], AF.Exp, scale=-0.5)

                def ch_c(ji):
                    b, dst, gcol, sl = jobs[ji]
                    P.stt(dst[:, sl], raw[ji % 2], gcol, lnv[ji % 2], ALU.mult, ALU.mult)

                nj = len(jobs)
                ch_a(0)
                if nj > 1:
                    ch_a(1)
                ch_b(0)
                for ji in range(nj):
                    if ji + 1 < nj:
                        ch_b(ji + 1)
                    ch_c(ji)
                    if ji + 2 < nj:
                        ch_a(ji + 2)
                for tg in range(NT // 4):
                    b = nextbank()
                    for t4 in range(4):
                        t = tg * 4 + t4
                        for k in range(8):
                            P.mm(ps[:, b, t4 * 128:(t4 + 1) * 128], lhsT=hn[:, k, t * 128:(t + 1) * 128],
                                 rhs=wslot[2][:, k, hc], start=(k == 0), stop=(k == 7))
                    P.copy("dve", vst[:, tg * 4:(tg + 1) * 4, :], ps[:, b, :].rearrange("p (a b) -> p a b", a=4))
                if h == 3:
                    P.dma("pool", wslot[0][:, :, 0:272], wv[:, :, 2048:2320], wgrp("ws0"))
                    P.dma("pool", wslot[1][:, :, 0:256], wv[:, :, 2320:2576], wgrp("ws1"))
                NC2 = S // 256
                steps = [(c, j) for c in range(NC2) for j in range(2 * c + 2)]
                LA = 3
                cf = pv[l][:, 22 + h:23 + h]
                qp_done = set()

                def geom(c, j):
                    q0 = max(j, 2 * c) * 128
                    q1 = (2 * c + 2) * 128
                    return q0, q1, q1 - q0, q0 - 2 * c * 128

                def scores(i):
                    c, j = steps[i]
                    q0, q1, n, off = geom(c, j)
                    sb = 4 + (i % 4)
                    if c not in qp_done:
                        qp_done.add(c)
                        P.copy("pool", qpad[c % 2][0:64, 0, :], qn[0:64, c * 256:(c + 1) * 256])
                        P.copy("pool", qpad[c % 2][64:128, 1, :], qn[64:128, c * 256:(c + 1) * 256])
                    for m in range(2):
                        P.mm(ps[:, sb, m * 256:m * 256 + n], lhsT=kn[:, j * 128:(j + 1) * 128],
                             rhs=qpad[c % 2][:, m, off:256])
                    sc3 = ps[:, sb, :].rearrange("p (m q) -> p m q", m=2)
                    P.act(pt4[i % 4][:, :, 0:n], sc3[:, :, 0:n], AF.Exp, bias=cf)
                    pt_ = pt4[i % 4]
                    if j >= 2 * c:
                        bt = biasT[:, h, 0, :]
                        btb = bass.AP(bt.tensor, bt.offset, [list(bt.ap[0]), [0, 2], [1, 128]])
                        P.tt("dve", pt_[:, :, 0:128], pt_[:, :, 0:128], btb, ALU.mult)
                    if 2 * c <= j + 1 <= 2 * c + 1:
                        o = (j + 1) * 128 - q0
                        bt = biasT[:, h, 1, :]
                        btb = bass.AP(bt.tensor, bt.offset, [list(bt.ap[0]), [0, 2], [1, 128]])
                        P.tt("dve", pt_[:, :, o:o + 128], pt_[:, :, o:o + 128], btb, ALU.mult)

                def pvs(i):
                    c, j = steps[i]
                    nk = 2 * c + 2
                    q0, q1, n, off = geom(c, j)
                    bO = 2 * (c % 2)
                    bL = bO + 1
                    for m in range(2):
                        pt = pt4[i % 4][:, m, 0:n]
                        st = (j == 0 and m == 0)
                        P.op("pe", lambda e, o_=ps[:, bO, m * 256 + off:(m + 1) * 256], pt=pt, st=st, j=j:
                             e.matmul(o_, lhsT=vst[:, j, :], rhs=pt, start=st, stop=(j == nk - 1), skip_group_check=True),
                             [vst[:, j, :], pt], [ps[:, bO, m * 256 + off:(m + 1) * 256]])
                        P.op("pe", lambda e, o_=ps[:, bL, m * 256 + off:(m + 1) * 256], pt=pt, st=st:
                             e.matmul(o_, lhsT=onesb, rhs=pt, start=st, stop=(j == nk - 1), skip_group_check=True),
                             [onesb, pt], [ps[:, bL, m * 256 + off:(m + 1) * 256]])
                    if j == nk - 1:
                        finalize(c)

                def finalize(c):
                    sl = slice(c * 256, (c + 1) * 256)
                    bO = 2 * (c % 2)
                    bL = bO + 1
                    f = fin[c % 2]
                    t0, t1, cc, sq2, lnf = f["t0"], f["t1"], f["cc"], f["sq2"], f["lnf"]
                    P.copy("dve", lc, ps[:, bL, :])
                    P.tt("dve", t0, ps[:, bO, 0:256], lc[:, 256:512], ALU.mult)
                    P.tt("dve", t1, ps[:, bO, 256:512], lc[:, 0:256], ALU.mult)
                    P.tt("pool", cc, lc[:, 0:256], lc[:, 256:512], ALU.mult)
                    P.stt(t0, t1, pv2[l][:, 1:2], t0, ALU.mult, ALU.add)
                    P.tt("pool", sq2, t0, t0, ALU.mult)
                    P.tt("pool", cc, cc, cc, ALU.mult)

                    def tail(h=h, sl=sl, bL=bL, t0=t0, cc=cc, sq2=sq2, lnf=lnf):
                        P.mm(ps[:, bL, 0:256], lhsT=onesb, rhs=sq2)
                        P.stt(lnf, cc, EPS * 128.0, ps[:, bL, 0:256], ALU.mult, ALU.add)
                        P.act(lnf, lnf, AF.Ln, scale=1.0 / 128)
                        P.act(lnf, lnf, AF.Exp, scale=-0.5)
                        P.stt(mixed[:, h, sl], t0, pv2[l][:, 2:3], lnf, ALU.mult, ALU.mult)
                    pending.append([min(6, 2 * c + 3), tail])

                for i in range(min(LA, len(steps))):
                    scores(i)
                for i in range(len(steps)):
                    if i + LA < len(steps):
                        scores(i + LA)
                    pvs(i)
                    for pnd in list(pending):
                        pnd[0] -= 1
                        if pnd[0] <= 0:
                            pending.remove(pnd)
                            pnd[1]()
            while pending:
                pending.pop(0)[1]()

        def gla_pool_phase(l):
            wv = win_view(l)
            wA, wB, wC = wslot[3], wslot[0], wslot[1]
            BW = 256
            NBG = S // BW
            TPB = BW // 128
            NTL = NBG * TPB
            UW = BW + 16
            G0 = T0

            def blkset(bs):
                o = G0 + bs * 6 * KB
                return dict(gqT=V(o, [128, BW]), gkT=V(o + 512, [128, BW]), gktok=V(o + 1024, [128, TPB, 128]),
                            gvp=V(o + 1536, [128, TPB, 256]), gvpad=V(o + 2560, [128, TPB, 4, 128]),
                            lr17=V(o + 4608, [17, BW]), srT=V(o + 5120, [128, 2, BW]))
            BS = [blkset(0), blkset(1)]
            F0 = G0 + 12 * KB

            def feset(ts):
                o = F0 + ts * 3 * KB
                return dict(qdec=V(o, [128, 128]), qdec32=V(o + 256, [128, 128], F32), kbd=V(o + 768, [128, 4, 128]),
                            dec=V(o + 1792, [128, 2], F32), AT=V(o + 1824, [128, 4, 128]))
            FS = [feset(0), feset(1)]
            S0 = F0 + 6 * KB
            e1 = V(S0, [128, 128], F32)
            spl = V(S0 + 512, [128, 128], F32)
            Eq = V(S0 + 1024, [128, 128], F32)
            Ek = V(S0 + 1536, [128, 128], F32)
            Ee = V(S0 + 2048, [128, 128], F32)
            kinv = V(S0 + 2560, [128, 128])
            kend = V(S0 + 2816, [128, 128])
            qbd = V(S0 + 3072, [128, 4, 128])
            Sfp = V(S0 + 4096, [128, 256], F32)
            U0 = S0 + 5 * KB
            ubuf = V(U0, [128, 2, UW], F32)
            Y1 = U0 + 2304
            Abuf = V(Y1, [128, 2, UW], F32)
            Bbuf = V(Y1 + 2176, [128, 2, UW], F32)
            pooled = V(Y1 + 4352, [128, 2, BW])
            tmp16 = V(Y1 + 5376, [128, 16], F32)
            Y2 = Y1 + 5632
            oT = V(Y2, [128, 2, BW], F32)
            sqg = V(Y2 + 2048, [128, 2, BW])
            lng = V(Y2 + 3072, [128, 2, BW], F32)
            assert Y2 + 5120 <= ARENA, (Y2 + 5120, ARENA)

            wo = w_out[l].rearrange("(k p) d -> p k d", p=128)
            P.dma("pool", wslot[2], wo[:, :, 0:512], wgrp("ws2"))

            P.memset("pool", BS[0]["gvpad"], 0.0)
            P.memset("pool", BS[1]["gvpad"], 0.0)
            P.memset("dve", Sfp, 0.0)
            P.memset("pool", ubuf[:, :, 0:16], 0.0)
            hm2 = cstb[:, B_HM:B_HM + 4]
            hm_b = bass.AP(hm2.tensor, hm2.offset, [list(hm2.ap[0]), [1, 4], [0, 128]])
            hmf = cstb[:, B_HMF:B_HMF + 512].rearrange("p (a b) -> p a b", a=4)

            def nb6():
                return nextbank(0, 6)

            def proj(tb):
                B_ = BS[tb % 2]
                sl = slice(tb * BW, (tb + 1) * BW)
                for (dst, c0) in ((B_["gqT"], 0), (B_["gkT"], 128)):
                    b = nb6()
                    for k in range(8):
                        P.mm(ps[:, b, 0:BW], lhsT=wA[:, k, c0:c0 + 128], rhs=hn[:, k, sl], start=(k == 0), stop=(k == 7))
                    P.copy("act", dst, ps[:, b, 0:BW])
                    yield
                b = nb6()
                for k in range(8):
                    P.mm(ps[0:16, b, 0:BW], lhsT=wB[:, k, 0:16], rhs=hn[:, k, sl], start=(k == 0), stop=(k == 7))
                P.memset("pool", B_["lr17"], 1.0)
                P.copy("dve", B_["lr17"][0:16, :], ps[0:16, b, 0:BW])
                yield
                for pt in range(2):
                    b = nb6()
                    for k in range(8):
                        P.mm(ps[:, b, 0:BW], lhsT=wB[:, k, 16 + pt * 128:16 + (pt + 1) * 128], rhs=hn[:, k, sl],
                             start=(k == 0), stop=(k == 7))
                    P.act(B_["srT"][:, pt, :], ps[:, b, 0:BW], AF.Silu)
                    yield
                for t4 in range(TPB):
                    t = tb * TPB + t4
                    b = nb6()
                    for k in range(8):
                        P.mm(ps[:, b, 0:384], lhsT=hn[:, k, t * 128:(t + 1) * 128], rhs=wA[:, k, 128:512],
                             start=(k == 0), stop=(k == 7))
                    P.copy("act", B_["gktok"][:, t4, :], ps[:, b, 0:128])
                    P.copy("dve", B_["gvp"][:, t4, :], ps[:, b, 128:384])
                    src = ps[:, b, 128:384].rearrange("p (h v) -> p h v", h=4)
                    P.copy("act", B_["gvpad"][:, t4, 0::2, 0:64], src[:, 0::2, :])
                    P.copy("dve", B_["gvpad"][:, t4, 1::2, 64:128], src[:, 1::2, :])
                    yield
                for pt in range(2):
                    b = nb6()
                    for k in range(8):
                        P.mm(ps[:, b, 0:BW], lhsT=wC[:, k, pt * 128:(pt + 1) * 128], rhs=hn[:, k, sl],
                             start=(k == 0), stop=(k == 7))
                    P.copy("act", ubuf[:, pt, 16:UW], ps[:, b, 0:BW])
                    yield
                P.tt("pool", Abuf[:, :, 1:UW], ubuf[:, :, 1:UW], ubuf[:, :, 0:UW - 1], ALU.add)
                P.tt("pool", Bbuf[:, :, 3:UW], Abuf[:, :, 3:UW], Abuf[:, :, 1:UW - 2], ALU.add)
                P.tt("pool", Abuf[:, 1, 7:UW], Bbuf[:, 1, 7:UW], Bbuf[:, 1, 3:UW - 4], ALU.add)
                P.tt("pool", Bbuf[:, 1, 15:UW], Abuf[:, 1, 15:UW], Abuf[:, 1, 7:UW - 8], ALU.add)
                yield
                for pt in range(2):
                    for half in range(2):
                        rows = slice(half * 64, half * 64 + 64)
                        src = (Abuf if half == 0 else Bbuf)
                        P.stt(pooled[rows, pt, :], src[rows, pt, 16:UW], cst[rows, F_INVW + pt:F_INVW + pt + 1],
                              ubuf[rows, pt, 16:UW], ALU.mult, ALU.subtract)
                        if tb == 0:
                            P.tt("dve", tmp16[rows, :], src[rows, pt, 16:32],
                                 cst[rows, F_INVC + pt * 16:F_INVC + pt * 16 + 16], ALU.mult)
                            P.tt("dve", pooled[rows, pt, 0:16], tmp16[rows, :], ubuf[rows, pt, 16:32], ALU.subtract)
                        yield
                for pt in range(2):
                    b = nb6()
                    P.mm(ps[:, b, 0:BW], lhsT=pwbd[l][:, pt, :], rhs=pooled[:, pt, :])
                    P.ts("dve", mixed[:, 6 + pt, sl], ps[:, b, 0:BW], pv[l][:, 20 + pt:21 + pt], ALU.mult)
                    yield
                P.copy("pool", ubuf[:, :, 0:16], ubuf[:, :, BW:UW])
                yield

            def front(t):
                B_ = BS[(t // TPB) % 2]
                F_ = FS[t % 2]
                t4 = t % TPB
                cols = slice(t4 * 128, (t4 + 1) * 128)
                b = nb6()
                P.mm(ps[:, b, 0:128], lhsT=B_["lr17"][0:17, cols], rhs=gw17[l][:])
                P.act(e1, ps[:, b, 0:128], AF.Exp, scale=-1.0)
                yield
                P.act(spl, e1, AF.Ln, bias=one_t)
                yield
                bA = nb6()
                P.mm(ps[:, bA, 0:130], lhsT=spl, rhs=tricat)
                bB = nb6()
                P.mm(ps[:, bB, 0:128], lhsT=umat, rhs=spl)
                P.act(Eq, ps[:, bA, 0:128], AF.Exp)
                P.act(Ek, ps[:, bA, 0:128], AF.Exp, scale=-1.0)
                P.act(F_["dec"], ps[:, bA, 128:130], AF.Exp)
                P.act(Ee, ps[:, bB, 0:128], AF.Exp)
                yield
                P.stt(F_["qdec32"], B_["gqT"][:, cols], 32.0 ** -0.5, Eq, ALU.mult, ALU.mult)
                yield
                P.copy("dve", F_["qdec"], F_["qdec32"])
                P.tt("dve", kinv, B_["gkT"][:, cols], Ek, ALU.mult)
                yield
                P.tt("dve", kend, B_["gktok"][:, t4, :], Ee, ALU.mult)
                yield
                kd = kend
                kd_b = bass.AP(kd.tensor, kd.offset, [list(kd.ap[0]), [0, 4], [1, 128]])
                P.tt("dve", F_["kbd"], kd_b, hmf, ALU.mult)
                yield
                qd = F_["qdec"]
                qd_b = bass.AP(qd.tensor, qd.offset, [list(qd.ap[0]), [0, 4], [1, 128]])
                P.tt("dve", qbd, qd_b, hm_b, ALU.mult)
                yield
                b3 = nb6()
                P.mm(ps[:, b3, :], lhsT=kinv, rhs=qbd.rearrange("p a b -> p (a b)"))
                P.tt("dve", F_["AT"].rearrange("p a b -> p (a b)"), ps[:, b3, :], glamask, ALU.mult)
                yield

            def back(t):
                B_ = BS[(t // TPB) % 2]
                F_ = FS[t % 2]
                t4 = t % TPB
                cols = slice(t4 * 128, (t4 + 1) * 128)
                AT, kbd, dec = F_["AT"], F_["kbd"], F_["dec"]
                b4 = [6, 7]
                for hp in range(2):
                    o_ap = ps[:, b4[hp], 0:128]
                    P.mm(o_ap, lhsT=B_["gvpad"][:, t4, 2 * hp, :], rhs=AT[:, 2 * hp, :], start=True, stop=False)
                    P.mm(o_ap, lhsT=B_["gvpad"][:, t4, 2 * hp + 1, :], rhs=AT[:, 2 * hp + 1, :], start=False, stop=False)
                    P.mm(ps[:, b4[hp], 0:64], lhsT=Sfp[:, hp * 128:(hp + 1) * 128], rhs=F_["qdec32"][:, 0:64],
                         start=False, stop=False)
                yield
                for ch in range(2):
                    rows = slice(ch * 64, ch * 64 + 64)
                    b5 = nb6()
                    for hh in range(4):
                        P.mm(ps[:, b5, hh * 64:(hh + 1) * 64], lhsT=kbd[rows, hh, :],
                             rhs=B_["gvp"][rows, t4, hh * 64:(hh + 1) * 64], start=(hh == 0), stop=(hh == 3))
                    P.stt(Sfp, Sfp, dec[:, ch:ch + 1], ps[:, b5, 0:256], ALU.mult, ALU.add)
                    yield
                    if ch == 0:
                        for hp in range(2):
                            P.mm(ps[:, b4[hp], 64:128], lhsT=Sfp[:, hp * 128:(hp + 1) * 128],
                                 rhs=F_["qdec32"][:, 64:128], start=False, stop=True)
                            P.copy("act", oT[:, hp, cols], ps[:, b4[hp], 0:128])
                        yield

            def norm_gate(tb):
                B_ = BS[tb % 2]
                sl = slice(tb * BW, (tb + 1) * BW)
                P.tt("pool", sqg, oT, oT, ALU.mult)
                yield
                for pt in range(2):
                    b = nb6()
                    P.mm(ps[:, b, 0:BW], lhsT=blk64, rhs=sqg[:, pt, :])
                    P.act(lng[:, pt, :], ps[:, b, 0:BW], AF.Ln, scale=1.0 / 64, bias=eps_t)
                    yield
                P.act(lng, lng, AF.Exp, scale=-0.5)
                yield
                for pt in range(2):
                    P.stt(lng[:, pt, :], oT[:, pt, :], pv[l][:, 19:20], lng[:, pt, :], ALU.mult, ALU.mult)
                    P.tt("dve", mixed[:, 4 + pt, sl], lng[:, pt, :], B_["srT"][:, pt, :], ALU.mult)
                    yield

            def chain(*gens):
                for g in gens:
                    yield from g

            def run(gens):
                gens = list(gens)
                while gens:
                    for g in list(gens):
                        try:
                            next(g)
                        except StopIteration:
                            gens.remove(g)

            run([chain(proj(0), front(0))])
            for t in range(NTL):
                tb = t // TPB
                s1 = [back(t)]
                if t % TPB == TPB - 1:
                    s1.append(norm_gate(tb))
                s2 = []
                if t + 1 < NTL:
                    if (t + 1) % TPB == 0:
                        s2.append(proj(tb + 1))
                    s2.append(front(t + 1))
                run([chain(*s1), chain(*s2)])

        def wout_phase(l):
            wo = w_out[l].rearrange("(k p) d -> p k d", p=128)
            P.dma("pool", wslot[3], wo[:, :, 512:1024], wgrp("ws3"))
            for dh in range(2):
                slot = wslot[2 + dh]
                for dc in range(4):
                    d = dh * 4 + dc
                    for tb in range(NB):
                        sl = slice(tb * 512, (tb + 1) * 512)
                        b = nextbank()
                        for k in range(8):
                            P.mm(ps[:, b, :], lhsT=slot[:, k, dc * 128:(dc + 1) * 128], rhs=mixed[:, k, sl],
                                 start=(k == 0), stop=(k == 7))
                        P.tt("dve", hT[:, d, sl], hT[:, d, sl], ps[:, b, :], ALU.add)

        def ffn_phase(l):
            wg = w_gate[l].rearrange("(k p) f -> p k f", p=128)
            wu = w_up[l].rearrange("(k p) f -> p k f", p=128)
            wd = w_down[l].rearrange("(c p) d -> p c d", p=128)
            ffT = V(96 * KB, [128, 11, S])
            gsl = [V(172 * KB, [128, 8, 512]), V(156 * KB, [128, 8, 512])]
            usl = [V(180 * KB, [128, 8, 512]), V(164 * KB, [128, 8, 512])]
            dsl = [V(140 * KB, [128, 11, 256]), V(140 * KB + 5632, [128, 11, 256])]
            sg = [V(152 * KB + i * 2 * KB, [128, 512], F32) for i in range(2)]
            groups = []
            for fh in range(2):
                f0 = fh * 1408
                for (o, w) in ((0, 512), (512, 512), (1024, 384)):
                    groups.append((fh, f0 + o, w))

            def load_group(gi):
                fh, fo, w = groups[gi]
                P.dma("pool", gsl[gi % 2][:, :, 0:w], wg[:, :, fo:fo + w], wgrp(f"fg{gi % 2}"))
                P.dma("pool", usl[gi % 2][:, :, 0:w], wu[:, :, fo:fo + w], wgrp(f"fu{gi % 2}"))

            def load_down(fh_, dp_):
                di = fh_ * 4 + dp_
                P.dma("pool", dsl[di % 2], wd[:, fh_ * 11:(fh_ + 1) * 11, dp_ * 256:(dp_ + 1) * 256], wgrp(f"fd{di % 2}"))

            load_group(0)
            load_group(1)
            rmsnorm(l, 8, 96 * KB)
            sgi = 0
            for gi, (fh, fo, w) in enumerate(groups):
                for fc in range(w // 128):
                    fidx = (fo - fh * 1408) // 128 + fc
                    fcs = slice(fc * 128, (fc + 1) * 128)
                    for tb in range(NB):
                        sl = slice(tb * 512, (tb + 1) * 512)
                        bg = nextbank()
                        for k in range(8):
                            P.mm(ps[:, bg, :], lhsT=gsl[gi % 2][:, k, fcs], rhs=hn[:, k, sl], start=(k == 0), stop=(k == 7))
                        bu = nextbank()
                        for k in range(8):
                            P.mm(ps[:, bu, :], lhsT=usl[gi % 2][:, k, fcs], rhs=hn[:, k, sl], start=(k == 0), stop=(k == 7))
                        s_ = sg[sgi % 2]
                        sgi += 1
                        P.act(s_, ps[:, bg, :], AF.Silu)
                        P.tt("dve", ffT[:, fidx, sl], s_, ps[:, bu, :], ALU.mult)
                if gi + 2 < len(groups):
                    load_group(gi + 2)
                if gi % 3 == 2:
                    for dp in range(4):
                        if dp + 1 < 4:
                            load_down(fh, dp + 1)
                        slot = dsl[(fh * 4 + dp) % 2]
                        for dc in range(2):
                            d = dp * 2 + dc
                            for tb in range(NB):
                                sl = slice(tb * 512, (tb + 1) * 512)
                                b = nextbank()
                                for c in range(11):
                                    P.mm(ps[:, b, :], lhsT=slot[:, c, dc * 128:(dc + 1) * 128], rhs=ffT[:, c, sl],
                                         start=(c == 0), stop=(c == 10))
                                P.tt("dve", hT[:, d, sl], hT[:, d, sl], ps[:, b, :], ALU.add)
                elif gi % 3 == 1:
                    load_down(fh, 0)

        import os as _os
        stages = _os.environ.get("KSTAGES", "setup,n,da,gla,wo,ffn").split(",")
        if "setup" in stages:
            setup()
        for s in range(NSEQ):
            load_x(s)
            for l in layers:
                if "n" in stages:
                    rmsnorm(l, 0, T0)
                if "da" in stages:
                    da_phase(l)
                if "gla" in stages:
                    gla_pool_phase(l)
                if "wo" in stages:
                    wout_phase(l)
                if "ffn" in stages:
                    ffn_phase(l)
            store_out(s)
        P.emit(nc, es)
    return nc


def _host_prep(inp):
    f = lambda k: np.ascontiguousarray(np.asarray(inp[k], dtype=np.float32))
    depth = DEPTH
    p = np.arange(128)
    pvec = np.zeros((depth, 128, NPV), np.float32)
    ang, fng = f("attn_norm_g"), f("ffn_norm_g")
    qg, kg, sg_, gg = f("q_norm_g"), f("k_norm_g"), f("da_subln_g"), f("gla_norm_g")
    psc, lv, rb = f("pool_scale"), f("lambda_vecs"), f("rel_bias")
    for l in range(depth):
        pvec[l, :, 0:8] = ang[l].reshape(8, 128).T
        pvec[l, :, 8:16] = fng[l].reshape(8, 128).T
        pvec[l, :, 16] = qg[l][p % 64]
        pvec[l, :, 17] = kg[l][p % 64]
        pvec[l, :, 18] = sg_[l]
        pvec[l, :, 19] = gg[l][p % 64]
        pvec[l, :, 20:22] = psc[l].reshape(2, 128).T
        pvec[l, :, 22:26] = rb[31][None, :]
    gw17 = np.concatenate([f("gla_gate_w"), f("gla_gate_b")[:, None, :]], axis=1)
    pw = f("pool_w")
    pwbd = np.zeros((depth, 128, 2, 128), np.float32)
    for l in range(depth):
        for g in range(4):
            pt, half = divmod(g, 2)
            pwbd[l, half * 64:(half + 1) * 64, pt, half * 64:(half + 1) * 64] = pw[l, g]
    idx = _bias_index()
    bt = rb[idx]
    biasT = np.ascontiguousarray(np.transpose(bt, (0, 3, 1, 2))).reshape(128, 4 * 2 * 128)
    return {
        "w_in": f("w_in"), "w_out": f("w_out"), "w_gate": f("w_gate"), "w_up": f("w_up"), "w_down": f("w_down"),
        "pvec": pvec, "gw17": np.ascontiguousarray(gw17), "pwbd": pwbd.reshape(depth, 128, 256),
        "biasT": biasT, "cstf": _consts()[0], "cstb": _consts()[1],
        "lvb": np.ascontiguousarray(np.broadcast_to(lv.reshape(depth, 1, 256), (depth, 128, 256))),
    }


_CACHE = {}


def kernel(**inputs):
    x = np.ascontiguousarray(np.asarray(inputs["x"], dtype=np.float32))
    B, S, _ = x.shape
    nseq = B // N_CORES
    shared = _host_prep(inputs)
    key = (S, nseq)
    if key not in _CACHE:
        _CACHE[key] = build_program(S, nseq, list(range(DEPTH)))
    nc = _CACHE[key]
    in_maps = []
    for c in range(N_CORES):
        m = dict(shared)
        m["x"] = np.ascontiguousarray(x[c * nseq:(c + 1) * nseq])
        in_maps.append(m)
    res = run_bass_kernel_spmd(nc, in_maps, core_ids=list(range(N_CORES)))
    return np.concatenate([np.asarray(r["out"]) for r in res.results], axis=0).astype(np.float32)
```

```python
import math
from contextlib import ExitStack

import numpy as np
import concourse.bass as bass
import concourse.mybir as mybir
from concourse.bass_utils import run_bass_kernel_spmd

F32 = mybir.dt.float32
BF16 = mybir.dt.bfloat16
AF = mybir.ActivationFunctionType
ALU = mybir.AluOpType
AX = mybir.AxisListType

D = 1024
DEPTH = 2
FFN = 2816
IN_TOTAL = 2576
N_CORES = 8
EPS = 1e-6
NEG = -30000.0
NPV = 26
ENGINES = ("pe", "act", "dve", "pool", "sp")
SAME_ENGINE_SYNC = True


def _esz(dt):
    s = str(dt)
    if "64" in s:
        return 8
    if "32" in s:
        return 4
    if "16" in s:
        return 2
    return 1


def region(ap):
    pat = ap.ap
    off = int(ap.offset)
    esz = _esz(ap.dtype)
    name = ap.tensor.name
    sp = str(ap.space).upper()
    if "DRAM" in sp or "HBM" in sp:
        lo = hi = off
        for s, c in pat:
            if s >= 0:
                hi += s * (c - 1)
            else:
                lo += s * (c - 1)
        return (name, 0, 1, lo * esz, (hi + 1) * esz)
    pstep, pcnt = pat[0]
    p0 = off // pstep
    f0 = off % pstep
    lo = hi = f0
    for s, c in pat[1:]:
        if s >= 0:
            hi += s * (c - 1)
        else:
            lo += s * (c - 1)
    if "PSUM" in sp:
        b0 = (lo * esz) // 2048 * 2048
        b1 = ((hi + 1) * esz + 2047) // 2048 * 2048
        return ("@" + name, 0, 128, b0, b1)
    return (name, p0, p0 + pcnt, lo * esz, (hi + 1) * esz)


class Op:
    __slots__ = ("eng", "fn", "deps", "needs_inc", "tick", "is_dma", "grp", "grp_val", "waits")

    def __init__(self, eng, fn):
        self.eng = eng
        self.fn = fn
        self.deps = set()
        self.needs_inc = False
        self.tick = None
        self.is_dma = False
        self.grp = None
        self.grp_val = None
        self.waits = None


class DmaGroup:
    def __init__(self, name):
        self.name = name
        self.sem = None
        self.count = 0
        self.final = False


class Prog:
    def __init__(self):
        self.ops = {e: [] for e in ENGINES}
        self.res = {}
        self.groups = []

    def group(self, name, final=False):
        g = DmaGroup(name)
        g.final = final
        self.groups.append(g)
        return g

    def _track(self, op, reads, writes):
        rregs = [region(a) for a in reads]
        wregs = [region(a) for a in writes]
        for (name, p0, p1, b0, b1) in rregs:
            psum = name[0] == "@"
            for e in self.res.setdefault(name, []):
                if (e[4] == "w" or (psum and e[5].eng != op.eng)) and e[0] < p1 and p0 < e[1] and e[2] < b1 and b0 < e[3]:
                    op.deps.add(e[5])
        for (name, p0, p1, b0, b1) in wregs:
            for e in self.res.setdefault(name, []):
                if e[0] < p1 and p0 < e[1] and e[2] < b1 and b0 < e[3]:
                    op.deps.add(e[5])
        op.deps.discard(op)
        for (name, p0, p1, b0, b1) in rregs:
            lst = self.res[name]
            if not op.is_dma:
                lst[:] = [e for e in lst if not (e[4] == "r" and e[5].eng == op.eng and not e[5].is_dma
                                                 and p0 <= e[0] and e[1] <= p1 and b0 <= e[2] and e[3] <= b1)]
            lst.append([p0, p1, b0, b1, "r", op])
        for (name, p0, p1, b0, b1) in wregs:
            lst = self.res[name]
            lst[:] = [e for e in lst if not (p0 <= e[0] and e[1] <= p1 and b0 <= e[2] and e[3] <= b1)]
            lst.append([p0, p1, b0, b1, "w", op])

    def op(self, eng, fn, reads=(), writes=()):
        o = Op(eng, fn)
        self._track(o, reads, writes)
        self.ops[eng].append(o)
        return o

    def dma(self, queue, out, in_, grp, **kw):
        o = Op(queue, lambda e: e.dma_start(out=out, in_=in_, **kw))
        o.is_dma = True
        o.grp = grp
        grp.count += 16
        o.grp_val = grp.count
        self._track(o, [in_], [out])
        self.ops[queue].append(o)
        return o

    def mm(self, out, lhsT, rhs, start=True, stop=True):
        return self.op("pe", lambda e: e.matmul(out, lhsT=lhsT, rhs=rhs, start=start, stop=stop),
                       [lhsT, rhs], [out])

    def transpose(self, out, in_, ident):
        return self.op("pe", lambda e: e.transpose(out, in_, ident), [in_, ident], [out])

    def act(self, out, in_, func, bias=None, scale=None):
        kw = {}
        reads = [in_]
        if bias is not None:
            kw["bias"] = bias
            if not isinstance(bias, (int, float)):
                reads.append(bias)
        if scale is not None:
            kw["scale"] = scale
            if not isinstance(scale, (int, float)):
                reads.append(scale)
        return self.op("act", lambda e: e.activation(out=out, in_=in_, func=func, **kw), reads, [out])

    def tt(self, eng, out, in0, in1, op):
        return self.op(eng, lambda e: e.tensor_tensor(out=out, in0=in0, in1=in1, op=op), [in0, in1], [out])

    def ts(self, eng, out, in0, s1, op0, s2=None, op1=None):
        reads = [in0]
        if not isinstance(s1, (int, float)):
            reads.append(s1)
        if s2 is not None and not isinstance(s2, (int, float)):
            reads.append(s2)
        if op1 is None:
            return self.op(eng, lambda e: e.tensor_scalar(out=out, in0=in0, scalar1=s1, scalar2=None, op0=op0),
                           reads, [out])
        return self.op(eng, lambda e: e.tensor_scalar(out=out, in0=in0, scalar1=s1, scalar2=s2, op0=op0, op1=op1),
                       reads, [out])

    def stt(self, out, in0, scalar, in1, op0, op1):
        reads = [in0, in1]
        if not isinstance(scalar, (int, float)):
            reads.append(scalar)
        return self.op("dve", lambda e: e.scalar_tensor_tensor(out=out, in0=in0, scalar=scalar, in1=in1,
                                                                 op0=op0, op1=op1), reads, [out])

    def copy(self, eng, out, in_):
        if eng == "act":
            return self.op(eng, lambda e: e.copy(out=out, in_=in_), [in_], [out])
        return self.op(eng, lambda e: e.tensor_copy(out=out, in_=in_), [in_], [out])

    def memset(self, eng, ap, val):
        return self.op(eng, lambda e: e.memset(ap, val), [], [ap])

    def recip(self, out, in_):
        return self.op("dve", lambda e: e.reciprocal(out=out, in_=in_), [in_], [out])

    def emit(self, nc, es):
        def skip(d, o):
            return d.eng == o.eng and not o.is_dma and (o.eng == "pe" or not SAME_ENGINE_SYNC)

        for e in ENGINES:
            for o in self.ops[e]:
                for d in o.deps:
                    if d.is_dma or skip(d, o):
                        continue
                    d.needs_inc = True
        esem = {e: es.enter_context(nc.semaphore("sem_" + e)) for e in ENGINES}
        for g in self.groups:
            g.sem = es.enter_context(nc.semaphore("dg_" + g.name))
        for e in ENGINES:
            t = 0
            for o in self.ops[e]:
                if o.needs_inc and not o.is_dma:
                    t += 1
                    o.tick = t
        for e in ENGINES:
            seen = {}
            for o in self.ops[e]:
                w = {}
                for d in o.deps:
                    if d.is_dma:
                        key = ("g", id(d.grp))
                        sem, val = d.grp.sem, d.grp_val
                    else:
                        if skip(d, o):
                            continue
                        key = ("e", d.eng)
                        sem, val = esem[d.eng], d.tick
                    if seen.get(key, 0) >= val:
                        continue
                    if key not in w or w[key][1] < val:
                        w[key] = (sem, val)
                for key, (sem, val) in w.items():
                    seen[key] = val
                o.waits = list(w.values())
        engobj = {"pe": "tensor", "act": "scalar", "dve": "vector", "pool": "gpsimd", "sp": "sync"}
        finals = [(g.sem, g.count) for g in self.groups if g.count > 0 and g.final]
        with nc.Block() as block:
            for e in ENGINES:
                def body(eng, ops=self.ops[e], e=e):
                    for o in ops:
                        for sem, val in o.waits:
                            eng.wait_ge(sem, val)
                        ins = o.fn(eng)
                        if o.is_dma:
                            ins.then_inc(o.grp.sem, 16)
                        elif o.needs_inc:
                            ins.then_inc(esem[e], 1)
                    if e == "sp":
                        for sem, val in finals:
                            eng.wait_ge(sem, val)

                getattr(block, engobj[e])(body)


def _t5_bucket_np(d):
    d = np.maximum(d, 0)
    max_exact = 16
    large = max_exact + (np.log(np.maximum(d, 1).astype(np.float32) / max_exact)
                         / math.log(128 / max_exact) * (32 - max_exact)).astype(np.int32)
    large = np.minimum(large, 31)
    return np.where(d < max_exact, d, large)


F_IDENT = 0
F_MASKNEG = 128
F_TRICAT = 256
F_U = 386
F_INVW = 514
F_INVC = 516
NCF = 548
B_GLAMASK = 0
B_BLK64 = 512
B_ONES = 640
B_BDMASK = 768
B_HM = 1024
B_HMF = 1028
NCB = 1540


def _consts():
    cf = np.zeros((128, NCF), np.float32)
    cb = np.zeros((128, NCB), np.float32)
    i = np.arange(128)
    cf[:, F_IDENT:F_IDENT + 128] = np.eye(128, dtype=np.float32)
    cf[:, F_MASKNEG:F_MASKNEG + 128] = np.where(i[:, None] > i[None, :], NEG, 0.0)
    same = (i[:, None] // 64) == (i[None, :] // 64)
    gm = (same & (i[:, None] <= i[None, :])).astype(np.float32)
    cb[:, B_GLAMASK:B_GLAMASK + 512] = np.tile(gm, (1, 4))
    cf[:, F_TRICAT:F_TRICAT + 128] = gm * (-1.0 / 16.0)
    cf[:, F_TRICAT + 128] = np.where(i < 64, -1.0 / 16.0, 0.0)
    cf[:, F_TRICAT + 129] = np.where(i >= 64, -1.0 / 16.0, 0.0)
    cf[:, F_U:F_U + 128] = (same & (i[:, None] > i[None, :])).astype(np.float32) * (-1.0 / 16.0)
    cb[:, B_BLK64:B_BLK64 + 128] = same.astype(np.float32)
    hrow = i // 32
    hcol = np.arange(256) // 64
    cb[:, B_BDMASK:B_BDMASK + 256] = (hrow[:, None] == hcol[None, :]).astype(np.float32)
    wins = (2, 4, 8, 16)
    for pt in range(2):
        for half in range(2):
            win = wins[pt * 2 + half]
            rows = slice(half * 64, half * 64 + 64)
            cf[rows, F_INVW + pt] = 1.0 / win
            t = np.arange(16)
            cf[rows, F_INVC + pt * 16:F_INVC + pt * 16 + 16] = 1.0 / np.minimum(t + 1, win)
    cb[:, B_ONES:B_ONES + 128] = 1.0
    cb[:, B_HM:B_HM + 4] = (hrow[:, None] == np.arange(4)[None, :]).astype(np.float32)
    hf = (np.arange(4)[:, None] == (np.arange(128) // 32)[None, :]).astype(np.float32).reshape(1, 512)
    cb[:, B_HMF:B_HMF + 512] = hf
    return cf, cb


def _bias_index():
    k = np.arange(128)[:, None]
    q = np.arange(128)[None, :]
    d0 = np.clip(q - k, 0, None)
    d1 = q - k + 128
    idx = np.stack([_t5_bucket_np(d0), _t5_bucket_np(d1)], axis=1)
    return idx


def build_program(S, NSEQ, layers):
    NT = S // 128
    NB = S // 512
    nc = bass.Bass("TRN2", target_bir_lowering=False)
    dt_in = lambda n, shp: nc.dram_tensor(n, shp, F32, kind="ExternalInput").ap()
    x = dt_in("x", [NSEQ, S, D])
    w_in = dt_in("w_in", [DEPTH, D, IN_TOTAL])
    w_out = dt_in("w_out", [DEPTH, D, D])
    w_gate = dt_in("w_gate", [DEPTH, D, FFN])
    w_up = dt_in("w_up", [DEPTH, D, FFN])
    w_down = dt_in("w_down", [DEPTH, FFN, D])
    pvec_d = dt_in("pvec", [DEPTH, 128, NPV])
    gw17_d = dt_in("gw17", [DEPTH, 17, 128])
    pwbd_d = dt_in("pwbd", [DEPTH, 128, 256])
    biasT_d = dt_in("biasT", [128, 4 * 2 * 128])
    cstf_d = dt_in("cstf", [128, NCF])
    cstb_d = dt_in("cstb", [128, NCB])
    lvb_d = dt_in("lvb", [DEPTH, 128, 256])
    out = nc.dram_tensor("out", [NSEQ, S, D], F32, kind="ExternalOutput").ap()

    P = Prog()
    KB = 1024
    ARENA = 196 * KB
    with ExitStack() as es:
        arena = es.enter_context(nc.sbuf_tensor("arena", [128, ARENA // 2], BF16))
        cst = es.enter_context(nc.sbuf_tensor("cstf_sb", [128, NCF], F32))
        cstb = es.enter_context(nc.sbuf_tensor("cstb_sb", [128, NCB], BF16))
        biasT = es.enter_context(nc.sbuf_tensor("biasT_sb", [128, 4, 2, 128], F32))
        pv = [es.enter_context(nc.sbuf_tensor(f"pv{l}", [128, NPV], F32)) for l in range(DEPTH)]
        pv2 = [es.enter_context(nc.sbuf_tensor(f"pvb{l}", [128, 8], F32)) for l in range(DEPTH)]
        gw17 = [es.enter_context(nc.sbuf_tensor(f"gw{l}", [17, 128], BF16)) for l in range(DEPTH)]
        pwbd = [es.enter_context(nc.sbuf_tensor(f"pw{l}", [128, 2, 128], BF16)) for l in range(DEPTH)]
        cvec = es.enter_context(nc.sbuf_tensor("cvec", [128, 4], F32))
        ps = es.enter_context(nc.psum_tensor("ps", [128, 8, 512], F32))

        def V(off, shape, dt=BF16, p0=0):
            n = 1
            for s in shape[1:]:
                n *= s
            esz = _esz(dt)
            a = arena[p0:p0 + shape[0], off // 2: off // 2 + (n * esz) // 2]
            if dt != BF16:
                a = a.bitcast(dt)
            if len(shape) == 3:
                a = a.rearrange("p (a b) -> p a b", a=shape[1])
            elif len(shape) == 4:
                a = a.rearrange("p (a b c) -> p a b c", a=shape[1], b=shape[2])
            return a

        hT = V(0, [128, 8, S], F32)
        hn = V(64 * KB, [128, 8, S])
        mixed = V(96 * KB, [128, 8, S])
        wslot = [V(128 * KB + 8 * KB * i, [128, 8, 512]) for i in range(4)]
        T0 = 160 * KB
        ident = cst[:, F_IDENT:F_IDENT + 128]
        onesb = cstb[:, B_ONES:B_ONES + 128]
        blk64 = cstb[:, B_BLK64:B_BLK64 + 128]
        glamask = cstb[:, B_GLAMASK:B_GLAMASK + 512]
        tricat = cst[:, F_TRICAT:F_TRICAT + 130]
        umat = cst[:, F_U:F_U + 128]
        bdmask = cstb[:, B_BDMASK:B_BDMASK + 256]
        eps_t = cvec[:, 0:1]
        one_t = cvec[:, 1:2]

        bank_ctr = [0]

        def nextbank(lo=0, n=8):
            b = lo + bank_ctr[0] % n
            bank_ctr[0] += 1
            return b

        gsetup = P.group("setup")
        gx = [P.group(f"x{i}") for i in range(4)]
        go = [P.group(f"o{i}", final=True) for i in range(4)]
        gW = {}

        def wgrp(name):
            if name not in gW:
                gW[name] = P.group(name)
            return gW[name]

        def setup():
            P.dma("sp", cst[:], cstf_d, P.group("c_cstf"))
            P.dma("pool", cstb[:], cstb_d, P.group("c_cstb"))
            P.dma("sp", biasT[:].rearrange("p a b c -> p (a b c)"), biasT_d, P.group("c_bias"))
            for l in layers:
                P.dma("sp", pv[l][:], pvec_d[l], P.group(f"c_pv{l}"))
                P.dma("pool", gw17[l][:], gw17_d[l], P.group(f"c_gw{l}"))
                P.dma("pool", pwbd[l][:].rearrange("p a b -> p (a b)"), pwbd_d[l], P.group(f"c_pw{l}"))
            P.memset("dve", cvec[:, 0:1], EPS)
            P.memset("dve", cvec[:, 1:2], 1.0)
            l0 = layers[0]
            for h in range(4):
                P.ts("dve", biasT[:, h, :, :], biasT[:, h, :, :], pv[l0][:, 22 + h:23 + h], ALU.subtract)
                P.tt("dve", biasT[:, h, 0, :], biasT[:, h, 0, :], cst[:, F_MASKNEG:F_MASKNEG + 128], ALU.add)
            P.act(biasT[:].rearrange("p a b c -> p (a b c)"), biasT[:].rearrange("p a b c -> p (a b c)"), AF.Exp)
            for l in layers:
                lam_init = 0.8 - 0.6 * math.exp(-0.3 * l)
                lvt = V(T0 + l * 2 * KB, [128, 256], F32)
                lamt = V(T0 + l * 2 * KB + KB, [128, 2, 64], F32)
                P.dma("sp", lvt, lvb_d[l], P.group(f"c_lv{l}"))
                lvb = lvt.rearrange("p (a b) -> p a b", a=4)
                P.tt("dve", lamt, lvb[:, 0::2, :], lvb[:, 1::2, :], ALU.mult)
                P.op("dve", lambda e, l=l, lamt=lamt: e.tensor_reduce(out=pv2[l][:, 4:6], in_=lamt, axis=AX.X, op=ALU.add),
                     [lamt], [pv2[l][:, 4:6]])
                P.act(pv2[l][:, 4:6], pv2[l][:, 4:6], AF.Exp)
                P.tt("dve", pv2[l][:, 6:7], pv2[l][:, 4:5], pv2[l][:, 5:6], ALU.subtract)
                P.ts("dve", pv2[l][:, 1:2], pv2[l][:, 6:7], lam_init, ALU.add, -1.0, ALU.mult)
                P.ts("dve", pv2[l][:, 0:1], pv[l][:, 16:17], 0.125, ALU.mult)
                P.ts("dve", pv2[l][:, 2:3], pv[l][:, 18:19], 1.0 - lam_init, ALU.mult)

        def load_x(s):
            for t in range(NT):
                xs = V(64 * KB + (t % 4) * 4 * KB, [128, D], F32)
                P.dma("sp", xs, x[s, t * 128:(t + 1) * 128, :], gx[t % 4])
                for half in range(2):
                    b = nextbank()
                    for kk in range(4):
                        k = half * 4 + kk
                        P.transpose(ps[:, b, kk * 128:(kk + 1) * 128], xs[:, k * 128:(k + 1) * 128], ident)
                    P.copy("dve" if half == 0 else "act", hT[:, half * 4:(half + 1) * 4, t * 128:(t + 1) * 128],
                           ps[:, b, :].rearrange("p (a b) -> p a b", a=4))

        def store_out(s):
            for t in range(NT):
                ys = V(64 * KB + (t % 4) * 4 * KB, [128, D], F32)
                for half in range(2):
                    b = nextbank()
                    for kk in range(4):
                        k = half * 4 + kk
                        P.transpose(ps[:, b, kk * 128:(kk + 1) * 128], hT[:, k, t * 128:(t + 1) * 128], ident)
                    P.copy("dve" if half == 0 else "act", ys[:, half * 512:(half + 1) * 512], ps[:, b, :])
                P.dma("sp", out[s, t * 128:(t + 1) * 128, :], ys, go[t % 4])

        def rmsnorm(l, gbase, toff):
            sqv = V(toff, [128, 8, 512])
            lnv2 = [V(toff + 8 * KB + i * 2 * KB, [128, 512], F32) for i in range(2)]
            banks = {}

            def stage_a(tb):
                sl = slice(tb * 512, (tb + 1) * 512)
                P.act(sqv, hT[:, :, sl], AF.Square)
                b = nextbank()
                banks[tb] = b
                for k in range(8):
                    P.mm(ps[:, b, :], lhsT=onesb, rhs=sqv[:, k, :], start=(k == 0), stop=(k == 7))

            def stage_b(tb):
                sl = slice(tb * 512, (tb + 1) * 512)
                lnv = lnv2[tb % 2]
                P.act(lnv, ps[:, banks[tb], :], AF.Ln, scale=1.0 / D, bias=eps_t)
                P.act(lnv, lnv, AF.Exp, scale=-0.5)
                for k in range(8):
                    P.stt(hn[:, k, sl], hT[:, k, sl], pv[l][:, gbase + k:gbase + k + 1], lnv, ALU.mult, ALU.mult)

            stage_a(0)
            for tb in range(NB):
                if tb + 1 < NB:
                    stage_a(tb + 1)
                stage_b(tb)

        def win_view(l):
            return w_in[l].rearrange("(k p) e -> p k e", p=128)

        def da_phase(l):
            wv = win_view(l)
            P.dma("pool", wslot[0], wv[:, :, 0:512], wgrp("ws0"))
            P.dma("pool", wslot[1], wv[:, :, 512:1024], wgrp("ws1"))
            P.dma("pool", wslot[2], wv[:, :, 1024:1536], wgrp("ws2"))
            P.dma("pool", wslot[3], wv[:, :, 1536:2048], wgrp("ws3"))
            qn = V(T0, [128, S])
            kn = V(T0 + 4 * KB, [128, S])
            vst = V(T0 + 8 * KB, [128, NT, 128])
            pt4 = [V(T0 + 12 * KB + i * KB, [128, 2, 256]) for i in range(4)]
            TT = T0 + 16 * KB
            raw = [V(TT + i * 2 * KB, [128, 512], F32) for i in range(2)]
            sqb = [V(TT + 4 * KB + i * KB, [128, 512]) for i in range(2)]
            lnv = [V(TT + 6 * KB + i * 2 * KB, [128, 512], F32) for i in range(2)]
            FT = TT + 10 * KB
            lc = V(FT, [128, 512], F32)
            fin = []
            for base in (FT + 2 * KB, TT):
                fin.append(dict(t0=V(base, [128, 256], F32), t1=V(base + KB, [128, 256], F32),
                                cc=V(base + 2 * KB, [128, 256], F32), sq2=V(base + 3 * KB, [128, 256]),
                                lnf=V(base + 3 * KB + 512, [128, 256], F32)))
            qpad = [V(FT + 7 * KB + i * KB, [128, 2, 256]) for i in range(2)]
            P.memset("pool", qpad[0], 0.0)
            P.memset("pool", qpad[1], 0.0)
            pending = []
            for h in range(4):
                hc = slice(h * 128, (h + 1) * 128)
                while pending:
                    pending.pop(0)[1]()
                jobs = []
                for (wi, dst, gcol) in ((0, qn, pv2[l][:, 0:1]), (1, kn, pv[l][:, 17:18])):
                    for tb in range(NB):
                        sl = slice(tb * 512, (tb + 1) * 512)
                        b = nextbank()
                        for k in range(8):
                            P.mm(ps[:, b, :], lhsT=wslot[wi][:, k, hc], rhs=hn[:, k, sl], start=(k == 0), stop=(k == 7))
                        jobs.append((b, dst, gcol, sl))
                cb = {}

                def ch_a(ji):
                    b, dst, gcol, sl = jobs[ji]
                    P.copy("dve", raw[ji % 2], ps[:, b, :])
                    P.tt("pool", sqb[ji % 2], raw[ji % 2], raw[ji % 2], ALU.mult)

                def ch_b(ji):
                    b2 = jobs[ji][0]
                    P.mm(ps[:, b2, :], lhsT=blk64, rhs=sqb[ji % 2])
                    P.act(lnv[ji % 2], ps[:, b2, :], AF.Ln, scale=1.0 / 64, bias=eps_t)
                    P.act(lnv[ji % 2], lnv[ji % 2], AF.Exp, scale=-0.5)

                def ch_c(ji):
                    b, dst, gcol, sl = jobs[ji]
                    P.stt(dst[:, sl], raw[ji % 2], gcol, lnv[ji % 2], ALU.mult, ALU.mult)

                def vgroup(tg, b):
                    for t4 in range(4):
                        t = tg * 4 + t4
                        for k in range(8):
                            P.mm(ps[:, b, t4 * 128:(t4 + 1) * 128], lhsT=hn[:, k, t * 128:(t + 1) * 128],
                                 rhs=wslot[2][:, k, hc], start=(k == 0), stop=(k == 7))
                    P.copy("act", vst[:, tg * 4:(tg + 1) * 4, :], ps[:, b, :].rearrange("p (a b) -> p a b", a=4))

                nj = len(jobs)
                nvg = NT // 4
                vdone = 0
                ch_a(0)
                if nj > 1:
                    ch_a(1)
                ch_b(0)
                for ji in range(nj):
                    if ji + 1 < nj:
                        ch_b(ji + 1)
                    ch_c(ji)
                    if ji + 2 < nj:
                        ch_a(ji + 2)
                    if vdone < nvg:
                        vgroup(vdone, jobs[ji][0])
                        vdone += 1
                while vdone < nvg:
                    vgroup(vdone, nextbank())
                    vdone += 1
                if h == 3:
                    P.dma("pool", wslot[0][:, :, 0:272], wv[:, :, 2048:2320], wgrp("ws0"))
                    P.dma("pool", wslot[1][:, :, 0:256], wv[:, :, 2320:2576], wgrp("ws1"))
                NC2 = S // 256
                steps = [(c, j) for c in range(NC2) for j in range(2 * c + 2)]
                LA = 3
                cf = pv[l][:, 22 + h:23 + h]
                qp_done = set()

                def geom(c, j):
                    q0 = max(j, 2 * c) * 128
                    q1 = (2 * c + 2) * 128
                    return q0, q1, q1 - q0, q0 - 2 * c * 128

                def scores(i):
                    c, j = steps[i]
                    q0, q1, n, off = geom(c, j)
                    sb = 4 + (i % 4)
                    if c not in qp_done:
                        qp_done.add(c)
                        P.copy("pool", qpad[c % 2][0:64, 0, :], qn[0:64, c * 256:(c + 1) * 256])
                        P.copy("pool", qpad[c % 2][64:128, 1, :], qn[64:128, c * 256:(c + 1) * 256])
                    for m in range(2):
                        P.mm(ps[:, sb, m * 256:m * 256 + n], lhsT=kn[:, j * 128:(j + 1) * 128],
                             rhs=qpad[c % 2][:, m, off:256])
                    sc3 = ps[:, sb, :].rearrange("p (m q) -> p m q", m=2)
                    P.act(pt4[i % 4][:, :, 0:n], sc3[:, :, 0:n], AF.Exp, bias=cf)
                    pt_ = pt4[i % 4]
                    if j >= 2 * c:
                        bt = biasT[:, h, 0, :]
                        btb = bass.AP(bt.tensor, bt.offset, [list(bt.ap[0]), [0, 2], [1, 128]])
                        P.tt("dve", pt_[:, :, 0:128], pt_[:, :, 0:128], btb, ALU.mult)
                    if 2 * c <= j + 1 <= 2 * c + 1:
                        o = (j + 1) * 128 - q0
                        bt = biasT[:, h, 1, :]
                        btb = bass.AP(bt.tensor, bt.offset, [list(bt.ap[0]), [0, 2], [1, 128]])
                        P.tt("dve", pt_[:, :, o:o + 128], pt_[:, :, o:o + 128], btb, ALU.mult)

                def pvs(i):
                    c, j = steps[i]
                    nk = 2 * c + 2
                    q0, q1, n, off = geom(c, j)
                    bO = 2 * (c % 2)
                    bL = bO + 1
                    for m in range(2):
                        pt = pt4[i % 4][:, m, 0:n]
                        st = (j == 0 and m == 0)
                        P.op("pe", lambda e, o_=ps[:, bO, m * 256 + off:(m + 1) * 256], pt=pt, st=st, j=j:
                             e.matmul(o_, lhsT=vst[:, j, :], rhs=pt, start=st, stop=(j == nk - 1), skip_group_check=True),
                             [vst[:, j, :], pt], [ps[:, bO, m * 256 + off:(m + 1) * 256]])
                        P.op("pe", lambda e, o_=ps[:, bL, m * 256 + off:(m + 1) * 256], pt=pt, st=st:
                             e.matmul(o_, lhsT=onesb, rhs=pt, start=st, stop=(j == nk - 1), skip_group_check=True),
                             [onesb, pt], [ps[:, bL, m * 256 + off:(m + 1) * 256]])
                    if j == nk - 1:
                        finalize(c)

                def finalize(c):
                    sl = slice(c * 256, (c + 1) * 256)
                    bO = 2 * (c % 2)
                    bL = bO + 1
                    f = fin[c % 2]
                    t0, t1, cc, sq2, lnf = f["t0"], f["t1"], f["cc"], f["sq2"], f["lnf"]
                    P.copy("dve", lc, ps[:, bL, :])
                    P.tt("dve", t0, ps[:, bO, 0:256], lc[:, 256:512], ALU.mult)
                    P.tt("dve", t1, ps[:, bO, 256:512], lc[:, 0:256], ALU.mult)
                    P.tt("pool", cc, lc[:, 0:256], lc[:, 256:512], ALU.mult)
                    P.stt(t0, t1, pv2[l][:, 1:2], t0, ALU.mult, ALU.add)
                    P.tt("pool", sq2, t0, t0, ALU.mult)
                    P.tt("pool", cc, cc, cc, ALU.mult)

                    def tail(h=h, sl=sl, bL=bL, t0=t0, cc=cc, sq2=sq2, lnf=lnf):
                        P.mm(ps[:, bL, 0:256], lhsT=onesb, rhs=sq2)
                        P.stt(lnf, cc, EPS * 128.0, ps[:, bL, 0:256], ALU.mult, ALU.add)
                        P.act(lnf, lnf, AF.Ln, scale=1.0 / 128)
                        P.act(lnf, lnf, AF.Exp, scale=-0.5)
                        P.stt(mixed[:, h, sl], t0, pv2[l][:, 2:3], lnf, ALU.mult, ALU.mult)
                    pending.append([min(6, 2 * c + 3), tail])

                for i in range(min(LA, len(steps))):
                    scores(i)
                for i in range(len(steps)):
                    if i + LA < len(steps):
                        scores(i + LA)
                    pvs(i)
                    for pnd in list(pending):
                        pnd[0] -= 1
                        if pnd[0] <= 0:
                            pending.remove(pnd)
                            pnd[1]()
            while pending:
                pending.pop(0)[1]()

        def gla_pool_phase(l):
            wv = win_view(l)
            wA, wB, wC = wslot[3], wslot[0], wslot[1]
            BW = 256
            NBG = S // BW
            TPB = BW // 128
            NTL = NBG * TPB
            UW = BW + 16
            G0 = T0

            def blkset(bs):
                o = G0 + bs * 6 * KB
                return dict(gqT=V(o, [128, BW]), gkT=V(o + 512, [128, BW]), gktok=V(o + 1024, [128, TPB, 128]),
                            gvp=V(o + 1536, [128, TPB, 256]), gvpad=V(o + 2560, [128, TPB, 4, 128]),
                            lr17=V(o + 4608, [17, BW]), srT=V(o + 5120, [128, 2, BW]))
            BS = [blkset(0), blkset(1)]
            F0 = G0 + 12 * KB

            def feset(ts):
                o = F0 + ts * 3 * KB
                return dict(qdec=V(o, [128, 128]), qdec32=V(o + 256, [128, 128], F32), kbd=V(o + 768, [128, 4, 128]),
                            dec=V(o + 1792, [128, 2], F32), AT=V(o + 1824, [128, 4, 128]))
            FS = [feset(0), feset(1)]
            S0 = F0 + 6 * KB
            e1 = V(S0, [128, 128], F32)
            spl = V(S0 + 512, [128, 128], F32)
            Eq = V(S0 + 1024, [128, 128], F32)
            Ek = V(S0 + 1536, [128, 128], F32)
            Ee = V(S0 + 2048, [128, 128], F32)
            kinv = V(S0 + 2560, [128, 128])
            kend = V(S0 + 2816, [128, 128])
            qbd = V(S0 + 3072, [128, 4, 128])
            Sfp = V(S0 + 4096, [128, 256], F32)
            U0 = S0 + 5 * KB
            ubuf = V(U0, [128, 2, UW], F32)
            Y1 = U0 + 2304
            Abuf = V(Y1, [128, 2, UW], F32)
            Bbuf = V(Y1 + 2176, [128, 2, UW], F32)
            pooled = V(Y1 + 4352, [128, 2, BW])
            tmp16 = V(Y1 + 5376, [128, 16], F32)
            Y2 = Y1 + 5632
            oT = V(Y2, [128, 2, BW], F32)
            sqg = V(Y2 + 2048, [128, 2, BW])
            lng = V(Y2 + 3072, [128, 2, BW], F32)
            assert Y2 + 5120 <= ARENA, (Y2 + 5120, ARENA)

            wo = w_out[l].rearrange("(k p) d -> p k d", p=128)
            P.dma("pool", wslot[2], wo[:, :, 0:512], wgrp("ws2"))

            P.memset("pool", BS[0]["gvpad"], 0.0)
            P.memset("pool", BS[1]["gvpad"], 0.0)
            P.memset("dve", Sfp, 0.0)
            P.memset("pool", ubuf[:, :, 0:16], 0.0)
            hm2 = cstb[:, B_HM:B_HM + 4]
            hm_b = bass.AP(hm2.tensor, hm2.offset, [list(hm2.ap[0]), [1, 4], [0, 128]])
            hmf = cstb[:, B_HMF:B_HMF + 512].rearrange("p (a b) -> p a b", a=4)

            def nb6():
                return nextbank(0, 6)

            def proj(tb):
                B_ = BS[tb % 2]
                sl = slice(tb * BW, (tb + 1) * BW)
                for (dst, c0) in ((B_["gqT"], 0), (B_["gkT"], 128)):
                    b = nb6()
                    for k in range(8):
                        P.mm(ps[:, b, 0:BW], lhsT=wA[:, k, c0:c0 + 128], rhs=hn[:, k, sl], start=(k == 0), stop=(k == 7))
                    P.copy("act", dst, ps[:, b, 0:BW])
                    yield
                b = nb6()
                for k in range(8):
                    P.mm(ps[0:16, b, 0:BW], lhsT=wB[:, k, 0:16], rhs=hn[:, k, sl], start=(k == 0), stop=(k == 7))
                P.memset("pool", B_["lr17"], 1.0)
                P.copy("dve", B_["lr17"][0:16, :], ps[0:16, b, 0:BW])
                yield
                for pt in range(2):
                    b = nb6()
                    for k in range(8):
                        P.mm(ps[:, b, 0:BW], lhsT=wB[:, k, 16 + pt * 128:16 + (pt + 1) * 128], rhs=hn[:, k, sl],
                             start=(k == 0), stop=(k == 7))
                    P.act(B_["srT"][:, pt, :], ps[:, b, 0:BW], AF.Silu)
                    yield
                for t4 in range(TPB):
                    t = tb * TPB + t4
                    b = nb6()
                    for k in range(8):
                        P.mm(ps[:, b, 0:384], lhsT=hn[:, k, t * 128:(t + 1) * 128], rhs=wA[:, k, 128:512],
                             start=(k == 0), stop=(k == 7))
                    P.copy("act", B_["gktok"][:, t4, :], ps[:, b, 0:128])
                    P.copy("dve", B_["gvp"][:, t4, :], ps[:, b, 128:384])
                    src = ps[:, b, 128:384].rearrange("p (h v) -> p h v", h=4)
                    P.copy("act", B_["gvpad"][:, t4, 0::2, 0:64], src[:, 0::2, :])
                    P.copy("dve", B_["gvpad"][:, t4, 1::2, 64:128], src[:, 1::2, :])
                    yield
                for pt in range(2):
                    b = nb6()
                    for k in range(8):
                        P.mm(ps[:, b, 0:BW], lhsT=wC[:, k, pt * 128:(pt + 1) * 128], rhs=hn[:, k, sl],
                             start=(k == 0), stop=(k == 7))
                    P.copy("act", ubuf[:, pt, 16:UW], ps[:, b, 0:BW])
                    yield
                P.tt("pool", Abuf[:, :, 1:UW], ubuf[:, :, 1:UW], ubuf[:, :, 0:UW - 1], ALU.add)
                P.tt("pool", Bbuf[:, :, 3:UW], Abuf[:, :, 3:UW], Abuf[:, :, 1:UW - 2], ALU.add)
                P.tt("pool", Abuf[:, 1, 7:UW], Bbuf[:, 1, 7:UW], Bbuf[:, 1, 3:UW - 4], ALU.add)
                P.tt("pool", Bbuf[:, 1, 15:UW], Abuf[:, 1, 15:UW], Abuf[:, 1, 7:UW - 8], ALU.add)
                yield
                for pt in range(2):
                    for half in range(2):
                        rows = slice(half * 64, half * 64 + 64)
                        src = (Abuf if half == 0 else Bbuf)
                        P.stt(pooled[rows, pt, :], src[rows, pt, 16:UW], cst[rows, F_INVW + pt:F_INVW + pt + 1],
                              ubuf[rows, pt, 16:UW], ALU.mult, ALU.subtract)
                        if tb == 0:
                            P.tt("dve", tmp16[rows, :], src[rows, pt, 16:32],
                                 cst[rows, F_INVC + pt * 16:F_INVC + pt * 16 + 16], ALU.mult)
                            P.tt("dve", pooled[rows, pt, 0:16], tmp16[rows, :], ubuf[rows, pt, 16:32], ALU.subtract)
                        yield
                for pt in range(2):
                    b = nb6()
                    P.mm(ps[:, b, 0:BW], lhsT=pwbd[l][:, pt, :], rhs=pooled[:, pt, :])
                    P.ts("dve", mixed[:, 6 + pt, sl], ps[:, b, 0:BW], pv[l][:, 20 + pt:21 + pt], ALU.mult)
                    yield
                P.copy("pool", ubuf[:, :, 0:16], ubuf[:, :, BW:UW])
                yield

            def front(t):
                B_ = BS[(t // TPB) % 2]
                F_ = FS[t % 2]
                t4 = t % TPB
                cols = slice(t4 * 128, (t4 + 1) * 128)
                b = nb6()
                P.mm(ps[:, b, 0:128], lhsT=B_["lr17"][0:17, cols], rhs=gw17[l][:])
                P.act(e1, ps[:, b, 0:128], AF.Exp, scale=-1.0)
                yield
                P.act(spl, e1, AF.Ln, bias=one_t)
                yield
                bA = nb6()
                P.mm(ps[:, bA, 0:130], lhsT=spl, rhs=tricat)
                bB = nb6()
                P.mm(ps[:, bB, 0:128], lhsT=umat, rhs=spl)
                P.act(Eq, ps[:, bA, 0:128], AF.Exp)
                P.act(Ek, ps[:, bA, 0:128], AF.Exp, scale=-1.0)
                P.act(F_["dec"], ps[:, bA, 128:130], AF.Exp)
                P.act(Ee, ps[:, bB, 0:128], AF.Exp)
                yield
                P.stt(F_["qdec32"], B_["gqT"][:, cols], 32.0 ** -0.5, Eq, ALU.mult, ALU.mult)
                yield
                P.copy("dve", F_["qdec"], F_["qdec32"])
                P.tt("dve", kinv, B_["gkT"][:, cols], Ek, ALU.mult)
                yield
                P.tt("dve", kend, B_["gktok"][:, t4, :], Ee, ALU.mult)
                yield
                kd = kend
                kd_b = bass.AP(kd.tensor, kd.offset, [list(kd.ap[0]), [0, 4], [1, 128]])
                P.tt("dve", F_["kbd"], kd_b, hmf, ALU.mult)
                yield
                qd = F_["qdec"]
                qd_b = bass.AP(qd.tensor, qd.offset, [list(qd.ap[0]), [0, 4], [1, 128]])
                P.tt("dve", qbd, qd_b, hm_b, ALU.mult)
                yield
                b3 = nb6()
                P.mm(ps[:, b3, :], lhsT=kinv, rhs=qbd.rearrange("p a b -> p (a b)"))
                P.tt("dve", F_["AT"].rearrange("p a b -> p (a b)"), ps[:, b3, :], glamask, ALU.mult)
                yield

            def back(t):
                B_ = BS[(t // TPB) % 2]
                F_ = FS[t % 2]
                t4 = t % TPB
                cols = slice(t4 * 128, (t4 + 1) * 128)
                AT, kbd, dec = F_["AT"], F_["kbd"], F_["dec"]
                b4 = [6, 7]
                for hp in range(2):
                    o_ap = ps[:, b4[hp], 0:128]
                    P.mm(o_ap, lhsT=B_["gvpad"][:, t4, 2 * hp, :], rhs=AT[:, 2 * hp, :], start=True, stop=False)
                    P.mm(o_ap, lhsT=B_["gvpad"][:, t4, 2 * hp + 1, :], rhs=AT[:, 2 * hp + 1, :], start=False, stop=False)
                    P.mm(ps[:, b4[hp], 0:64], lhsT=Sfp[:, hp * 128:(hp + 1) * 128], rhs=F_["qdec32"][:, 0:64],
                         start=False, stop=False)
                yield
                for ch in range(2):
                    rows = slice(ch * 64, ch * 64 + 64)
                    b5 = nb6()
                    for hh in range(4):
                        P.mm(ps[:, b5, hh * 64:(hh + 1) * 64], lhsT=kbd[rows, hh, :],
                             rhs=B_["gvp"][rows, t4, hh * 64:(hh + 1) * 64], start=(hh == 0), stop=(hh == 3))
                    P.stt(Sfp, Sfp, dec[:, ch:ch + 1], ps[:, b5, 0:256], ALU.mult, ALU.add)
                    yield
                    if ch == 0:
                        for hp in range(2):
                            P.mm(ps[:, b4[hp], 64:128], lhsT=Sfp[:, hp * 128:(hp + 1) * 128],
                                 rhs=F_["qdec32"][:, 64:128], start=False, stop=True)
                            P.copy("act", oT[:, hp, cols], ps[:, b4[hp], 0:128])
                        yield

            def norm_gate(tb):
                B_ = BS[tb % 2]
                sl = slice(tb * BW, (tb + 1) * BW)
                P.tt("pool", sqg, oT, oT, ALU.mult)
                yield
                for pt in range(2):
                    b = nb6()
                    P.mm(ps[:, b, 0:BW], lhsT=blk64, rhs=sqg[:, pt, :])
                    P.act(lng[:, pt, :], ps[:, b, 0:BW], AF.Ln, scale=1.0 / 64, bias=eps_t)
                    yield
                P.act(lng, lng, AF.Exp, scale=-0.5)
                yield
                for pt in range(2):
                    P.stt(lng[:, pt, :], oT[:, pt, :], pv[l][:, 19:20], lng[:, pt, :], ALU.mult, ALU.mult)
                    P.tt("dve", mixed[:, 4 + pt, sl], lng[:, pt, :], B_["srT"][:, pt, :], ALU.mult)
                    yield

            def chain(*gens):
                for g in gens:
                    yield from g

            def run(gens):
                gens = list(gens)
                while gens:
                    for g in list(gens):
                        try:
                            next(g)
                        except StopIteration:
                            gens.remove(g)

            run([chain(proj(0), front(0))])
            for t in range(NTL):
                tb = t // TPB
                s1 = [back(t)]
                if t % TPB == TPB - 1:
                    s1.append(norm_gate(tb))
                s2 = []
                if t + 1 < NTL:
                    if (t + 1) % TPB == 0:
                        s2.append(proj(tb + 1))
                    s2.append(front(t + 1))
                run([chain(*s1), chain(*s2)])

        def wout_phase(l):
            wo = w_out[l].rearrange("(k p) d -> p k d", p=128)
            P.dma("pool", wslot[3], wo[:, :, 512:1024], wgrp("ws3"))
            for dh in range(2):
                slot = wslot[2 + dh]
                for dc in range(4):
                    d = dh * 4 + dc
                    for tb in range(NB):
                        sl = slice(tb * 512, (tb + 1) * 512)
                        b = nextbank()
                        for k in range(8):
                            P.mm(ps[:, b, :], lhsT=slot[:, k, dc * 128:(dc + 1) * 128], rhs=mixed[:, k, sl],
                                 start=(k == 0), stop=(k == 7))
                        P.tt("dve", hT[:, d, sl], hT[:, d, sl], ps[:, b, :], ALU.add)

        def ffn_phase(l):
            wg = w_gate[l].rearrange("(k p) f -> p k f", p=128)
            wu = w_up[l].rearrange("(k p) f -> p k f", p=128)
            wd = w_down[l].rearrange("(c p) d -> p c d", p=128)
            ffT = V(96 * KB, [128, 11, S])
            gsl = [V(172 * KB, [128, 8, 512]), V(156 * KB, [128, 8, 512])]
            usl = [V(180 * KB, [128, 8, 512]), V(164 * KB, [128, 8, 512])]
            dsl = [V(140 * KB, [128, 11, 256]), V(140 * KB + 5632, [128, 11, 256])]
            sg = [V(152 * KB + i * 2 * KB, [128, 512], F32) for i in range(2)]
            groups = []
            for fh in range(2):
                f0 = fh * 1408
                for (o, w) in ((0, 512), (512, 512), (1024, 384)):
                    groups.append((fh, f0 + o, w))

            def load_group(gi):
                fh, fo, w = groups[gi]
                P.dma("pool", gsl[gi % 2][:, :, 0:w], wg[:, :, fo:fo + w], wgrp(f"fg{gi % 2}"))
                P.dma("pool", usl[gi % 2][:, :, 0:w], wu[:, :, fo:fo + w], wgrp(f"fu{gi % 2}"))

            def load_down(fh_, dp_):
                di = fh_ * 4 + dp_
                P.dma("pool", dsl[di % 2], wd[:, fh_ * 11:(fh_ + 1) * 11, dp_ * 256:(dp_ + 1) * 256], wgrp(f"fd{di % 2}"))

            load_group(0)
            load_group(1)
            rmsnorm(l, 8, 96 * KB)
            sgi = 0
            for gi, (fh, fo, w) in enumerate(groups):
                for fc in range(w // 128):
                    fidx = (fo - fh * 1408) // 128 + fc
                    fcs = slice(fc * 128, (fc + 1) * 128)
                    for tb in range(NB):
                        sl = slice(tb * 512, (tb + 1) * 512)
                        bg = nextbank()
                        for k in range(8):
                            P.mm(ps[:, bg, :], lhsT=gsl[gi % 2][:, k, fcs], rhs=hn[:, k, sl], start=(k == 0), stop=(k == 7))
                        bu = nextbank()
                        for k in range(8):
                            P.mm(ps[:, bu, :], lhsT=usl[gi % 2][:, k, fcs], rhs=hn[:, k, sl], start=(k == 0), stop=(k == 7))
                        s_ = sg[sgi % 2]
                        sgi += 1
                        P.act(s_, ps[:, bg, :], AF.Silu)
                        P.tt("dve", ffT[:, fidx, sl], s_, ps[:, bu, :], ALU.mult)
                if gi + 2 < len(groups):
                    load_group(gi + 2)
                if gi % 3 == 2:
                    for dp in range(4):
                        if dp + 1 < 4:
                            load_down(fh, dp + 1)
                        slot = dsl[(fh * 4 + dp) % 2]
                        for dc in range(2):
                            d = dp * 2 + dc
                            for tb in range(NB):
                                sl = slice(tb * 512, (tb + 1) * 512)
                                b = nextbank()
                                for c in range(11):
                                    P.mm(ps[:, b, :], lhsT=slot[:, c, dc * 128:(dc + 1) * 128], rhs=ffT[:, c, sl],
                                         start=(c == 0), stop=(c == 10))
                                P.tt("dve", hT[:, d, sl], hT[:, d, sl], ps[:, b, :], ALU.add)
                elif gi % 3 == 1:
                    load_down(fh, 0)

        import os as _os
        stages = _os.environ.get("KSTAGES", "setup,n,da,gla,wo,ffn").split(",")
        if "setup" in stages:
            setup()
        for s in range(NSEQ):
            load_x(s)
            for l in layers:
                if "n" in stages:
                    rmsnorm(l, 0, T0)
                if "da" in stages:
                    da_phase(l)
                if "gla" in stages:
                    gla_pool_phase(l)
                if "wo" in stages:
                    wout_phase(l)
                if "ffn" in stages:
                    ffn_phase(l)
            store_out(s)
        P.emit(nc, es)
    return nc


def _host_prep(inp):
    f = lambda k: np.ascontiguousarray(np.asarray(inp[k], dtype=np.float32))
    depth = DEPTH
    p = np.arange(128)
    pvec = np.zeros((depth, 128, NPV), np.float32)
    ang, fng = f("attn_norm_g"), f("ffn_norm_g")
    qg, kg, sg_, gg = f("q_norm_g"), f("k_norm_g"), f("da_subln_g"), f("gla_norm_g")
    psc, lv, rb = f("pool_scale"), f("lambda_vecs"), f("rel_bias")
    for l in range(depth):
        pvec[l, :, 0:8] = ang[l].reshape(8, 128).T
        pvec[l, :, 8:16] = fng[l].reshape(8, 128).T
        pvec[l, :, 16] = qg[l][p % 64]
        pvec[l, :, 17] = kg[l][p % 64]
        pvec[l, :, 18] = sg_[l]
        pvec[l, :, 19] = gg[l][p % 64]
        pvec[l, :, 20:22] = psc[l].reshape(2, 128).T
        pvec[l, :, 22:26] = rb[31][None, :]
    gw17 = np.concatenate([f("gla_gate_w"), f("gla_gate_b")[:, None, :]], axis=1)
    pw = f("pool_w")
    pwbd = np.zeros((depth, 128, 2, 128), np.float32)
    for l in range(depth):
        for g in range(4):
            pt, half = divmod(g, 2)
            pwbd[l, half * 64:(half + 1) * 64, pt, half * 64:(half + 1) * 64] = pw[l, g]
    idx = _bias_index()
    bt = rb[idx]
    biasT = np.ascontiguousarray(np.transpose(bt, (0, 3, 1, 2))).reshape(128, 4 * 2 * 128)
    return {
        "w_in": f("w_in"), "w_out": f("w_out"), "w_gate": f("w_gate"), "w_up": f("w_up"), "w_down": f("w_down"),
        "pvec": pvec, "gw17": np.ascontiguousarray(gw17), "pwbd": pwbd.reshape(depth, 128, 256),
        "biasT": biasT, "cstf": _consts()[0], "cstb": _consts()[1],
        "lvb": np.ascontiguousarray(np.broadcast_to(lv.reshape(depth, 1, 256), (depth, 128, 256))),
    }


_CACHE = {}


def kernel(**inputs):
    x = np.ascontiguousarray(np.asarray(inputs["x"], dtype=np.float32))
    B, S, _ = x.shape
    nseq = B // N_CORES
    shared = _host_prep(inputs)
    key = (S, nseq)
    if key not in _CACHE:
        _CACHE[key] = build_program(S, nseq, list(range(DEPTH)))
    nc = _CACHE[key]
    in_maps = []
    for c in range(N_CORES):
        m = dict(shared)
        m["x"] = np.ascontiguousarray(x[c * nseq:(c + 1) * nseq])
        in_maps.append(m)
    res = run_bass_kernel_spmd(nc, in_maps, core_ids=list(range(N_CORES)))
    return np.concatenate([np.asarray(r["out"]) for r in res.results], axis=0).astype(np.float32)
```

```python
import math
from contextlib import ExitStack

import numpy as np
import concourse.bass as bass
import concourse.mybir as mybir
from concourse.bass_utils import run_bass_kernel_spmd

F32 = mybir.dt.float32
BF16 = mybir.dt.bfloat16
AF = mybir.ActivationFunctionType
ALU = mybir.AluOpType
AX = mybir.AxisListType

D = 1024
DEPTH = 2
FFN = 2816
IN_TOTAL = 2576
N_CORES = 8
EPS = 1e-6
NEG = -30000.0
NPV = 26
ENGINES = ("pe", "act", "dve", "pool", "sp")
SAME_ENGINE_SYNC = True


def _esz(dt):
    s = str(dt)
    if "64" in s:
        return 8
    if "32" in s:
        return 4
    if "16" in s:
        return 2
    return 1


def region(ap):
    pat = ap.ap
    off = int(ap.offset)
    esz = _esz(ap.dtype)
    name = ap.tensor.name
    sp = str(ap.space).upper()
    if "DRAM" in sp or "HBM" in sp:
        lo = hi = off
        for s, c in pat:
            if s >= 0:
                hi += s * (c - 1)
            else:
                lo += s * (c - 1)
        return (name, 0, 1, lo * esz, (hi + 1) * esz)
    pstep, pcnt = pat[0]
    p0 = off // pstep
    f0 = off % pstep
    lo = hi = f0
    for s, c in pat[1:]:
        if s >= 0:
            hi += s * (c - 1)
        else:
            lo += s * (c - 1)
    if "PSUM" in sp:
        b0 = (lo * esz) // 2048 * 2048
        b1 = ((hi + 1) * esz + 2047) // 2048 * 2048
        return ("@" + name, 0, 128, b0, b1)
    return (name, p0, p0 + pcnt, lo * esz, (hi + 1) * esz)


class Op:
    __slots__ = ("eng", "fn", "deps", "needs_inc", "tick", "is_dma", "grp", "grp_val", "waits")

    def __init__(self, eng, fn):
        self.eng = eng
        self.fn = fn
        self.deps = set()
        self.needs_inc = False
        self.tick = None
        self.is_dma = False
        self.grp = None
        self.grp_val = None
        self.waits = None


class DmaGroup:
    def __init__(self, name):
        self.name = name
        self.sem = None
        self.count = 0
        self.final = False


class Prog:
    def __init__(self):
        self.ops = {e: [] for e in ENGINES}
        self.res = {}
        self.groups = []

    def group(self, name, final=False):
        g = DmaGroup(name)
        g.final = final
        self.groups.append(g)
        return g

    def _track(self, op, reads, writes):
        rregs = [region(a) for a in reads]
        wregs = [region(a) for a in writes]
        for (name, p0, p1, b0, b1) in rregs:
            psum = name[0] == "@"
            for e in self.res.setdefault(name, []):
                if (e[4] == "w" or (psum and e[5].eng != op.eng)) and e[0] < p1 and p0 < e[1] and e[2] < b1 and b0 < e[3]:
                    op.deps.add(e[5])
        for (name, p0, p1, b0, b1) in wregs:
            for e in self.res.setdefault(name, []):
                if e[0] < p1 and p0 < e[1] and e[2] < b1 and b0 < e[3]:
                    op.deps.add(e[5])
        op.deps.discard(op)
        for (name, p0, p1, b0, b1) in rregs:
            lst = self.res[name]
            if not op.is_dma:
                lst[:] = [e for e in lst if not (e[4] == "r" and e[5].eng == op.eng and not e[5].is_dma
                                                 and p0 <= e[0] and e[1] <= p1 and b0 <= e[2] and e[3] <= b1)]
            lst.append([p0, p1, b0, b1, "r", op])
        for (name, p0, p1, b0, b1) in wregs:
            lst = self.res[name]
            lst[:] = [e for e in lst if not (p0 <= e[0] and e[1] <= p1 and b0 <= e[2] and e[3] <= b1)]
            lst.append([p0, p1, b0, b1, "w", op])

    def op(self, eng, fn, reads=(), writes=()):
        o = Op(eng, fn)
        self._track(o, reads, writes)
        self.ops[eng].append(o)
        return o

    def dma(self, queue, out, in_, grp, **kw):
        o = Op(queue, lambda e: e.dma_start(out=out, in_=in_, **kw))
        o.is_dma = True
        o.grp = grp
        grp.count += 16
        o.grp_val = grp.count
        self._track(o, [in_], [out])
        self.ops[queue].append(o)
        return o

    def mm(self, out, lhsT, rhs, start=True, stop=True):
        return self.op("pe", lambda e: e.matmul(out, lhsT=lhsT, rhs=rhs, start=start, stop=stop),
                       [lhsT, rhs], [out])

    def transpose(self, out, in_, ident):
        return self.op("pe", lambda e: e.transpose(out, in_, ident), [in_, ident], [out])

    def act(self, out, in_, func, bias=None, scale=None):
        kw = {}
        reads = [in_]
        if bias is not None:
            kw["bias"] = bias
            if not isinstance(bias, (int, float)):
                reads.append(bias)
        if scale is not None:
            kw["scale"] = scale
            if not isinstance(scale, (int, float)):
                reads.append(scale)
        return self.op("act", lambda e: e.activation(out=out, in_=in_, func=func, **kw), reads, [out])

    def tt(self, eng, out, in0, in1, op):
        return self.op(eng, lambda e: e.tensor_tensor(out=out, in0=in0, in1=in1, op=op), [in0, in1], [out])

    def ts(self, eng, out, in0, s1, op0, s2=None, op1=None):
        reads = [in0]
        if not isinstance(s1, (int, float)):
            reads.append(s1)
        if s2 is not None and not isinstance(s2, (int, float)):
            reads.append(s2)
        if op1 is None:
            return self.op(eng, lambda e: e.tensor_scalar(out=out, in0=in0, scalar1=s1, scalar2=None, op0=op0),
                           reads, [out])
        return self.op(eng, lambda e: e.tensor_scalar(out=out, in0=in0, scalar1=s1, scalar2=s2, op0=op0, op1=op1),
                       reads, [out])

    def stt(self, out, in0, scalar, in1, op0, op1):
        reads = [in0, in1]
        if not isinstance(scalar, (int, float)):
            reads.append(scalar)
        return self.op("dve", lambda e: e.scalar_tensor_tensor(out=out, in0=in0, scalar=scalar, in1=in1,
                                                                 op0=op0, op1=op1), reads, [out])

    def copy(self, eng, out, in_):
        if eng == "act":
            return self.op(eng, lambda e: e.copy(out=out, in_=in_), [in_], [out])
        return self.op(eng, lambda e: e.tensor_copy(out=out, in_=in_), [in_], [out])

    def memset(self, eng, ap, val):
        return self.op(eng, lambda e: e.memset(ap, val), [], [ap])

    def recip(self, out, in_):
        return self.op("dve", lambda e: e.reciprocal(out=out, in_=in_), [in_], [out])

    def emit(self, nc, es):
        def skip(d, o):
            return d.eng == o.eng and not o.is_dma and (o.eng == "pe" or not SAME_ENGINE_SYNC)

        for e in ENGINES:
            for o in self.ops[e]:
                for d in o.deps:
                    if d.is_dma or skip(d, o):
                        continue
                    d.needs_inc = True
        esem = {e: es.enter_context(nc.semaphore("sem_" + e)) for e in ENGINES}
        for g in self.groups:
            g.sem = es.enter_context(nc.semaphore("dg_" + g.name))
        for e in ENGINES:
            t = 0
            for o in self.ops[e]:
                if o.needs_inc and not o.is_dma:
                    t += 1
                    o.tick = t
        for e in ENGINES:
            seen = {}
            for o in self.ops[e]:
                w = {}
                for d in o.deps:
                    if d.is_dma:
                        key = ("g", id(d.grp))
                        sem, val = d.grp.sem, d.grp_val
                    else:
                        if skip(d, o):
                            continue
                        key = ("e", d.eng)
                        sem, val = esem[d.eng], d.tick
                    if seen.get(key, 0) >= val:
                        continue
                    if key not in w or w[key][1] < val:
                        w[key] = (sem, val)
                for key, (sem, val) in w.items():
                    seen[key] = val
                o.waits = list(w.values())
        engobj = {"pe": "tensor", "act": "scalar", "dve": "vector", "pool": "gpsimd", "sp": "sync"}
        finals = [(g.sem, g.count) for g in self.groups if g.count > 0 and g.final]
        with nc.Block() as block:
            for e in ENGINES:
                def body(eng, ops=self.ops[e], e=e):
                    for o in ops:
                        for sem, val in o.waits:
                            eng.wait_ge(sem, val)
                        ins = o.fn(eng)
                        if o.is_dma:
                            ins.then_inc(o.grp.sem, 16)
                        elif o.needs_inc:
                            ins.then_inc(esem[e], 1)
                    if e == "sp":
                        for sem, val in finals:
                            eng.wait_ge(sem, val)

                getattr(block, engobj[e])(body)


def _t5_bucket_np(d):
    d = np.maximum(d, 0)
    max_exact = 16
    large = max_exact + (np.log(np.maximum(d, 1).astype(np.float32) / max_exact)
                         / math.log(128 / max_exact) * (32 - max_exact)).astype(np.int32)
    large = np.minimum(large, 31)
    return np.where(d < max_exact, d, large)


F_IDENT = 0
F_MASKNEG = 128
F_TRICAT = 256
F_U = 386
F_INVW = 514
F_INVC = 516
NCF = 548
B_GLAMASK = 0
B_BLK64 = 512
B_ONES = 640
B_BDMASK = 768
B_HM = 1024
B_HMF = 1028
NCB = 1540


def _consts():
    cf = np.zeros((128, NCF), np.float32)
    cb = np.zeros((128, NCB), np.float32)
    i = np.arange(128)
    cf[:, F_IDENT:F_IDENT + 128] = np.eye(128, dtype=np.float32)
    cf[:, F_MASKNEG:F_MASKNEG + 128] = np.where(i[:, None] > i[None, :], NEG, 0.0)
    same = (i[:, None] // 64) == (i[None, :] // 64)
    gm = (same & (i[:, None] <= i[None, :])).astype(np.float32)
    cb[:, B_GLAMASK:B_GLAMASK + 512] = np.tile(gm, (1, 4))
    cf[:, F_TRICAT:F_TRICAT + 128] = gm * (-1.0 / 16.0)
    cf[:, F_TRICAT + 128] = np.where(i < 64, -1.0 / 16.0, 0.0)
    cf[:, F_TRICAT + 129] = np.where(i >= 64, -1.0 / 16.0, 0.0)
    cf[:, F_U:F_U + 128] = (same & (i[:, None] > i[None, :])).astype(np.float32) * (-1.0 / 16.0)
    cb[:, B_BLK64:B_BLK64 + 128] = same.astype(np.float32)
    hrow = i // 32
    hcol = np.arange(256) // 64
    cb[:, B_BDMASK:B_BDMASK + 256] = (hrow[:, None] == hcol[None, :]).astype(np.float32)
    wins = (2, 4, 8, 16)
    for pt in range(2):
        for half in range(2):
            win = wins[pt * 2 + half]
            rows = slice(half * 64, half * 64 + 64)
            cf[rows, F_INVW + pt] = 1.0 / win
            t = np.arange(16)
            cf[rows, F_INVC + pt * 16:F_INVC + pt * 16 + 16] = 1.0 / np.minimum(t + 1, win)
    cb[:, B_ONES:B_ONES + 128] = 1.0
    cb[:, B_HM:B_HM + 4] = (hrow[:, None] == np.arange(4)[None, :]).astype(np.float32)
    hf = (np.arange(4)[:, None] == (np.arange(128) // 32)[None, :]).astype(np.float32).reshape(1, 512)
    cb[:, B_HMF:B_HMF + 512] = hf
    return cf, cb


def _bias_index():
    k = np.arange(128)[:, None]
    q = np.arange(128)[None, :]
    d0 = np.clip(q - k, 0, None)
    d1 = q - k + 128
    idx = np.stack([_t5_bucket_np(d0), _t5_bucket_np(d1)], axis=1)
    return idx


def build_program(S, NSEQ, layers):
    NT = S // 128
    NB = S // 512
    nc = bass.Bass("TRN2", target_bir_lowering=False)
    dt_in = lambda n, shp: nc.dram_tensor(n, shp, F32, kind="ExternalInput").ap()
    x = dt_in("x", [NSEQ, S, D])
    w_in = dt_in("w_in", [DEPTH, D, IN_TOTAL])
    w_out = dt_in("w_out", [DEPTH, D, D])
    w_gate = dt_in("w_gate", [DEPTH, D, FFN])
    w_up = dt_in("w_up", [DEPTH, D, FFN])
    w_down = dt_in("w_down", [DEPTH, FFN, D])
    pvec_d = dt_in("pvec", [DEPTH, 128, NPV])
    gw17_d = dt_in("gw17", [DEPTH, 17, 128])
    pwbd_d = dt_in("pwbd", [DEPTH, 128, 256])
    biasT_d = dt_in("biasT", [128, 4 * 2 * 128])
    cstf_d = dt_in("cstf", [128, NCF])
    cstb_d = dt_in("cstb", [128, NCB])
    lvb_d = dt_in("lvb", [DEPTH, 128, 256])
    out = nc.dram_tensor("out", [NSEQ, S, D], F32, kind="ExternalOutput").ap()

    P = Prog()
    KB = 1024
    ARENA = 196 * KB
    with ExitStack() as es:
        arena = es.enter_context(nc.sbuf_tensor("arena", [128, ARENA // 2], BF16))
        cst = es.enter_context(nc.sbuf_tensor("cstf_sb", [128, NCF], F32))
        cstb = es.enter_context(nc.sbuf_tensor("cstb_sb", [128, NCB], BF16))
        biasT = es.enter_context(nc.sbuf_tensor("biasT_sb", [128, 4, 2, 128], F32))
        pv = [es.enter_context(nc.sbuf_tensor(f"pv{l}", [128, NPV], F32)) for l in range(DEPTH)]
        pv2 = [es.enter_context(nc.sbuf_tensor(f"pvb{l}", [128, 8], F32)) for l in range(DEPTH)]
        gw17 = [es.enter_context(nc.sbuf_tensor(f"gw{l}", [17, 128], BF16)) for l in range(DEPTH)]
        pwbd = [es.enter_context(nc.sbuf_tensor(f"pw{l}", [128, 2, 128], BF16)) for l in range(DEPTH)]
        cvec = es.enter_context(nc.sbuf_tensor("cvec", [128, 4], F32))
        ps = es.enter_context(nc.psum_tensor("ps", [128, 8, 512], F32))

        def V(off, shape, dt=BF16, p0=0):
            n = 1
            for s in shape[1:]:
                n *= s
            esz = _esz(dt)
            a = arena[p0:p0 + shape[0], off // 2: off // 2 + (n * esz) // 2]
            if dt != BF16:
                a = a.bitcast(dt)
            if len(shape) == 3:
                a = a.rearrange("p (a b) -> p a b", a=shape[1])
            elif len(shape) == 4:
                a = a.rearrange("p (a b c) -> p a b c", a=shape[1], b=shape[2])
            return a

        hT = V(0, [128, 8, S], F32)
        hn = V(64 * KB, [128, 8, S])
        mixed = V(96 * KB, [128, 8, S])
        wslot = [V(128 * KB + 8 * KB * i, [128, 8, 512]) for i in range(4)]
        T0 = 160 * KB
        ident = cst[:, F_IDENT:F_IDENT + 128]
        onesb = cstb[:, B_ONES:B_ONES + 128]
        blk64 = cstb[:, B_BLK64:B_BLK64 + 128]
        glamask = cstb[:, B_GLAMASK:B_GLAMASK + 512]
        tricat = cst[:, F_TRICAT:F_TRICAT + 130]
        umat = cst[:, F_U:F_U + 128]
        bdmask = cstb[:, B_BDMASK:B_BDMASK + 256]
        eps_t = cvec[:, 0:1]
        one_t = cvec[:, 1:2]

        bank_ctr = [0]

        def nextbank(lo=0, n=8):
            b = lo + bank_ctr[0] % n
            bank_ctr[0] += 1
            return b

        gsetup = P.group("setup")
        gx = [P.group(f"x{i}") for i in range(4)]
        go = [P.group(f"o{i}", final=True) for i in range(4)]
        gW = {}

        def wgrp(name):
            if name not in gW:
                gW[name] = P.group(name)
            return gW[name]

        def setup():
            P.dma("sp", cst[:], cstf_d, P.group("c_cstf"))
            P.dma("pool", cstb[:], cstb_d, P.group("c_cstb"))
            P.dma("sp", biasT[:].rearrange("p a b c -> p (a b c)"), biasT_d, P.group("c_bias"))
            for l in layers:
                P.dma("sp", pv[l][:], pvec_d[l], P.group(f"c_pv{l}"))
                P.dma("pool", gw17[l][:], gw17_d[l], P.group(f"c_gw{l}"))
                P.dma("pool", pwbd[l][:].rearrange("p a b -> p (a b)"), pwbd_d[l], P.group(f"c_pw{l}"))
            P.memset("dve", cvec[:, 0:1], EPS)
            P.memset("dve", cvec[:, 1:2], 1.0)
            l0 = layers[0]
            for h in range(4):
                P.ts("dve", biasT[:, h, :, :], biasT[:, h, :, :], pv[l0][:, 22 + h:23 + h], ALU.subtract)
                P.tt("dve", biasT[:, h, 0, :], biasT[:, h, 0, :], cst[:, F_MASKNEG:F_MASKNEG + 128], ALU.add)
            P.act(biasT[:].rearrange("p a b c -> p (a b c)"), biasT[:].rearrange("p a b c -> p (a b c)"), AF.Exp)
            for l in layers:
                lam_init = 0.8 - 0.6 * math.exp(-0.3 * l)
                lvt = V(T0 + l * 2 * KB, [128, 256], F32)
                lamt = V(T0 + l * 2 * KB + KB, [128, 2, 64], F32)
                P.dma("sp", lvt, lvb_d[l], P.group(f"c_lv{l}"))
                lvb = lvt.rearrange("p (a b) -> p a b", a=4)
                P.tt("dve", lamt, lvb[:, 0::2, :], lvb[:, 1::2, :], ALU.mult)
                P.op("dve", lambda e, l=l, lamt=lamt: e.tensor_reduce(out=pv2[l][:, 4:6], in_=lamt, axis=AX.X, op=ALU.add),
                     [lamt], [pv2[l][:, 4:6]])
                P.act(pv2[l][:, 4:6], pv2[l][:, 4:6], AF.Exp)
                P.tt("dve", pv2[l][:, 6:7], pv2[l][:, 4:5], pv2[l][:, 5:6], ALU.subtract)
                P.ts("dve", pv2[l][:, 1:2], pv2[l][:, 6:7], lam_init, ALU.add, -1.0, ALU.mult)
                P.ts("dve", pv2[l][:, 0:1], pv[l][:, 16:17], 0.125, ALU.mult)
                P.ts("dve", pv2[l][:, 2:3], pv[l][:, 18:19], 1.0 - lam_init, ALU.mult)

        def load_x(s):
            for t in range(NT):
                xs = V(64 * KB + (t % 4) * 4 * KB, [128, D], F32)
                P.dma("sp", xs, x[s, t * 128:(t + 1) * 128, :], gx[t % 4])
                for half in range(2):
                    b = nextbank()
                    for kk in range(4):
                        k = half * 4 + kk
                        P.transpose(ps[:, b, kk * 128:(kk + 1) * 128], xs[:, k * 128:(k + 1) * 128], ident)
                    P.copy("dve" if half == 0 else "act", hT[:, half * 4:(half + 1) * 4, t * 128:(t + 1) * 128],
                           ps[:, b, :].rearrange("p (a b) -> p a b", a=4))

        def store_out(s):
            for t in range(NT):
                ys = V(64 * KB + (t % 4) * 4 * KB, [128, D], F32)
                for half in range(2):
                    b = nextbank()
                    for kk in range(4):
                        k = half * 4 + kk
                        P.transpose(ps[:, b, kk * 128:(kk + 1) * 128], hT[:, k, t * 128:(t + 1) * 128], ident)
                    P.copy("dve" if half == 0 else "act", ys[:, half * 512:(half + 1) * 512], ps[:, b, :])
                P.dma("sp", out[s, t * 128:(t + 1) * 128, :], ys, go[t % 4])

        def rmsnorm(l, gbase, toff):
            sqv = V(toff, [128, 8, 512])
            lnv2 = [V(toff + 8 * KB + i * 2 * KB, [128, 512], F32) for i in range(2)]
            banks = {}

            def stage_a(tb):
                sl = slice(tb * 512, (tb + 1) * 512)
                P.act(sqv, hT[:, :, sl], AF.Square)
                b = nextbank()
                banks[tb] = b
                for k in range(8):
                    P.mm(ps[:, b, :], lhsT=onesb, rhs=sqv[:, k, :], start=(k == 0), stop=(k == 7))

            def stage_b(tb):
                sl = slice(tb * 512, (tb + 1) * 512)
                lnv = lnv2[tb % 2]
                P.act(lnv, ps[:, banks[tb], :], AF.Ln, scale=1.0 / D, bias=eps_t)
                P.act(lnv, lnv, AF.Exp, scale=-0.5)
                for k in range(8):
                    P.stt(hn[:, k, sl], hT[:, k, sl], pv[l][:, gbase + k:gbase + k + 1], lnv, ALU.mult, ALU.mult)

            stage_a(0)
            for tb in range(NB):
                if tb + 1 < NB:
                    stage_a(tb + 1)
                stage_b(tb)

        def win_view(l):
            return w_in[l].rearrange("(k p) e -> p k e", p=128)

        def da_phase(l):
            wv = win_view(l)
            P.dma("pool", wslot[0], wv[:, :, 0:512], wgrp("ws0"))
            P.dma("pool", wslot[1], wv[:, :, 512:1024], wgrp("ws1"))
            P.dma("pool", wslot[2], wv[:, :, 1024:1536], wgrp("ws2"))
            P.dma("pool", wslot[3], wv[:, :, 1536:2048], wgrp("ws3"))
            qn = V(T0, [128, S])
            kn = V(T0 + 4 * KB, [128, S])
            vst = V(T0 + 8 * KB, [128, NT, 128])
            pt4 = [V(T0 + 12 * KB + i * KB, [128, 2, 256]) for i in range(4)]
            TT = T0 + 16 * KB
            raw = [V(TT + i * 2 * KB, [128, 512], F32) for i in range(2)]
            sqb = [V(TT + 4 * KB + i * KB, [128, 512]) for i in range(2)]
            lnv = [V(TT + 6 * KB + i * 2 * KB, [128, 512], F32) for i in range(2)]
            FT = TT + 10 * KB
            lc = V(FT, [128, 512], F32)
            fin = []
            for base in (FT + 2 * KB, TT):
                fin.append(dict(t0=V(base, [128, 256], F32), t1=V(base + KB, [128, 256], F32),
                                cc=V(base + 2 * KB, [128, 256], F32), sq2=V(base + 3 * KB, [128, 256]),
                                lnf=V(base + 3 * KB + 512, [128, 256], F32)))
            qpad = [V(FT + 7 * KB + i * KB, [128, 2, 256]) for i in range(2)]
            P.memset("pool", qpad[0], 0.0)
            P.memset("pool", qpad[1], 0.0)
            pending = []
            for h in range(4):
                hc = slice(h * 128, (h + 1) * 128)
                while pending:
                    pending.pop(0)[1]()
                jobs = []
                for (wi, dst, gcol) in ((0, qn, pv2[l][:, 0:1]), (1, kn, pv[l][:, 17:18])):
                    for tb in range(NB):
                        sl = slice(tb * 512, (tb + 1) * 512)
                        b = nextbank()
                        for k in range(8):
                            P.mm(ps[:, b, :], lhsT=wslot[wi][:, k, hc], rhs=hn[:, k, sl], start=(k == 0), stop=(k == 7))
                        jobs.append((b, dst, gcol, sl))
                cb = {}

                def ch_a(ji):
                    b, dst, gcol, sl = jobs[ji]
                    P.copy("dve", raw[ji % 2], ps[:, b, :])
                    P.tt("pool", sqb[ji % 2], raw[ji % 2], raw[ji % 2], ALU.mult)

                def ch_b(ji):
                    b2 = jobs[ji][0]
                    P.mm(ps[:, b2, :], lhsT=blk64, rhs=sqb[ji % 2])
                    P.act(lnv[ji % 2], ps[:, b2, :], AF.Ln, scale=1.0 / 64, bias=eps_t)
                    P.act(lnv[ji % 2], lnv[ji % 2], AF.Exp, scale=-0.5)

                def ch_c(ji):
                    b, dst, gcol, sl = jobs[ji]
                    P.stt(dst[:, sl], raw[ji % 2], gcol, lnv[ji % 2], ALU.mult, ALU.mult)

                def vgroup(tg, b):
                    for t4 in range(4):
                        t = tg * 4 + t4
                        for k in range(8):
                            P.mm(ps[:, b, t4 * 128:(t4 + 1) * 128], lhsT=hn[:, k, t * 128:(t + 1) * 128],
                                 rhs=wslot[2][:, k, hc], start=(k == 0), stop=(k == 7))
                    P.copy("act", vst[:, tg * 4:(tg + 1) * 4, :], ps[:, b, :].rearrange("p (a b) -> p a b", a=4))

                nj = len(jobs)
                nvg = NT // 4
                vdone = 0
                ch_a(0)
                if nj > 1:
                    ch_a(1)
                ch_b(0)
                for ji in range(nj):
                    if ji + 1 < nj:
                        ch_b(ji + 1)
                    ch_c(ji)
                    if ji + 2 < nj:
                        ch_a(ji + 2)
                    if vdone < nvg:
                        vgroup(vdone, jobs[ji][0])
                        vdone += 1
                while vdone < nvg:
                    vgroup(vdone, nextbank())
                    vdone += 1
                if h == 3:
                    P.dma("pool", wslot[0][:, :, 0:272], wv[:, :, 2048:2320], wgrp("ws0"))
                    P.dma("pool", wslot[1][:, :, 0:256], wv[:, :, 2320:2576], wgrp("ws1"))
                NC2 = S // 256
                steps = [(c, j) for c in range(NC2) for j in range(2 * c + 2)]
                LA = 3
                cf = pv[l][:, 22 + h:23 + h]
                qp_done = set()

                def geom(c, j):
                    q0 = max(j, 2 * c) * 128
                    q1 = (2 * c + 2) * 128
                    return q0, q1, q1 - q0, q0 - 2 * c * 128

                def scores(i):
                    c, j = steps[i]
                    q0, q1, n, off = geom(c, j)
                    sb = 4 + (i % 4)
                    if c not in qp_done:
                        qp_done.add(c)
                        P.copy("pool", qpad[c % 2][0:64, 0, :], qn[0:64, c * 256:(c + 1) * 256])
                        P.copy("pool", qpad[c % 2][64:128, 1, :], qn[64:128, c * 256:(c + 1) * 256])
                    for m in range(2):
                        P.mm(ps[:, sb, m * 256:m * 256 + n], lhsT=kn[:, j * 128:(j + 1) * 128],
                             rhs=qpad[c % 2][:, m, off:256])
                    sc3 = ps[:, sb, :].rearrange("p (m q) -> p m q", m=2)
                    P.act(pt4[i % 4][:, :, 0:n], sc3[:, :, 0:n], AF.Exp, bias=cf)
                    pt_ = pt4[i % 4]
                    if j >= 2 * c:
                        bt = biasT[:, h, 0, :]
                        btb = bass.AP(bt.tensor, bt.offset, [list(bt.ap[0]), [0, 2], [1, 128]])
                        P.tt("dve", pt_[:, :, 0:128], pt_[:, :, 0:128], btb, ALU.mult)
                    if 2 * c <= j + 1 <= 2 * c + 1:
                        o = (j + 1) * 128 - q0
                        bt = biasT[:, h, 1, :]
                        btb = bass.AP(bt.tensor, bt.offset, [list(bt.ap[0]), [0, 2], [1, 128]])
                        P.tt("dve", pt_[:, :, o:o + 128], pt_[:, :, o:o + 128], btb, ALU.mult)

                def pvs(i):
                    c, j = steps[i]
                    nk = 2 * c + 2
                    q0, q1, n, off = geom(c, j)
                    bO = 2 * (c % 2)
                    bL = bO + 1
                    for m in range(2):
                        pt = pt4[i % 4][:, m, 0:n]
                        st = (j == 0 and m == 0)
                        P.op("pe", lambda e, o_=ps[:, bO, m * 256 + off:(m + 1) * 256], pt=pt, st=st, j=j:
                             e.matmul(o_, lhsT=vst[:, j, :], rhs=pt, start=st, stop=(j == nk - 1), skip_group_check=True),
                             [vst[:, j, :], pt], [ps[:, bO, m * 256 + off:(m + 1) * 256]])
                        P.op("pe", lambda e, o_=ps[:, bL, m * 256 + off:(m + 1) * 256], pt=pt, st=st:
                             e.matmul(o_, lhsT=onesb, rhs=pt, start=st, stop=(j == nk - 1), skip_group_check=True),
                             [onesb, pt], [ps[:, bL, m * 256 + off:(m + 1) * 256]])
                    if j == nk - 1:
                        finalize(c)

                def finalize(c):
                    sl = slice(c * 256, (c + 1) * 256)
                    bO = 2 * (c % 2)
                    bL = bO + 1
                    f = fin[c % 2]
                    t0, t1, cc, sq2, lnf = f["t0"], f["t1"], f["cc"], f["sq2"], f["lnf"]
                    P.copy("dve", lc, ps[:, bL, :])
                    P.tt("dve", t0, ps[:, bO, 0:256], lc[:, 256:512], ALU.mult)
                    P.tt("dve", t1, ps[:, bO, 256:512], lc[:, 0:256], ALU.mult)
                    P.tt("pool", cc, lc[:, 0:256], lc[:, 256:512], ALU.mult)
                    P.stt(t0, t1, pv2[l][:, 1:2], t0, ALU.mult, ALU.add)
                    P.tt("pool", sq2, t0, t0, ALU.mult)
                    P.tt("pool", cc, cc, cc, ALU.mult)

                    def tail(h=h, sl=sl, bL=bL, t0=t0, cc=cc, sq2=sq2, lnf=lnf):
                        P.mm(ps[:, bL, 0:256], lhsT=onesb, rhs=sq2)
                        P.stt(lnf, cc, EPS * 128.0, ps[:, bL, 0:256], ALU.mult, ALU.add)
                        P.act(lnf, lnf, AF.Ln, scale=1.0 / 128)
                        P.act(lnf, lnf, AF.Exp, scale=-0.5)
                        P.stt(mixed[:, h, sl], t0, pv2[l][:, 2:3], lnf, ALU.mult, ALU.mult)
                    pending.append([min(6, 2 * c + 3), tail])

                for i in range(min(LA, len(steps))):
                    scores(i)
                for i in range(len(steps)):
                    if i + LA < len(steps):
                        scores(i + LA)
                    pvs(i)
                    for pnd in list(pending):
                        pnd[0] -= 1
                        if pnd[0] <= 0:
                            pending.remove(pnd)
                            pnd[1]()
            while pending:
                pending.pop(0)[1]()

        def gla_pool_phase(l):
            wv = win_view(l)
            wA, wB, wC = wslot[3], wslot[0], wslot[1]
            BW = 256
            NBG = S // BW
            TPB = BW // 128
            NTL = NBG * TPB
            UW = BW + 16
            G0 = T0

            def blkset(bs):
                o = G0 + bs * 6 * KB
                return dict(gqT=V(o, [128, BW]), gkT=V(o + 512, [128, BW]), gktok=V(o + 1024, [128, TPB, 128]),
                            gvp=V(o + 1536, [128, TPB, 256]), gvpad=V(o + 2560, [128, TPB, 4, 128]),
                            lr17=V(o + 4608, [17, BW]), srT=V(o + 5120, [128, 2, BW]))
            BS = [blkset(0), blkset(1)]
            F0 = G0 + 12 * KB

            def feset(ts):
                o = F0 + ts * 3 * KB
                return dict(qdec=V(o, [128, 128]), qdec32=V(o + 256, [128, 128], F32), kbd=V(o + 768, [128, 4, 128]),
                            dec=V(o + 1792, [128, 2], F32), AT=V(o + 1824, [128, 4, 128]))
            FS = [feset(0), feset(1)]
            S0 = F0 + 6 * KB
            e1 = V(S0, [128, 128], F32)
            spl = V(S0 + 512, [128, 128], F32)
            Eq = V(S0 + 1024, [128, 128], F32)
            Ek = V(S0 + 1536, [128, 128], F32)
            Ee = V(S0 + 2048, [128, 128], F32)
            kinv = V(S0 + 2560, [128, 128])
            kend = V(S0 + 2816, [128, 128])
            qbd = V(S0 + 3072, [128, 4, 128])
            Sfp = V(S0 + 4096, [128, 256], F32)
            U0 = S0 + 5 * KB
            ubuf = V(U0, [128, 2, UW], F32)
            Y1 = U0 + 2304
            Abuf = V(Y1, [128, 2, UW], F32)
            Bbuf = V(Y1 + 2176, [128, 2, UW], F32)
            pooled = V(Y1 + 4352, [128, 2, BW])
            tmp16 = V(Y1 + 5376, [128, 16], F32)
            Y2 = Y1 + 5632
            oT = V(Y2, [128, 2, BW], F32)
            sqg = V(Y2 + 2048, [128, 2, BW])
            lng = V(Y2 + 3072, [128, 2, BW], F32)
            assert Y2 + 5120 <= ARENA, (Y2 + 5120, ARENA)

            wo = w_out[l].rearrange("(k p) d -> p k d", p=128)
            P.dma("pool", wslot[2], wo[:, :, 0:512], wgrp("ws2"))

            P.memset("pool", BS[0]["gvpad"], 0.0)
            P.memset("pool", BS[1]["gvpad"], 0.0)
            P.memset("dve", Sfp, 0.0)
            P.memset("pool", ubuf[:, :, 0:16], 0.0)
            hm2 = cstb[:, B_HM:B_HM + 4]
            hm_b = bass.AP(hm2.tensor, hm2.offset, [list(hm2.ap[0]), [1, 4], [0, 128]])
            hmf = cstb[:, B_HMF:B_HMF + 512].rearrange("p (a b) -> p a b", a=4)

            def nb6():
                return nextbank(0, 6)

            def proj(tb):
                B_ = BS[tb % 2]
                sl = slice(tb * BW, (tb + 1) * BW)
                for (dst, c0) in ((B_["gqT"], 0), (B_["gkT"], 128)):
                    b = nb6()
                    for k in range(8):
                        P.mm(ps[:, b, 0:BW], lhsT=wA[:, k, c0:c0 + 128], rhs=hn[:, k, sl], start=(k == 0), stop=(k == 7))
                    P.copy("act", dst, ps[:, b, 0:BW])
                    yield
                b = nb6()
                for k in range(8):
                    P.mm(ps[0:16, b, 0:BW], lhsT=wB[:, k, 0:16], rhs=hn[:, k, sl], start=(k == 0), stop=(k == 7))
                P.memset("pool", B_["lr17"], 1.0)
                P.copy("dve", B_["lr17"][0:16, :], ps[0:16, b, 0:BW])
                yield
                for pt in range(2):
                    b = nb6()
                    for k in range(8):
                        P.mm(ps[:, b, 0:BW], lhsT=wB[:, k, 16 + pt * 128:16 + (pt + 1) * 128], rhs=hn[:, k, sl],
                             start=(k == 0), stop=(k == 7))
                    P.act(B_["srT"][:, pt, :], ps[:, b, 0:BW], AF.Silu)
                    yield
                for t4 in range(TPB):
                    t = tb * TPB + t4
                    b = nb6()
                    for k in range(8):
                        P.mm(ps[:, b, 0:384], lhsT=hn[:, k, t * 128:(t + 1) * 128], rhs=wA[:, k, 128:512],
                             start=(k == 0), stop=(k == 7))
                    P.copy("act", B_["gktok"][:, t4, :], ps[:, b, 0:128])
                    P.copy("dve", B_["gvp"][:, t4, :], ps[:, b, 128:384])
                    src = ps[:, b, 128:384].rearrange("p (h v) -> p h v", h=4)
                    P.copy("act", B_["gvpad"][:, t4, 0::2, 0:64], src[:, 0::2, :])
                    P.copy("dve", B_["gvpad"][:, t4, 1::2, 64:128], src[:, 1::2, :])
                    yield
                for pt in range(2):
                    b = nb6()
                    for k in range(8):
                        P.mm(ps[:, b, 0:BW], lhsT=wC[:, k, pt * 128:(pt + 1) * 128], rhs=hn[:, k, sl],
                             start=(k == 0), stop=(k == 7))
                    P.copy("act", ubuf[:, pt, 16:UW], ps[:, b, 0:BW])
                    yield
                P.tt("pool", Abuf[:, :, 1:UW], ubuf[:, :, 1:UW], ubuf[:, :, 0:UW - 1], ALU.add)
                P.tt("pool", Bbuf[:, :, 3:UW], Abuf[:, :, 3:UW], Abuf[:, :, 1:UW - 2], ALU.add)
                P.tt("pool", Abuf[:, 1, 7:UW], Bbuf[:, 1, 7:UW], Bbuf[:, 1, 3:UW - 4], ALU.add)
                P.tt("pool", Bbuf[:, 1, 15:UW], Abuf[:, 1, 15:UW], Abuf[:, 1, 7:UW - 8], ALU.add)
                yield
                for pt in range(2):
                    for half in range(2):
                        rows = slice(half * 64, half * 64 + 64)
                        src = (Abuf if half == 0 else Bbuf)
                        P.stt(pooled[rows, pt, :], src[rows, pt, 16:UW], cst[rows, F_INVW + pt:F_INVW + pt + 1],
                              ubuf[rows, pt, 16:UW], ALU.mult, ALU.subtract)
                        if tb == 0:
                            P.tt("dve", tmp16[rows, :], src[rows, pt, 16:32],
                                 cst[rows, F_INVC + pt * 16:F_INVC + pt * 16 + 16], ALU.mult)
                            P.tt("dve", pooled[rows, pt, 0:16], tmp16[rows, :], ubuf[rows, pt, 16:32], ALU.subtract)
                        yield
                for pt in range(2):
                    b = nb6()
                    P.mm(ps[:, b, 0:BW], lhsT=pwbd[l][:, pt, :], rhs=pooled[:, pt, :])
                    P.ts("dve", mixed[:, 6 + pt, sl], ps[:, b, 0:BW], pv[l][:, 20 + pt:21 + pt], ALU.mult)
                    yield
                P.copy("pool", ubuf[:, :, 0:16], ubuf[:, :, BW:UW])
                yield

            def front(t):
                B_ = BS[(t // TPB) % 2]
                F_ = FS[t % 2]
                t4 = t % TPB
                cols = slice(t4 * 128, (t4 + 1) * 128)
                b = nb6()
                P.mm(ps[:, b, 0:128], lhsT=B_["lr17"][0:17, cols], rhs=gw17[l][:])
                P.act(e1, ps[:, b, 0:128], AF.Exp, scale=-1.0)
                yield
                P.act(spl, e1, AF.Ln, bias=one_t)
                yield
                bA = nb6()
                P.mm(ps[:, bA, 0:130], lhsT=spl, rhs=tricat)
                bB = nb6()
                P.mm(ps[:, bB, 0:128], lhsT=umat, rhs=spl)
                P.act(Eq, ps[:, bA, 0:128], AF.Exp)
                P.act(Ek, ps[:, bA, 0:128], AF.Exp, scale=-1.0)
                P.act(F_["dec"], ps[:, bA, 128:130], AF.Exp)
                P.act(Ee, ps[:, bB, 0:128], AF.Exp)
                yield
                P.stt(F_["qdec32"], B_["gqT"][:, cols], 32.0 ** -0.5, Eq, ALU.mult, ALU.mult)
                yield
                P.copy("dve", F_["qdec"], F_["qdec32"])
                P.tt("dve", kinv, B_["gkT"][:, cols], Ek, ALU.mult)
                yield
                P.tt("dve", kend, B_["gktok"][:, t4, :], Ee, ALU.mult)
                yield
                kd = kend
                kd_b = bass.AP(kd.tensor, kd.offset, [list(kd.ap[0]), [0, 4], [1, 128]])
                P.tt("dve", F_["kbd"], kd_b, hmf, ALU.mult)
                yield
                qd = F_["qdec"]
                qd_b = bass.AP(qd.tensor, qd.offset, [list(qd.ap[0]), [0, 4], [1, 128]])
                P.tt("dve", qbd, qd_b, hm_b, ALU.mult)
                yield
                b3 = nb6()
                P.mm(ps[:, b3, :], lhsT=kinv, rhs=qbd.rearrange("p a b -> p (a b)"))
                P.tt("dve", F_["AT"].rearrange("p a b -> p (a b)"), ps[:, b3, :], glamask, ALU.mult)
                yield

            def back(t):
                B_ = BS[(t // TPB) % 2]
                F_ = FS[t % 2]
                t4 = t % TPB
                cols = slice(t4 * 128, (t4 + 1) * 128)
                AT, kbd, dec = F_["AT"], F_["kbd"], F_["dec"]
                b4 = [6, 7]
                for hp in range(2):
                    o_ap = ps[:, b4[hp], 0:128]
                    P.mm(o_ap, lhsT=B_["gvpad"][:, t4, 2 * hp, :], rhs=AT[:, 2 * hp, :], start=True, stop=False)
                    P.mm(o_ap, lhsT=B_["gvpad"][:, t4, 2 * hp + 1, :], rhs=AT[:, 2 * hp + 1, :], start=False, stop=False)
                    P.mm(ps[:, b4[hp], 0:64], lhsT=Sfp[:, hp * 128:(hp + 1) * 128], rhs=F_["qdec32"][:, 0:64],
                         start=False, stop=False)
                yield
                for ch in range(2):
                    rows = slice(ch * 64, ch * 64 + 64)
                    b5 = nb6()
                    for hh in range(4):
                        P.mm(ps[:, b5, hh * 64:(hh + 1) * 64], lhsT=kbd[rows, hh, :],
                             rhs=B_["gvp"][rows, t4, hh * 64:(hh + 1) * 64], start=(hh == 0), stop=(hh == 3))
                    P.stt(Sfp, Sfp, dec[:, ch:ch + 1], ps[:, b5, 0:256], ALU.mult, ALU.add)
                    yield
                    if ch == 0:
                        for hp in range(2):
                            P.mm(ps[:, b4[hp], 64:128], lhsT=Sfp[:, hp * 128:(hp + 1) * 128],
                                 rhs=F_["qdec32"][:, 64:128], start=False, stop=True)
                            P.copy("act", oT[:, hp, cols], ps[:, b4[hp], 0:128])
                        yield

            def norm_gate(tb):
                B_ = BS[tb % 2]
                sl = slice(tb * BW, (tb + 1) * BW)
                P.tt("pool", sqg, oT, oT, ALU.mult)
                yield
                for pt in range(2):
                    b = nb6()
                    P.mm(ps[:, b, 0:BW], lhsT=blk64, rhs=sqg[:, pt, :])
                    P.act(lng[:, pt, :], ps[:, b, 0:BW], AF.Ln, scale=1.0 / 64, bias=eps_t)
                    yield
                P.act(lng, lng, AF.Exp, scale=-0.5)
                yield
                for pt in range(2):
                    P.stt(lng[:, pt, :], oT[:, pt, :], pv[l][:, 19:20], lng[:, pt, :], ALU.mult, ALU.mult)
                    P.tt("dve", mixed[:, 4 + pt, sl], lng[:, pt, :], B_["srT"][:, pt, :], ALU.mult)
                    yield

            def chain(*gens):
                for g in gens:
                    yield from g

            def run(gens):
                gens = list(gens)
                while gens:
                    for g in list(gens):
                        try:
                            next(g)
                        except StopIteration:
                            gens.remove(g)

            run([chain(proj(0), front(0))])
            for t in range(NTL):
                tb = t // TPB
                s1 = [back(t)]
                if t % TPB == TPB - 1:
                    s1.append(norm_gate(tb))
                streams = [chain(*s1)]
                if t + 1 < NTL:
                    if TPB == 1 and (t + 1) % TPB == 0:
                        streams.append(chain(proj(tb + 1), front(t + 1)))
                    else:
                        streams.append(front(t + 1))
                if TPB > 1 and t % TPB == TPB - 2 and tb + 1 < NBG:
                    streams.append(proj(tb + 1))
                run(streams)

        def wout_phase(l):
            wo = w_out[l].rearrange("(k p) d -> p k d", p=128)
            P.dma("pool", wslot[3], wo[:, :, 512:1024], wgrp("ws3"))
            for dh in range(2):
                slot = wslot[2 + dh]
                for dc in range(4):
                    d = dh * 4 + dc
                    for tb in range(NB):
                        sl = slice(tb * 512, (tb + 1) * 512)
                        b = nextbank()
                        for k in range(8):
                            P.mm(ps[:, b, :], lhsT=slot[:, k, dc * 128:(dc + 1) * 128], rhs=mixed[:, k, sl],
                                 start=(k == 0), stop=(k == 7))
                        P.tt("dve", hT[:, d, sl], hT[:, d, sl], ps[:, b, :], ALU.add)

        def ffn_phase(l):
            wg = w_gate[l].rearrange("(k p) f -> p k f", p=128)
            wu = w_up[l].rearrange("(k p) f -> p k f", p=128)
            wd = w_down[l].rearrange("(c p) d -> p c d", p=128)
            ffT = V(96 * KB, [128, 11, S])
            gsl = [V(172 * KB, [128, 8, 512]), V(156 * KB, [128, 8, 512])]
            usl = [V(180 * KB, [128, 8, 512]), V(164 * KB, [128, 8, 512])]
            dsl = [V(140 * KB, [128, 11, 256]), V(140 * KB + 5632, [128, 11, 256])]
            sg = [V(152 * KB + i * 2 * KB, [128, 512], F32) for i in range(2)]
            groups = []
            for fh in range(2):
                f0 = fh * 1408
                for (o, w) in ((0, 512), (512, 512), (1024, 384)):
                    groups.append((fh, f0 + o, w))

            def load_group(gi):
                fh, fo, w = groups[gi]
                P.dma("pool", gsl[gi % 2][:, :, 0:w], wg[:, :, fo:fo + w], wgrp(f"fg{gi % 2}"))
                P.dma("pool", usl[gi % 2][:, :, 0:w], wu[:, :, fo:fo + w], wgrp(f"fu{gi % 2}"))

            def load_down(fh_, dp_):
                di = fh_ * 4 + dp_
                P.dma("pool", dsl[di % 2], wd[:, fh_ * 11:(fh_ + 1) * 11, dp_ * 256:(dp_ + 1) * 256], wgrp(f"fd{di % 2}"))

            load_group(0)
            load_group(1)
            rmsnorm(l, 8, 96 * KB)
            sgi = 0
            for gi, (fh, fo, w) in enumerate(groups):
                for fc in range(w // 128):
                    fidx = (fo - fh * 1408) // 128 + fc
                    fcs = slice(fc * 128, (fc + 1) * 128)
                    for tb in range(NB):
                        sl = slice(tb * 512, (tb + 1) * 512)
                        bg = nextbank()
                        for k in range(8):
                            P.mm(ps[:, bg, :], lhsT=gsl[gi % 2][:, k, fcs], rhs=hn[:, k, sl], start=(k == 0), stop=(k == 7))
                        bu = nextbank()
                        for k in range(8):
                            P.mm(ps[:, bu, :], lhsT=usl[gi % 2][:, k, fcs], rhs=hn[:, k, sl], start=(k == 0), stop=(k == 7))
                        s_ = sg[sgi % 2]
                        sgi += 1
                        P.act(s_, ps[:, bg, :], AF.Silu)
                        P.tt("dve", ffT[:, fidx, sl], s_, ps[:, bu, :], ALU.mult)
                if gi + 2 < len(groups):
                    load_group(gi + 2)
                if gi % 3 == 2:
                    for dp in range(4):
                        if dp + 1 < 4:
                            load_down(fh, dp + 1)
                        slot = dsl[(fh * 4 + dp) % 2]
                        for dc in range(2):
                            d = dp * 2 + dc
                            for tb in range(NB):
                                sl = slice(tb * 512, (tb + 1) * 512)
                                b = nextbank()
                                for c in range(11):
                                    P.mm(ps[:, b, :], lhsT=slot[:, c, dc * 128:(dc + 1) * 128], rhs=ffT[:, c, sl],
                                         start=(c == 0), stop=(c == 10))
                                P.tt("dve", hT[:, d, sl], hT[:, d, sl], ps[:, b, :], ALU.add)
                elif gi % 3 == 1:
                    load_down(fh, 0)

        import os as _os
        stages = _os.environ.get("KSTAGES", "setup,n,da,gla,wo,ffn").split(",")
        if "setup" in stages:
            setup()
        for s in range(NSEQ):
            load_x(s)
            for l in layers:
                if "n" in stages:
                    rmsnorm(l, 0, T0)
                if "da" in stages:
                    da_phase(l)
                if "gla" in stages:
                    gla_pool_phase(l)
                if "wo" in stages:
                    wout_phase(l)
                if "ffn" in stages:
                    ffn_phase(l)
            store_out(s)
        P.emit(nc, es)
    return nc


def _host_prep(inp):
    f = lambda k: np.ascontiguousarray(np.asarray(inp[k], dtype=np.float32))
    depth = DEPTH
    p = np.arange(128)
    pvec = np.zeros((depth, 128, NPV), np.float32)
    ang, fng = f("attn_norm_g"), f("ffn_norm_g")
    qg, kg, sg_, gg = f("q_norm_g"), f("k_norm_g"), f("da_subln_g"), f("gla_norm_g")
    psc, lv, rb = f("pool_scale"), f("lambda_vecs"), f("rel_bias")
    for l in range(depth):
        pvec[l, :, 0:8] = ang[l].reshape(8, 128).T
        pvec[l, :, 8:16] = fng[l].reshape(8, 128).T
        pvec[l, :, 16] = qg[l][p % 64]
        pvec[l, :, 17] = kg[l][p % 64]
        pvec[l, :, 18] = sg_[l]
        pvec[l, :, 19] = gg[l][p % 64]
        pvec[l, :, 20:22] = psc[l].reshape(2, 128).T
        pvec[l, :, 22:26] = rb[31][None, :]
    gw17 = np.concatenate([f("gla_gate_w"), f("gla_gate_b")[:, None, :]], axis=1)
    pw = f("pool_w")
    pwbd = np.zeros((depth, 128, 2, 128), np.float32)
    for l in range(depth):
        for g in range(4):
            pt, half = divmod(g, 2)
            pwbd[l, half * 64:(half + 1) * 64, pt, half * 64:(half + 1) * 64] = pw[l, g]
    idx = _bias_index()
    bt = rb[idx]
    biasT = np.ascontiguousarray(np.transpose(bt, (0, 3, 1, 2))).reshape(128, 4 * 2 * 128)
    return {
        "w_in": f("w_in"), "w_out": f("w_out"), "w_gate": f("w_gate"), "w_up": f("w_up"), "w_down": f("w_down"),
        "pvec": pvec, "gw17": np.ascontiguousarray(gw17), "pwbd": pwbd.reshape(depth, 128, 256),
        "biasT": biasT, "cstf": _consts()[0], "cstb": _consts()[1],
        "lvb": np.ascontiguousarray(np.broadcast_to(lv.reshape(depth, 1, 256), (depth, 128, 256))),
    }


_CACHE = {}


def kernel(**inputs):
    x = np.ascontiguousarray(np.asarray(inputs["x"], dtype=np.float32))
    B, S, _ = x.shape
    nseq = B // N_CORES
    shared = _host_prep(inputs)
    key = (S, nseq)
    if key not in _CACHE:
        _CACHE[key] = build_program(S, nseq, list(range(DEPTH)))
    nc = _CACHE[key]
    in_maps = []
    for c in range(N_CORES):
        m = dict(shared)
        m["x"] = np.ascontiguousarray(x[c * nseq:(c + 1) * nseq])
        in_maps.append(m)
    res = run_bass_kernel_spmd(nc, in_maps, core_ids=list(range(N_CORES)))
    return np.concatenate([np.asarray(r["out"]) for r in res.results], axis=0).astype(np.float32)
```
